# Optimizing a Trainium2 kernel written in Bass

```python
import jax, jax.numpy as jnp
from jax import lax
import numpy as np

D_MODEL = 1024
BATCH = 16
SEQ = 2048
DEPTH = 2

CHUNK = 64
Q_BLOCK = 128
HEAD_DIM = 64
N_HEADS_FOX = 6
N_HEADS_SB = 5
N_HEADS_DSA = 5
W_FOX = N_HEADS_FOX * HEAD_DIM
W_SB = N_HEADS_SB * HEAD_DIM
W_DSA = N_HEADS_DSA * HEAD_DIM
KV_LATENT = 128
N_IDX_HEADS = 4
IDX_DIM = 64
TOPK_MAX = 256
ROPE_THETA = 500000.0
ROPE_DIM = HEAD_DIM // 4
D_FF = 2816
N_BRANCH = 3
EPS = 1e-6
D_IN = 3 * W_FOX + N_HEADS_FOX + 3 * W_SB + W_DSA + KV_LATENT + N_IDX_HEADS * IDX_DIM + IDX_DIM + N_IDX_HEADS + N_BRANCH * D_MODEL

kernel_name = "hybrid_fox_stickbreak_dsa_macaron"


def _in_sizes():
    return [W_FOX, W_FOX, W_FOX, N_HEADS_FOX,
            W_SB, W_SB, W_SB,
            W_DSA, KV_LATENT, N_IDX_HEADS * IDX_DIM, IDX_DIM, N_IDX_HEADS,
            N_BRANCH * D_MODEL]


def _split_points():
    pts, acc = [], 0
    for s in _in_sizes()[:-1]:
        acc += s
        pts.append(acc)
    return pts


def rmsnorm(x, g):
    xf = x.astype(jnp.float32)
    y = xf * lax.rsqrt(jnp.mean(xf * xf, axis=-1, keepdims=True) + EPS)
    return (y * g.astype(jnp.float32)).astype(x.dtype)


def swiglu(h, w_gu, w_down):
    g, u = jnp.split(h @ w_gu, 2, axis=-1)
    return (jax.nn.silu(g) * u) @ w_down


def rope_tables(seq):
    pos = jnp.arange(seq, dtype=jnp.float32)
    inv = ROPE_THETA ** (-jnp.arange(0, ROPE_DIM, 2, dtype=jnp.float32) / ROPE_DIM)
    ang = pos[:, None] * inv[None, :]
    return jnp.cos(ang), jnp.sin(ang)


def partial_rope(x, cos, sin):
    half = ROPE_DIM // 2
    x1 = x[..., :half].astype(jnp.float32)
    x2 = x[..., half:ROPE_DIM].astype(jnp.float32)
    c = cos[None, :, None, :]
    s = sin[None, :, None, :]
    r1 = (x1 * c - x2 * s).astype(x.dtype)
    r2 = (x2 * c + x1 * s).astype(x.dtype)
    return jnp.concatenate([r1, r2, x[..., ROPE_DIM:]], axis=-1)


def forgetting_attention(q, k, v, f_logit):
    B, S, H, Dh = q.shape
    scale = Dh ** -0.5
    c = jnp.cumsum(jax.nn.log_sigmoid(f_logit.astype(jnp.float32)), axis=1)
    c = c.transpose(0, 2, 1)
    outs = []
    for start in range(0, S, Q_BLOCK):
        end = start + Q_BLOCK
        s = jnp.einsum('bqhd,bkhd->bhqk', q[:, start:end], k[:, :end]).astype(jnp.float32) * scale
        bias = c[:, :, start:end, None] - c[:, :, None, :end]
        t_pos = jnp.arange(start, end)
        s_pos = jnp.arange(end)
        mask = s_pos[None, :] <= t_pos[:, None]
        p = jax.nn.softmax(jnp.where(mask, s + bias, -jnp.inf), axis=-1)
        outs.append(jnp.einsum('bhqk,bkhd->bqhd', p.astype(v.dtype), v[:, :end]))
    return jnp.concatenate(outs, axis=1)


def stick_breaking_attention(q, k, v):
    B, S, H, Dh = q.shape
    scale = Dh ** -0.5
    outs = []
    for start in range(0, S, Q_BLOCK):
        end = start + Q_BLOCK
        z = jnp.einsum('bqhd,bkhd->bhqk', q[:, start:end], k[:, :end]).astype(jnp.float32) * scale
        t_pos = jnp.arange(start, end)
        s_pos = jnp.arange(end)
        mask = s_pos[None, :] < t_pos[:, None]
        log_1m = jnp.where(mask, jax.nn.log_sigmoid(-z), 0.0)
        acc = lax.cumsum(log_1m, axis=3, reverse=True) - log_1m
        a = jnp.where(mask, jnp.exp(jax.nn.log_sigmoid(z) + acc), 0.0)
        outs.append(jnp.einsum('bhqk,bkhd->bqhd', a.astype(v.dtype), v[:, :end]))
    return jnp.concatenate(outs, axis=1)


def dsa_attention(q, k, v, iq, ik, iw):
    B, S, H, Dh = q.shape
    scale = Dh ** -0.5
    chunk_id = jnp.arange(S) // CHUNK
    topk = min(TOPK_MAX, S // 4)
    outs = []
    for start in range(0, S, Q_BLOCK):
        end = start + Q_BLOCK
        kk = min(topk, end)
        q_chunk = chunk_id[start:end]
        rel = jax.nn.relu(jnp.einsum('bqhd,bkd->bqhk', iq[:, start:end], ik[:, :end]).astype(jnp.float32))
        score = jnp.einsum('bqhk,bqh->bqk', rel, iw[:, start:end].astype(jnp.float32))
        admissible = chunk_id[None, :end] <= q_chunk[:, None]
        score = jnp.where(admissible[None], score, -jnp.inf)
        _, sel = lax.top_k(score, kk)
        valid = chunk_id[sel] <= q_chunk[None, :, None]
        k_sel = jax.vmap(lambda kb, ib: kb[ib])(k[:, :end], sel)
        v_sel = jax.vmap(lambda vb, ib: vb[ib])(v[:, :end], sel)
        s = jnp.einsum('bqhd,bqkd->bqhk', q[:, start:end], k_sel).astype(jnp.float32) * scale
        p = jax.nn.softmax(jnp.where(valid[:, :, None, :], s, -jnp.inf), axis=-1)
        outs.append(jnp.einsum('bqhk,bqkd->bqhd', p.astype(v.dtype), v_sel))
    return jnp.concatenate(outs, axis=1)


def hybrid_mixer(h, w_in, b_forget, g_kv, w_kv_up, g_idx_k, w_up_fox, w_up_sb, w_up_dsa, w_out, cos, sin):
    B, S, _ = h.shape
    (q_a, k_a, v_a, f_a, q_b, k_b, v_b, q_c, c_kv, q_i, k_i, w_i, gates) = jnp.split(h @ w_in, _split_points(), axis=-1)
    heads = lambda t, n: t.reshape(B, S, n, HEAD_DIM)
    y_a = forgetting_attention(heads(q_a, N_HEADS_FOX), heads(k_a, N_HEADS_FOX), heads(v_a, N_HEADS_FOX), f_a + b_forget)
    y_b = stick_breaking_attention(heads(q_b, N_HEADS_SB), heads(k_b, N_HEADS_SB), heads(v_b, N_HEADS_SB))
    k_c, v_c = jnp.split(rmsnorm(c_kv, g_kv) @ w_kv_up, 2, axis=-1)
    q_c = partial_rope(heads(q_c, N_HEADS_DSA), cos, sin)
    k_c = partial_rope(k_c[:, :, None, :], cos, sin)[:, :, 0]
    q_i = partial_rope(q_i.reshape(B, S, N_IDX_HEADS, IDX_DIM), cos, sin)
    k_i = partial_rope(rmsnorm(k_i, g_idx_k)[:, :, None, :], cos, sin)[:, :, 0]
    y_c = dsa_attention(q_c, k_c, v_c, q_i, k_i, w_i)
    g_a, g_b, g_c = jnp.split(jax.nn.sigmoid(gates), N_BRANCH, axis=-1)
    merged = (g_a * (y_a.reshape(B, S, W_FOX) @ w_up_fox)
              + g_b * (y_b.reshape(B, S, W_SB) @ w_up_sb)
              + g_c * (y_c.reshape(B, S, W_DSA) @ w_up_dsa))
    return merged @ w_out


def setup_inputs(seed: int = 0) -> dict:
    key = jax.random.key(seed)
    ks = jax.random.split(key, 20)
    f32 = jnp.float32
    nrm = lambda k, shape, fan_in: jax.random.normal(k, shape, f32) * (fan_in ** -0.5)
    gain = lambda k, shape: 1.0 + 0.02 * jax.random.normal(k, shape, f32)
    return {
        "x": jax.random.normal(ks[0], (BATCH, SEQ, D_MODEL), f32),
        "g_ffn1": gain(ks[1], (DEPTH, D_MODEL)),
        "w_ffn1_gu": nrm(ks[2], (DEPTH, D_MODEL, 2 * D_FF), D_MODEL),
        "w_ffn1_down": nrm(ks[3], (DEPTH, D_FF, D_MODEL), D_FF),
        "g_mix": gain(ks[4], (DEPTH, D_MODEL)),
        "w_in": nrm(ks[5], (DEPTH, D_MODEL, D_IN), D_MODEL),
        "b_forget": 2.0 + 0.5 * jax.random.normal(ks[6], (DEPTH, N_HEADS_FOX), f32),
        "g_kv_latent": gain(ks[7], (DEPTH, KV_LATENT)),
        "w_kv_up": nrm(ks[8], (DEPTH, KV_LATENT, 2 * HEAD_DIM), KV_LATENT),
        "g_idx_k": gain(ks[9], (DEPTH, IDX_DIM)),
        "w_up_fox": nrm(ks[10], (DEPTH, W_FOX, D_MODEL), W_FOX),
        "w_up_sb": nrm(ks[11], (DEPTH, W_SB, D_MODEL), W_SB),
        "w_up_dsa": nrm(ks[12], (DEPTH, W_DSA, D_MODEL), W_DSA),
        "w_out": nrm(ks[13], (DEPTH, D_MODEL, D_MODEL), D_MODEL),
        "g_ffn2": gain(ks[14], (DEPTH, D_MODEL)),
        "w_ffn2_gu": nrm(ks[15], (DEPTH, D_MODEL, 2 * D_FF), D_MODEL),
        "w_ffn2_down": nrm(ks[16], (DEPTH, D_FF, D_MODEL), D_FF),
        "g_final": gain(ks[17], (D_MODEL,)),
    }


def reference(x, g_ffn1, w_ffn1_gu, w_ffn1_down, g_mix, w_in, b_forget, g_kv_latent, w_kv_up, g_idx_k,
              w_up_fox, w_up_sb, w_up_dsa, w_out, g_ffn2, w_ffn2_gu, w_ffn2_down, g_final):
    cos, sin = rope_tables(x.shape[1])
    for l in range(DEPTH):
        x = x + 0.5 * swiglu(rmsnorm(x, g_ffn1[l]), w_ffn1_gu[l], w_ffn1_down[l])
        x = x + hybrid_mixer(rmsnorm(x, g_mix[l]), w_in[l], b_forget[l], g_kv_latent[l], w_kv_up[l], g_idx_k[l],
                             w_up_fox[l], w_up_sb[l], w_up_dsa[l], w_out[l], cos, sin)
        x = x + 0.5 * swiglu(rmsnorm(x, g_ffn2[l]), w_ffn2_gu[l], w_ffn2_down[l])
    return rmsnorm(x, g_final)
```

```python
import numpy as np
import concourse.bass as bass
import concourse.mybir as mybir
from concourse.bass_utils import run_bass_kernel_spmd

F32 = mybir.dt.float32
BF16 = mybir.dt.bfloat16
U8 = mybir.dt.uint8
AF = mybir.ActivationFunctionType
ALU = mybir.AluOpType

D = 1024
S = 2048
DEPTH = 2
DFF = 2816
NFF = DFF // 128
HD = 64
NH_A, NH_B, NH_C = 6, 5, 5
W_A, W_B, W_C = 384, 320, 320
KVL = 128
NIH = 4
D_IN = 5962
EPS = 1e-6
TB = 512
NTB = S // TB
NQB = S // 128

O_QA = 0
O_KA = O_QA + W_A
O_VA = O_KA + W_A
O_FA = O_VA + W_A
O_QB = O_FA + NH_A
O_KB = O_QB + W_B
O_VB = O_KB + W_B
O_QC = O_VB + W_B
O_CKV = O_QC + W_C
O_QI = O_CKV + KVL
O_KI = O_QI + NIH * 64
O_WI = O_KI + 64
O_G = O_WI + NIH
assert O_G + 3 * D == D_IN

EPOCH = 4096
ENGS = ['pe', 'act', 'dve', 'pool', 'sp']


class Buf:
    __slots__ = ('name', 'lw', 'rd')

    def __init__(self, name):
        self.name = name
        self.lw = None
        self.rd = {}


class Prog:
    NRING = 8

    def __init__(self):
        self.ops = {e: [] for e in ENGS}
        self.waited = {e: {} for e in ENGS}
        self.ndma = {e: 0 for e in ENGS}
        self.bufs = {}

    def buf(self, name):
        b = self.bufs.get(name)
        if b is None:
            b = Buf(name)
            self.bufs[name] = b
        return b

    def _tok(self, names):
        return [self.buf(n) if isinstance(n, str) else n for n in names]

    def op(self, eng, emit, reads=(), writes=(), dma=False):
        reads = self._tok(reads)
        writes = self._tok(writes)
        idx = len(self.ops[eng])
        deps = set()
        for b in reads:
            if b.lw is not None:
                deps.add(b.lw)
        for b in writes:
            if b.lw is not None:
                deps.add(b.lw)
            for t in b.rd.values():
                deps.add(t)
        if dma:
            d = self.ndma[eng]
            self.ndma[eng] += 1
            tok = ('d', eng, d)
            if d >= self.NRING:
                deps.add(('d', eng, d - self.NRING))
        else:
            tok = ('c', eng, idx)
        waits = []
        w = self.waited[eng]
        for t in deps:
            if t[0] == 'c':
                _, e, i = t
                if e == eng and eng == 'pe':
                    continue
                if w.get(('c', e), -1) >= i:
                    continue
                w[('c', e)] = i
                self.ops[e][i][2] = True
                waits.append(t)
            else:
                _, q, d0 = t
                key = ('d', q, d0 % self.NRING)
                if w.get(key, -1) >= d0:
                    continue
                w[key] = d0
                waits.append(t)
        self.ops[eng].append([emit, waits, False, tok if dma else None])
        rkey = (tok[0], tok[1]) if tok[0] == 'c' else (tok[0], tok[1], tok[2] % self.NRING)
        for b in reads:
            b.rd[rkey] = tok
        for b in writes:
            b.lw = tok
            b.rd = {}
        return tok

    def barrier(self):
        last = {}
        for e in ENGS:
            for i in range(len(self.ops[e]) - 1, -1, -1):
                o = self.ops[e][i]
                if o[0] is not None and o[3] is None:
                    last[e] = i
                    break
        for eng in ENGS:
            waits = []
            w = self.waited[eng]
            for e, i in last.items():
                if e == eng:
                    continue
                if w.get(('c', e), -1) >= i:
                    continue
                w[('c', e)] = i
                self.ops[e][i][2] = True
                waits.append(('c', e, i))
            for q in ENGS:
                n = self.ndma[q]
                for d0 in range(max(0, n - self.NRING), n):
                    key = ('d', q, d0 % self.NRING)
                    if w.get(key, -1) >= d0:
                        continue
                    w[key] = d0
                    waits.append(('d', q, d0))
            self.ops[eng].append([None, waits, False, None])
        for b in self.bufs.values():
            b.lw = None
            b.rd = {}

    def wait_all_dma(self, eng):
        waits = []
        for q in ENGS:
            n = self.ndma[q]
            for d in range(max(0, n - self.NRING), n):
                waits.append(('d', q, d))
        self.ops[eng].append([None, waits, False, None])

    def emit(self, nc, block_cm):
        cnt = {}
        nsem = {}
        for e in ENGS:
            c = 0
            arr = []
            for o in self.ops[e]:
                if o[2]:
                    c += 1
                arr.append(c)
            cnt[e] = arr
            nsem[e] = (c + EPOCH - 1) // EPOCH
        csem = {e: [nc.alloc_semaphore(name=f"c_{e}_{k}") for k in range(nsem[e])] for e in ENGS}
        dsem = {e: [nc.alloc_semaphore(name=f"d_{e}_{k}") for k in range(self.NRING)]
                for e in ENGS if self.ndma[e] > 0}

        def resolve(t):
            if t[0] == 'c':
                _, e, i = t
                c = cnt[e][i]
                return csem[e][(c - 1) // EPOCH], (c - 1) % EPOCH + 1
            _, q, d0 = t
            return dsem[q][d0 % self.NRING], 16 * (d0 // self.NRING + 1)

        prog = self

        def run(e, eng):
            for k, (emit, waits, marked, dtok) in enumerate(prog.ops[e]):
                for t in waits:
                    s, v = resolve(t)
                    eng.wait_ge(s, v)
                if emit is None:
                    continue
                ins = emit(eng)
                if dtok is not None:
                    s, _ = resolve(dtok)
                    ins.then_inc(s, 16)
                elif marked:
                    c = cnt[e][k]
                    ins.then_inc(csem[e][(c - 1) // EPOCH], 1)

        with block_cm as block:
            @block.tensor
            def _(eng):
                run('pe', eng)

            @block.scalar
            def _(eng):
                run('act', eng)

            @block.vector
            def _(eng):
                run('dve', eng)

            @block.gpsimd
            def _(eng):
                run('pool', eng)

            @block.sync
            def _(eng):
                run('sp', eng)


class Arena:
    def __init__(self, ap_u8, nbytes):
        self.ap = ap_u8
        self.nbytes = nbytes

    def carve(self, off, parts, free_shape, dtype, pbase=0):
        esz = 4 if dtype == F32 else 2
        n = 1
        for s in free_shape:
            n *= s
        assert off % 4 == 0 and off + n * esz <= self.nbytes, (off, n * esz, self.nbytes)
        a = self.ap[pbase:pbase + parts, off:off + n * esz].bitcast(dtype)
        if len(free_shape) == 2:
            a = a.rearrange('p (a b) -> p a b', a=free_shape[0])
        elif len(free_shape) == 3:
            a = a.rearrange('p (a b c) -> p a b c', a=free_shape[0], b=free_shape[1])
        return a


OFF_X = 0
OFF_H = OFF_X + 8 * S * 4
OFF_C = OFF_H + 8 * S * 2
OFF_Y = OFF_C + 10240
OFF_A = OFF_Y + 8 * S * 2
SB_BYTES = 212800
ARENA_BYTES = SB_BYTES - OFF_A
NGAIN = 8 * (3 * DEPTH + 1)
NCBF = 2944
NCF32 = 272


class _Stop(Exception):
    pass


class Model:
    def __init__(self, n_seq=2, depth=DEPTH, stage='full'):
        self.n_seq = n_seq
        self.depth = depth
        self.stage = stage
        nc = bass.Bass("TRN2", target_bir_lowering=False)
        self.nc = nc
        dt = nc.dram_tensor
        self.xT = dt("xT", [n_seq, D, S], F32, kind="ExternalInput").ap()
        self.outT = dt("outT", [n_seq, D, S], F32, kind="ExternalOutput").ap()
        self.w_gu = [dt(f"w_ffn{i}_gu", [DEPTH, D, 2 * DFF], F32, kind="ExternalInput").ap() for i in (1, 2)]
        self.w_dn = [dt(f"w_ffn{i}_down", [DEPTH, DFF, D], F32, kind="ExternalInput").ap() for i in (1, 2)]
        self.w_in = dt("w_in", [DEPTH, D, D_IN], F32, kind="ExternalInput").ap()
        self.w_kv_up = dt("w_kv_up", [DEPTH, KVL, 2 * HD], F32, kind="ExternalInput").ap()
        self.w_up_fox = dt("w_up_fox", [DEPTH, W_A, D], F32, kind="ExternalInput").ap()
        self.w_up_sb = dt("w_up_sb", [DEPTH, W_B, D], F32, kind="ExternalInput").ap()
        self.w_up_dsa = dt("w_up_dsa", [DEPTH, W_C, D], F32, kind="ExternalInput").ap()
        self.w_out = dt("w_out", [DEPTH, D, D], F32, kind="ExternalInput").ap()
        self.gains = dt("gains", [128, NGAIN], F32, kind="ExternalInput").ap()
        self.smallp = dt("smallp", [128, 256], F32, kind="ExternalInput").ap()
        self.rope = dt("rope", [128, 2 * S], F32, kind="ExternalInput").ap()
        self.cbf = dt("cbf", [128, NCBF], F32, kind="ExternalInput").ap()
        self.cf32 = dt("cf32", [128, NCF32], F32, kind="ExternalInput").ap()
        self.P = Prog()

    def build(self):
        nc = self.nc
        P = self.P
        with nc.sbuf_tensor("sb", [128, SB_BYTES], U8) as sb:
            self.psum_cms = [nc.psum_tensor(f"ps{k}", [128, 512], F32) for k in range(8)]
            self.ps = [cm.__enter__()[:] for cm in self.psum_cms]
            A = Arena(sb, SB_BYTES)
            self.A = A
            self.X = A.carve(OFF_X, 128, [8, S], F32)
            self.H = A.carve(OFF_H, 128, [8, S], BF16)
            self.Y = A.carve(OFF_Y, 128, [8, S], BF16)
            o = OFF_C
            self.gain_sb = A.carve(o, 128, [NGAIN], F32); o += NGAIN * 4
            self.small_sb = A.carve(o, 128, [256], F32); o += 1024
            self.cbf_sb = A.carve(o, 128, [NCBF], BF16); o += NCBF * 2
            self.cf32_sb = A.carve(o, 128, [NCF32], F32); o += NCF32 * 4
            cb = self.cbf_sb
            self.tri_incl = cb[:, 0:128]
            self.smask = [cb[:, 128 + 512 * k: 128 + 512 * (k + 1)] for k in range(4)]
            self.negtri = cb[:, 2176:2304]
            self.negones = cb[:, 2304:2432]
            self.ident = cb[:, 2432:2560]
            self.ones_mean = cb[:, 2560:2688]
            self.ones_128th = cb[:, 2688:2816]
            self.ones_64th = cb[:, 2816:2944]
            self.tri_f32 = self.cf32_sb[:, 0:128]
            self.ones_f32 = self.cf32_sb[:, 128:256]
            self.pow2 = self.cf32_sb[:, 256:272]
            self.cst = A.carve(o, 128, [16], F32); o += 64
            self.c_off = o
            assert o <= OFF_C + 10240, o
            self.consts()
            for s in range(self.n_seq):
                self.load_x(s)
                try:
                    for l in range(self.depth):
                        self.ffn(l, 0)
                        if self.stage == 'ffn1':
                            break
                        self.mixer(l)
                        self.ffn(l, 1)
                except _Stop:
                    pass
                P.barrier()
                self.final(s)
                P.barrier()
            P.wait_all_dma('sp')
            P.emit(nc, nc.Block())
            for cm in reversed(self.psum_cms):
                cm.__exit__(None, None, None)
        return nc

    def consts(self):
        P = self.P
        P.op('pool', lambda e: e.dma_start(out=self.cbf_sb, in_=self.cbf), writes=['ones_mean'], dma=True)
        P.op('sp', lambda e: e.dma_start(out=self.cf32_sb, in_=self.cf32), writes=['cf32'], dma=True)
        P.op('pool', lambda e: e.memset(self.cst[:, 0:1], EPS), writes=['cst'])
        P.op('pool', lambda e: e.memset(self.cst[:, 1:2], 1.0), writes=['cst'])
        P.op('pool', lambda e: e.memset(self.cst[:, 2:3], 0.0), writes=['cst'])
        g = self.gain_sb
        P.op('sp', lambda e: e.dma_start(out=g, in_=self.gains), writes=['gains'], dma=True)
        sm = self.small_sb
        P.op('sp', lambda e: e.dma_start(out=sm, in_=self.smallp), writes=['smallp'], dma=True)

    def load_x(self, s):
        P = self.P
        for c in range(8):
            src = self.xT[s, c * 128:(c + 1) * 128, :]
            dst = self.X[:, c, :]
            P.op('sp', lambda e, src=src, dst=dst: e.dma_start(out=dst, in_=src),
                 writes=[f'x{c}_{t}' for t in range(NTB)], dma=True)

    def rmsnorm(self, gi, out_fn, tmp_off, t_list=None):
        P = self.P
        A = self.A
        sq = A.carve(tmp_off, 128, [2, 8, TB], BF16)
        rstd = A.carve(tmp_off + 2 * 8 * TB * 2, 128, [2, TB], F32)
        for t in (range(NTB) if t_list is None else t_list):
            k = t % 2
            ts = slice(t * TB, (t + 1) * TB)
            xin = self.X[:, :, ts]
            sqk = sq[:, k]
            P.op('pool', lambda e, xin=xin, sqk=sqk: e.tensor_tensor(out=sqk, in0=xin, in1=xin, op=ALU.mult),
                 reads=[f'x{c}_{t}' for c in range(8)], writes=[f'sq{k}'])
            ps = self.ps[6 + k]
            for c in range(8):
                P.op('pe', lambda e, ps=ps, c=c, sqk=sqk: e.matmul(ps, self.ones_mean, sqk[:, c, :],
                                                                   start=(c == 0), stop=(c == 7)),
                     reads=[f'sq{k}', 'ones_mean'], writes=[f'ps{6 + k}'])
            rk = rstd[:, k]
            P.op('act', lambda e, ps=ps, rk=rk: e.activation(out=rk, in_=ps, func=AF.Ln, bias=self.cst[:, 0:1]),
                 reads=[f'ps{6 + k}', 'cst'], writes=[f'rstd{k}'])
            P.op('act', lambda e, rk=rk: e.activation(out=rk, in_=rk, func=AF.Exp, scale=-0.5),
                 reads=[f'rstd{k}'], writes=[f'rstd{k}'])
            for c in range(8):
                dst, bname = out_fn(c, t)
                xin_c = self.X[:, c, ts]
                gcol = self.gain_sb[:, gi * 8 + c: gi * 8 + c + 1]
                eng = 'dve'
                P.op(eng, lambda e, dst=dst, xin_c=xin_c, gcol=gcol, rk=rk:
                     e.scalar_tensor_tensor(out=dst, in0=xin_c, scalar=gcol, in1=rk, op0=ALU.mult, op1=ALU.mult),
                     reads=[f'x{c}_{t}', f'rstd{k}', 'gains'], writes=[bname])

    def norm_to_H(self, gi, tmp_off):
        self.rmsnorm(gi, lambda c, t: (self.H[:, c, t * TB:(t + 1) * TB], f'h_{t}'), tmp_off)

    def ffn(self, l, which):
        P = self.P
        A = self.A
        gi = l * 3 + (0 if which == 0 else 2)
        o = OFF_A
        actT = A.carve(o, 128, [NFF // 2, S], BF16); o += (NFF // 2) * S * 2
        wgu = [A.carve(o + k * 4096, 128, [8, 2, 128], BF16) for k in range(2)]; o += 8192
        wdn = [A.carve(o + k * 2816, 128, [NFF // 2, 128], BF16) for k in range(2)]; o += 2 * 2816
        sg = [A.carve(o + k * 2048, 128, [TB], F32) for k in range(2)]; o += 4096
        assert o <= SB_BYTES, o
        P.barrier()
        self.norm_to_H(gi, OFF_A)
        P.barrier()
        self.chk(f'f{which}norm')
        w_gu = self.w_gu[which][l].rearrange("(kc p) (two n) -> p kc two n", p=128, two=2)
        w_dn = self.w_dn[which][l].rearrange("(j p) n -> p j n", p=128)
        NH = NFF // 2
        cnt = 0
        for half in range(2):
            for jj in range(NH):
                j = half * NH + jj
                wb = cnt % 2
                dst = wgu[wb]
                for two in range(2):
                    src = w_gu[:, :, two, j * 128:(j + 1) * 128]
                    dd = dst[:, :, two, :]
                    P.op('pool', lambda e, src=src, dd=dd: e.dma_start(out=dd, in_=src),
                         writes=[f'wgu{wb}_{two}'], dma=True)
                for t in range(NTB):
                    pb = (cnt * NTB + t) % 2
                    ts = slice(t * TB, (t + 1) * TB)
                    pg, pu = self.ps[pb], self.ps[2 + pb]
                    for kc in range(8):
                        P.op('pe', lambda e, pg=pg, dst=dst, kc=kc, ts=ts: e.matmul(
                            pg, dst[:, kc, 0, :], self.H[:, kc, ts], start=(kc == 0), stop=(kc == 7)),
                            reads=[f'wgu{wb}_0', f'h_{t}'], writes=[f'ps{pb}'])
                    for kc in range(8):
                        P.op('pe', lambda e, pu=pu, dst=dst, kc=kc, ts=ts: e.matmul(
                            pu, dst[:, kc, 1, :], self.H[:, kc, ts], start=(kc == 0), stop=(kc == 7)),
                            reads=[f'wgu{wb}_1', f'h_{t}'], writes=[f'ps{2 + pb}'])
                    sgk = sg[pb]
                    P.op('act', lambda e, sgk=sgk, pg=pg: e.activation(out=sgk, in_=pg, func=AF.Silu),
                         reads=[f'ps{pb}'], writes=[f'sg{pb}'])
                    adst = actT[:, jj, ts]
                    P.op('dve', lambda e, adst=adst, sgk=sgk, pu=pu: e.tensor_tensor(
                        out=adst, in0=sgk, in1=pu, op=ALU.mult),
                        reads=[f'sg{pb}', f'ps{2 + pb}'], writes=[f'act{jj}_{t}'])
                cnt += 1
            for dc in range(8):
                db = dc % 2
                src = w_dn[:, half * NH:(half + 1) * NH, dc * 128:(dc + 1) * 128]
                dst = wdn[db]
                P.op('pool', lambda e, src=src, dst=dst: e.dma_start(out=dst, in_=src),
                     writes=[f'wdn{db}'], dma=True)
                for t in range(NTB):
                    pb = 4 + (dc * NTB + t) % 2
                    ts = slice(t * TB, (t + 1) * TB)
                    po = self.ps[pb]
                    for jj in range(NH):
                        P.op('pe', lambda e, po=po, dst=dst, jj=jj, ts=ts: e.matmul(
                            po, dst[:, jj, :], actT[:, jj, ts], start=(jj == 0), stop=(jj == NH - 1)),
                            reads=[f'wdn{db}', f'act{jj}_{t}'], writes=[f'ps{pb}'])
                    xd = self.X[:, dc, ts]
                    P.op('dve', lambda e, xd=xd, po=po: e.scalar_tensor_tensor(
                        out=xd, in0=po, scalar=0.5, in1=xd, op0=ALU.mult, op1=ALU.add),
                        reads=[f'ps{pb}', f'x{dc}_{t}'], writes=[f'x{dc}_{t}'])
        self.chk(f'f{which}end')

    def chk(self, name):
        if self.stage.rstrip('XYH') == name:
            raise _Stop()

    def load_w(self, slot, src, ncols, name, dcol=0):
        dst = slot[:, :, dcol:dcol + ncols]
        self.P.op('pool', lambda e, src=src, dst=dst: e.dma_start(out=dst, in_=src), writes=[name], dma=True)

    def proj_T(self, slot, wname, M, evac, banks=(0, 1)):
        P = self.P
        for T in range(NTB):
            b = banks[T % 2]
            ps = self.ps[b]
            ts = slice(T * TB, (T + 1) * TB)
            for kc in range(8):
                P.op('pe', lambda e, ps=ps, kc=kc, ts=ts: e.matmul(ps[0:M, :], slot[:, kc, 0:M], self.H[:, kc, ts],
                                                                  start=(kc == 0), stop=(kc == 7)),
                     reads=[wname, f'h_{T}'], writes=[f'ps{b}'])
            evac(T, ps, f'ps{b}')

    def proj_tok(self, slot, wname, N, evac, banks=(0, 1)):
        P = self.P
        for g in range(4):
            b = banks[g % 2]
            ps = self.ps[b]
            for cc in range(4):
                ch = 4 * g + cc
                for kc in range(8):
                    P.op('pe', lambda e, ps=ps, kc=kc, ch=ch, cc=cc: e.matmul(
                        ps[:, cc * 128:cc * 128 + N], self.H[:, kc, ch * 128:(ch + 1) * 128], slot[:, kc, 0:N],
                        start=(kc == 0), stop=(kc == 7)),
                        reads=[wname, f'h_{ch // 4}'], writes=[f'ps{b}'])
            self.chk('tok_mm')
            evac(g, ps.rearrange("p (a b) -> p a b", a=4), f'ps{b}')
            self.chk('tok_ev')

    def attn_finish(self, O, oname, hh, ychunk, T, normalize, rden, bcs, width=TB):
        P = self.P
        pb = 64 * hh
        ts = slice(T * width, (T + 1) * width)
        ydst = self.Y[pb:pb + 64, ychunk, ts]
        yname = f'y{ychunk}_{hh}_{T}_{width}'
        O = O[:, 0:width]
        rden = rden[:, 0:width]
        bcs = bcs[:, 0:width]
        if not normalize:
            P.op('act', lambda e: e.copy(out=ydst, in_=O[pb:pb + 64, :]), reads=[oname], writes=[yname])
            return
        p = 64 if hh == 0 else 0
        P.op('dve', lambda e: e.reciprocal(out=rden[p:p + 1, :], in_=O[p:p + 1, :]), reads=[oname], writes=['rden'])
        BC = self.ps[6][:, 0:width]
        P.op('pe', lambda e: e.matmul(BC, self.ones_f32[p:p + 1, :], rden[p:p + 1, :], start=True, stop=True),
             reads=['rden'], writes=['ps6'])
        P.op('act', lambda e: e.copy(out=bcs[pb:pb + 64, :], in_=BC[pb:pb + 64, :]), reads=['ps6'], writes=['bcs'])
        P.op('dve', lambda e: e.tensor_tensor(out=ydst, in0=O[pb:pb + 64, :], in1=bcs[pb:pb + 64, :], op=ALU.mult),
             reads=[oname, 'bcs'], writes=[yname])

    def mixer(self, l):
        P = self.P
        A = self.A
        P.barrier()
        self.norm_to_H(l * 3 + 1, OFF_A)
        P.barrier()
        self.chk('norm')
        w_in = self.w_in[l].rearrange("(kc p) n -> p kc n", p=128)
        o = [OFF_A]

        def take(n):
            r = o[0]
            o[0] += n
            assert o[0] <= SB_BYTES, o[0]
            return r
        WSL = [A.carve(take(2048), 128, [8, 128], BF16) for _ in range(6)]
        qT = A.carve(take(4096), 128, [S], BF16)
        kT = A.carve(take(4096), 128, [S], BF16)
        Vp = A.carve(take(16 * 192 * 2), 128, [16, 192], BF16)
        Pt = [A.carve(take(1024), 128, [512], BF16) for _ in range(4)]
        rden = A.carve(take(2048), 128, [512], F32)
        bcs = A.carve(take(2048), 128, [512], F32)
        base_common = o[0]
        self.cnt = 0
        self.ocnt = 0

        P.op('pool', lambda e: e.memset(Vp[:, :, 64:65], 1.0), writes=['Vp_c'])
        P.op('pool', lambda e: e.memset(Vp[:, :, 65:128], 0.0), writes=['Vp_c'])

        def proj_qkv(qcol, kcol, vcol, nc_, wi):
            s0, s1, s2 = WSL[wi], WSL[wi + 1], WSL[wi + 2]
            self.load_w(s0, w_in[:, :, qcol:qcol + nc_], nc_, f'wsl{wi}')
            self.load_w(s1, w_in[:, :, kcol:kcol + nc_], nc_, f'wsl{wi + 1}')
            self.load_w(s2, w_in[:, :, vcol:vcol + nc_], nc_, f'wsl{wi + 2}')
            self.proj_T(s0, f'wsl{wi}', nc_, lambda T, ps, pn: P.op(
                'dve', lambda e: e.tensor_scalar(out=qT[0:nc_, T * TB:(T + 1) * TB], in0=ps[0:nc_, :], scalar1=0.125,
                                                 scalar2=None, op0=ALU.mult),
                reads=[pn], writes=[f'qT_{T}']))
            self.chk('projq')
            self.proj_T(s1, f'wsl{wi + 1}', nc_, lambda T, ps, pn: P.op(
                'dve', lambda e: e.tensor_copy(out=kT[0:nc_, T * TB:(T + 1) * TB], in_=ps[0:nc_, :]),
                reads=[pn], writes=[f'kT_{T}']))
            self.chk('projk')

            import os
            VAR = os.environ.get('EVVAR', 'ab')

            def ev(g, ps3, pn):
                if 'a' in VAR:
                  P.op('dve', lambda e: e.tensor_copy(out=Vp[:, 4 * g:4 * g + 4, 0:64], in_=ps3[:, :, 0:64]),
                     reads=[pn], writes=[f'Vp_{g}a'])
                if nc_ > 64 and 'b' in VAR:
                    P.op('dve', lambda e: e.tensor_copy(out=Vp[:, 4 * g:4 * g + 4, 128:192], in_=ps3[:, :, 64:128]),
                         reads=[pn], writes=[f'Vp_{g}b'])
            self.proj_tok(s2, f'wsl{wi + 2}', nc_, ev)
            self.chk('proj')

        ls = A.carve(take(384), 128, [16, 6], F32)
        tot = A.carve(take(384), 128, [16, 6], F32)
        pre = A.carve(take(17 * 24), 128, [17, 6], F32)
        cpos = A.carve(take(384), 128, [16, 6], F32)
        Btab = A.carve(take(6 * 256 * 4), 128, [6, 16, 16], F32)
        base_fox = o[0]
        self.load_w(WSL[5], w_in[:, :, O_FA:O_FA + 6], 6, 'wsl5')
        ps7 = self.ps[7]
        for ch in range(16):
            for kc in range(8):
                P.op('pe', lambda e, ch=ch, kc=kc: e.matmul(ps7[:, ch * 6:(ch + 1) * 6],
                                                              self.H[:, kc, ch * 128:(ch + 1) * 128],
                                                              WSL[5][:, kc, 0:6], start=(kc == 0), stop=(kc == 7)),
                     reads=['wsl5', f'h_{ch // 4}'], writes=['ps7'])
        lsf = ls.rearrange("p a b -> p (a b)")
        P.op('dve', lambda e: e.tensor_tensor(out=lsf, in0=ps7[:, 0:96], in1=self.small_sb[:, l * 96:(l + 1) * 96],
                                              op=ALU.add), reads=['ps7'], writes=['ls'])
        P.op('act', lambda e: e.activation(out=lsf, in_=lsf, func=AF.Exp, scale=-1.0), reads=['ls'], writes=['ls'])
        P.op('act', lambda e: e.activation(out=lsf, in_=lsf, func=AF.Ln, bias=self.cst[:, 1:2]),
             reads=['ls'], writes=['ls'])
        ps6 = self.ps[6]
        P.op('pe', lambda e: e.matmul(ps6[:, 0:96], self.tri_f32, lsf, start=True, stop=True),
             reads=['ls'], writes=['ps6'])
        P.op('pe', lambda e: e.matmul(ps6[:, 128:224], self.ones_f32, lsf, start=True, stop=True),
             reads=['ls'], writes=['ps6'])
        P.op('dve', lambda e: e.tensor_copy(out=tot.rearrange("p a b -> p (a b)"), in_=ps6[:, 128:224]),
             reads=['ps6'], writes=['tot'])
        P.op('dve', lambda e: e.memset(pre[:, 0, :], 0.0), writes=['pre'])
        for ch in range(1, 17):
            P.op('dve', lambda e, ch=ch: e.tensor_tensor(out=pre[:, ch, :], in0=pre[:, ch - 1, :],
                                                         in1=tot[:, ch - 1, :], op=ALU.add),
                 reads=['tot', 'pre'], writes=['pre'])
        P.op('dve', lambda e: e.tensor_tensor(out=cpos.rearrange("p a b -> p (a b)"), in0=ps6[:, 0:96],
                                              in1=pre[:, 0:16, :].rearrange("p a b -> p (a b)"), op=ALU.add),
             reads=['ps6', 'pre'], writes=['cpos'])
        for h in range(6):
            for tb in range(16):
                P.op('dve', lambda e, h=h, tb=tb: e.tensor_scalar(
                    out=Btab[:, h, tb, :], in0=cpos[:, :, h], scalar1=pre[:, tb + 1, h:h + 1], scalar2=None,
                    op0=ALU.subtract), reads=['cpos', 'pre'], writes=['Btab'])

        self.chk('pre')
        def softmax_attn(hh, ychunk, bias_fn, mask_fn):
            pb = 64 * hh
            for T in range(NTB):
                nsc = 4 * T + 4
                ob = 4 + (self.ocnt % 2)
                self.ocnt += 1
                O = self.ps[ob]
                for sc in range(nsc):
                    zb = 2 + (self.cnt % 2)
                    k = self.cnt % 4
                    self.cnt += 1
                    Z = self.ps[zb]
                    P.op('pe', lambda e, Z=Z, sc=sc, T=T: e.matmul(
                        Z, kT[pb:pb + 64, sc * 128:(sc + 1) * 128], qT[pb:pb + 64, T * TB:(T + 1) * TB],
                        start=True, stop=True), reads=[f'kT_{sc // 4}', f'qT_{T}'], writes=[f'ps{zb}'])
                    Ptk = Pt[k]
                    for tl in range(4):
                        tb = 4 * T + tl
                        cs = slice(tl * 128, (tl + 1) * 128)
                        pn = f'Pt{k}_{tl}'
                        if tb < sc:
                            P.op('pool', lambda e, Ptk=Ptk, cs=cs: e.memset(Ptk[:, cs], 0.0), writes=[pn])
                            continue
                        bias = bias_fn(sc, tb)
                        P.op('act', lambda e, Ptk=Ptk, cs=cs, Z=Z, bias=bias: e.activation(
                            out=Ptk[:, cs], in_=Z[:, cs], func=AF.Exp, bias=bias),
                            reads=[f'ps{zb}', 'Btab'], writes=[pn])
                        mask_fn(sc, tb, Ptk[:, cs], pn)
                    vs = slice(0, 65) if hh == 0 else slice(64, 192)
                    M = 65 if hh == 0 else 128
                    P.op('pe', lambda e, O=O, sc=sc, Ptk=Ptk, vs=vs, M=M, nsc=nsc: e.matmul(
                        O[0:M, :], Vp[:, sc, vs], Ptk, start=(sc == 0), stop=(sc == nsc - 1)),
                        reads=[f'Vp_{sc // 4}a', f'Vp_{sc // 4}b', 'Vp_c'] + [f'Pt{k}_{tl}' for tl in range(4)], writes=[f'ps{ob}'])
                    if sc == 0:
                        self.chk('fox_sc0')
                self.chk('fox_T0n')
                self.attn_finish(O, f'ps{ob}', hh, ychunk, T, True, rden, bcs)
                self.chk('fox_T0')

        def fox_mask(sc, tb, ap, pn):
            if sc == tb:
                P.op('pool', lambda e: e.tensor_tensor(out=ap, in0=ap, in1=self.tri_incl, op=ALU.mult),
                     reads=[pn], writes=[pn])

        if True:
            for hp in range(3):
                proj_qkv(O_QA + hp * 128, O_KA + hp * 128, O_VA + hp * 128, 128, 3 * (hp % 2))
                for hh in range(2):
                    h = 2 * hp + hh
                    softmax_attn(hh, hp, lambda sc, tb, h=h: Btab[:, h, tb, sc:sc + 1], fox_mask)
        P.barrier()

        o[0] = base_common
        e32 = A.carve(take(2048), 128, [512], F32)
        spb = [A.carve(take(1024), 128, [512], BF16) for _ in range(2)]
        Ab = [A.carve(take(1024), 128, [512], BF16) for _ in range(2)]
        R = A.carve(take(2048), 128, [512], F32)
        Rb = [A.carve(take(1024), 128, [512], BF16) for _ in range(2)]

        def sb_attn(hh, ychunk):
            pb = 64 * hh
            for T in range(NTB):
                nsc = 4 * T + 4
                ob = 6 + (T % 2)
                O = self.ps[ob]
                rcount = 0
                for sc in reversed(range(nsc)):
                    first = (sc == nsc - 1)
                    c2 = self.cnt % 2
                    self.cnt += 1
                    zb, lb = 2 + c2, 4 + c2
                    Z, L = self.ps[zb], self.ps[lb]
                    kk = kT[pb:pb + 64, sc * 128:(sc + 1) * 128]
                    qq = qT[pb:pb + 64, T * TB:(T + 1) * TB]
                    P.op('pe', lambda e, Z=Z, kk=kk, qq=qq: e.matmul(Z, kk, qq, start=True, stop=True),
                         reads=[f'kT_{sc // 4}', f'qT_{T}'], writes=[f'ps{zb}'])
                    P.op('act', lambda e, Z=Z: e.activation(out=e32, in_=Z, func=AF.Exp),
                         reads=[f'ps{zb}'], writes=['e32'])
                    sp = spb[c2]
                    P.op('act', lambda e, sp=sp: e.activation(out=sp, in_=e32, func=AF.Ln, bias=self.cst[:, 1:2]),
                         reads=['e32'], writes=[f'spb{c2}'])
                    if sc >= 4 * T:
                        m = self.smask[sc - 4 * T]
                        P.op('pool', lambda e, sp=sp, m=m: e.tensor_tensor(out=sp, in0=sp, in1=m, op=ALU.mult),
                             reads=[f'spb{c2}'], writes=[f'spb{c2}'])
                    P.op('pe', lambda e, L=L, kk=kk, qq=qq: e.matmul(L, kk, qq, start=True, stop=False),
                         reads=[f'kT_{sc // 4}', f'qT_{T}'], writes=[f'ps{lb}'])
                    P.op('pe', lambda e, L=L, sp=sp, first=first: e.matmul(L, self.negtri, sp, start=False, stop=first),
                         reads=[f'spb{c2}'], writes=[f'ps{lb}'])
                    if not first:
                        rb = Rb[(rcount - 1) % 2]
                        P.op('pe', lambda e, L=L, rb=rb: e.matmul(L, self.negones, rb, start=False, stop=True),
                             reads=[f'Rb{(rcount - 1) % 2}'], writes=[f'ps{lb}'])
                    ab = Ab[c2]
                    P.op('act', lambda e, ab=ab, L=L: e.activation(out=ab, in_=L, func=AF.Exp),
                         reads=[f'ps{lb}'], writes=[f'Ab{c2}'])
                    if sc >= 4 * T:
                        m = self.smask[sc - 4 * T]
                        P.op('pool', lambda e, ab=ab, m=m: e.tensor_tensor(out=ab, in0=ab, in1=m, op=ALU.mult),
                             reads=[f'Ab{c2}'], writes=[f'Ab{c2}'])
                    vs = slice(0, 64) if hh == 0 else slice(64, 192)
                    M = 64 if hh == 0 else 128
                    P.op('pe', lambda e, O=O, sc=sc, ab=ab, vs=vs, M=M, first=first: e.matmul(
                        O[0:M, :], Vp[:, sc, vs], ab, start=first, stop=(sc == 0)),
                        reads=[f'Vp_{sc // 4}a', f'Vp_{sc // 4}b', 'Vp_c', f'Ab{c2}'], writes=[f'ps{ob}'])
                    if sc > 0:
                        if first:
                            P.op('pool', lambda e, sp=sp: e.tensor_copy(out=R, in_=sp), reads=[f'spb{c2}'], writes=['R'])
                        else:
                            P.op('pool', lambda e, sp=sp: e.tensor_tensor(out=R, in0=R, in1=sp, op=ALU.add),
                                 reads=[f'spb{c2}', 'R'], writes=['R'])
                        rb = Rb[rcount % 2]
                        P.op('pool', lambda e, rb=rb: e.tensor_copy(out=rb, in_=R), reads=['R'],
                             writes=[f'Rb{rcount % 2}'])
                        rcount += 1
                self.attn_finish(O, f'ps{ob}', hh, ychunk, T, False, rden, bcs)

        self.chk('fox')
        if True:
            for hp in range(3):
                nc_ = 128 if hp < 2 else 64
                proj_qkv(O_QB + hp * 128, O_KB + hp * 128, O_VB + hp * 128, nc_, 3 * (hp % 2))
                for hh in range(2 if hp < 2 else 1):
                    sb_attn(hh, 3 + hp)
        P.barrier()
        self.chk('sb')
        self.dsa(l, w_in)
        P.barrier()
        self.chk('dsa')
        self.merge(l, w_in)
        P.barrier()
        self.chk('merge')

    def dsa(self, l, w_in):
        P = self.P
        A = self.A
        o = [OFF_A]

        def take(n):
            r = o[0]
            o[0] += n
            assert o[0] <= SB_BYTES, o[0]
            return r
        qc = [A.carve(take(4096), 128, [S], BF16) for _ in range(3)]
        kcT = A.carve(take(4096), 128, [S], BF16)
        Vc = A.carve(take(16 * 192 * 2), 128, [16, 192], BF16)
        iq = [A.carve(take(4096), 128, [S], BF16) for _ in range(2)]
        ikT = A.carve(take(4096), 128, [S], BF16)
        wI = A.carve(take(256), 128, [16, 4], F32)
        base_d2 = o[0]
        WSL = [A.carve(take(2048), 128, [8, 128], BF16) for _ in range(4)]
        ROPE = A.carve(take(16384), 128, [2, S], F32)
        ckvn = A.carve(take(4096), 128, [S], BF16)
        t32 = [A.carve(take(2048), 128, [512], F32) for _ in range(3)]
        sqb = A.carve(take(1024), 128, [512], BF16)
        COS, SIN = ROPE[:, 0, :], ROPE[:, 1, :]
        P.op('sp', lambda e: e.dma_start(out=ROPE.rearrange("p a b -> p (a b)"), in_=self.rope),
             writes=['rope'], dma=True)
        P.op('pool', lambda e: e.memset(Vc[:, :, 64:65], 1.0), writes=['Vc_c'])
        P.op('pool', lambda e: e.memset(Vc[:, :, 65:128], 0.0), writes=['Vc_c'])
        gkv = self.small_sb[:, 192 + l:193 + l]
        gidx = self.small_sb[:, 194 + l:195 + l]
        gidx_sw = self.small_sb[:, 196 + l:197 + l]

        def rstd_of(src32, T):
            P.op('pool', lambda e: e.tensor_tensor(out=sqb, in0=src32, in1=src32, op=ALU.mult),
                 reads=['t32_0'], writes=['sqb'])
            P.op('pe', lambda e: e.matmul(self.ps[6], self.ones_128th, sqb, start=True, stop=True),
                 reads=['sqb'], writes=['ps6'])
            P.op('act', lambda e: e.activation(out=t32[1], in_=self.ps[6], func=AF.Ln, bias=self.cst[:, 0:1]),
                 reads=['ps6'], writes=['t32_1'])
            P.op('act', lambda e: e.activation(out=t32[1], in_=t32[1], func=AF.Exp, scale=-0.5),
                 reads=['t32_1'], writes=['t32_1'])

        self.load_w(WSL[0], w_in[:, :, O_CKV:O_CKV + 128], 128, 'dw0')

        def ev_ckv(T, ps, pn):
            ts = slice(T * TB, (T + 1) * TB)
            P.op('act', lambda e: e.copy(out=t32[0], in_=ps), reads=[pn], writes=['t32_0'])
            rstd_of(t32[0], T)
            P.op('dve', lambda e: e.scalar_tensor_tensor(out=ckvn[:, ts], in0=t32[0], scalar=gkv, in1=t32[1],
                                                         op0=ALU.mult, op1=ALU.mult),
                 reads=['t32_0', 't32_1'], writes=[f'ckvn_{T}'])
        self.proj_T(WSL[0], 'dw0', 128, ev_ckv)

        wkv = WSL[1]
        src_kv = self.w_kv_up[l]
        P.op('pool', lambda e: e.dma_start(out=wkv[:, 0, :], in_=src_kv), writes=['dw1a'], dma=True)
        P.op('pool', lambda e: e.dma_start(out=wkv[:, 2, 0:64], in_=src_kv[:, 0:64]), writes=['dw1b'], dma=True)
        P.op('pool', lambda e: e.dma_start(out=wkv[:, 2, 64:128], in_=src_kv[:, 0:64]), writes=['dw1c'], dma=True)

        def make_swapped(dst, src, names):
            d4 = dst.rearrange("p k (h d) -> p k h d", h=2)
            s4 = src.rearrange("p k (h d) -> p k h d", h=2)
            P.op('pool', lambda e: e.tensor_copy(out=dst, in_=src), reads=names, writes=['swp'])
            P.op('pool', lambda e: e.tensor_copy(out=d4[:, :, :, 0:8], in_=s4[:, :, :, 8:16]), reads=names, writes=['swp'])
            P.op('pool', lambda e: e.tensor_copy(out=d4[:, :, :, 8:16], in_=s4[:, :, :, 0:8]), reads=names, writes=['swp'])
        make_swapped(wkv[:, 3:4, :], wkv[:, 2:3, :], ['dw1b', 'dw1c'])

        def rope_combine(T, psn, pss, names, dst, dname, pre_n=None, pre_s=None):
            ts = slice(T * TB, (T + 1) * TB)
            P.op('dve', lambda e: e.tensor_tensor(out=t32[0], in0=psn, in1=COS[:, ts], op=ALU.mult),
                 reads=[names[0], 'rope'], writes=['t32_0'])
            P.op('dve', lambda e: e.tensor_tensor(out=t32[2], in0=pss, in1=SIN[:, ts], op=ALU.mult),
                 reads=[names[1], 'rope'], writes=['t32_2'])
            P.op('pool', lambda e: e.tensor_tensor(out=dst[:, ts], in0=t32[0], in1=t32[2], op=ALU.add),
                 reads=['t32_0', 't32_2'], writes=[dname])

        for T in range(NTB):
            ts = slice(T * TB, (T + 1) * TB)
            P.op('pe', lambda e, ts=ts: e.matmul(self.ps[0], wkv[:, 2, :], ckvn[:, ts], start=True, stop=True),
                 reads=['dw1b', 'dw1c', f'ckvn_{T}'], writes=['ps0'])
            P.op('pe', lambda e, ts=ts: e.matmul(self.ps[1], wkv[:, 3, :], ckvn[:, ts], start=True, stop=True),
                 reads=['swp', f'ckvn_{T}'], writes=['ps1'])
            rope_combine(T, self.ps[0], self.ps[1], ['ps0', 'ps1'], kcT, 'kcT')
        for g in range(4):
            b = 2 + g % 2
            ps = self.ps[b]
            for cc in range(4):
                ch = 4 * g + cc
                P.op('pe', lambda e, ps=ps, cc=cc, ch=ch: e.matmul(ps[:, cc * 128:cc * 128 + 64],
                                                                  ckvn[:, ch * 128:(ch + 1) * 128], wkv[:, 0, 64:128],
                                                                  start=True, stop=True),
                     reads=['dw1a', f'ckvn_{g}'], writes=[f'ps{b}'])
            ps3 = ps.rearrange("p (a b) -> p a b", a=4)
            P.op('dve', lambda e, ps3=ps3, g=g: e.tensor_copy(out=Vc[:, 4 * g:4 * g + 4, 0:64], in_=ps3[:, :, 0:64]),
                 reads=[f'ps{b}'], writes=['Vc_a'])
            P.op('dve', lambda e, ps3=ps3, g=g: e.tensor_copy(out=Vc[:, 4 * g:4 * g + 4, 128:192], in_=ps3[:, :, 0:64]),
                 reads=[f'ps{b}'], writes=['Vc_b'])

        def rope_pair(col_lo, col_hi, dst, dname):
            wn, ws = WSL[2], WSL[3]
            if col_hi == col_lo + 64:
                self.load_w(wn, w_in[:, :, col_lo:col_lo + 128], 128, 'dw2a')
                nm = ['dw2a']
            else:
                self.load_w(wn, w_in[:, :, col_lo:col_lo + 64], 64, 'dw2a')
                self.load_w(wn, w_in[:, :, col_hi:col_hi + 64], 64, 'dw2b', dcol=64)
                nm = ['dw2a', 'dw2b']
            make_swapped(ws, wn, nm)
            for T in range(NTB):
                ts = slice(T * TB, (T + 1) * TB)
                for kc in range(8):
                    P.op('pe', lambda e, kc=kc, ts=ts: e.matmul(self.ps[0], wn[:, kc, :], self.H[:, kc, ts],
                                                                start=(kc == 0), stop=(kc == 7)),
                         reads=nm + [f'h_{T}'], writes=['ps0'])
                for kc in range(8):
                    P.op('pe', lambda e, kc=kc, ts=ts: e.matmul(self.ps[1], ws[:, kc, :], self.H[:, kc, ts],
                                                                start=(kc == 0), stop=(kc == 7)),
                         reads=['swp', f'h_{T}'], writes=['ps1'])
                rope_combine(T, self.ps[0], self.ps[1], ['ps0', 'ps1'], dst, dname)

        rope_pair(O_QC, O_QC, qc[0], 'qc0')
        rope_pair(O_QC + 64, O_QC + 128, qc[1], 'qc1')
        rope_pair(O_QC + 192, O_QC + 256, qc[2], 'qc2')
        rope_pair(O_QI, O_QI + 64, iq[0], 'iq0')
        rope_pair(O_QI + 128, O_QI + 192, iq[1], 'iq1')

        wn, ws = WSL[2], WSL[3]
        self.load_w(wn, w_in[:, :, O_KI:O_KI + 64], 64, 'dw2a')
        self.load_w(wn, w_in[:, :, O_KI:O_KI + 64], 64, 'dw2b', dcol=64)
        make_swapped(ws, wn, ['dw2a', 'dw2b'])
        for T in range(NTB):
            ts = slice(T * TB, (T + 1) * TB)
            for kc in range(8):
                P.op('pe', lambda e, kc=kc, ts=ts: e.matmul(self.ps[0], wn[:, kc, :], self.H[:, kc, ts],
                                                            start=(kc == 0), stop=(kc == 7)),
                     reads=['dw2a', 'dw2b', f'h_{T}'], writes=['ps0'])
            for kc in range(8):
                P.op('pe', lambda e, kc=kc, ts=ts: e.matmul(self.ps[1], ws[:, kc, :], self.H[:, kc, ts],
                                                            start=(kc == 0), stop=(kc == 7)),
                     reads=['swp', f'h_{T}'], writes=['ps1'])
            P.op('act', lambda e: e.copy(out=t32[0], in_=self.ps[0]), reads=['ps0'], writes=['t32_0'])
            rstd_of(t32[0], T)
            P.op('dve', lambda e: e.scalar_tensor_tensor(out=t32[0], in0=t32[0], scalar=gidx, in1=t32[1],
                                                         op0=ALU.mult, op1=ALU.mult),
                 reads=['t32_0', 't32_1'], writes=['t32_0'])
            P.op('dve', lambda e: e.scalar_tensor_tensor(out=t32[2], in0=self.ps[1], scalar=gidx_sw, in1=t32[1],
                                                         op0=ALU.mult, op1=ALU.mult),
                 reads=['ps1', 't32_1'], writes=['t32_2'])
            P.op('dve', lambda e, ts=ts: e.tensor_tensor(out=t32[0], in0=t32[0], in1=COS[:, ts], op=ALU.mult),
                 reads=['t32_0', 'rope'], writes=['t32_0'])
            P.op('dve', lambda e, ts=ts: e.tensor_tensor(out=t32[2], in0=t32[2], in1=SIN[:, ts], op=ALU.mult),
                 reads=['t32_2', 'rope'], writes=['t32_2'])
            P.op('pool', lambda e, ts=ts: e.tensor_tensor(out=ikT[:, ts], in0=t32[0], in1=t32[2], op=ALU.add),
                 reads=['t32_0', 't32_2'], writes=['ikT'])

        self.load_w(WSL[0], w_in[:, :, O_WI:O_WI + 4], 4, 'dw0')
        self.proj_tok(WSL[0], 'dw0', 4, lambda g, ps3, pn: P.op(
            'dve', lambda e: e.tensor_copy(out=wI[:, 4 * g:4 * g + 4, :], in_=ps3[:, :, 0:4]),
            reads=[pn], writes=['wI']), banks=(2, 3))
        P.barrier()

        o[0] = base_d2
        sc32 = A.carve(take(8192), 128, [S], F32)
        mask = A.carve(take(4096), 128, [S], BF16)
        maskT = A.carve(take(4096), 128, [16, 128], BF16)
        Pt = [A.carve(take(256), 128, [128], BF16) for _ in range(4)]
        tt = [A.carve(take(2048), 128, [512], F32) for _ in range(2)]
        rden = A.carve(take(2048), 128, [512], F32)
        bcs = A.carve(take(2048), 128, [512], F32)
        sm = A.carve(take(256), 128, [64], F32)
        mx, mn, d0, mid, cntc, tmp1, theta = [sm[:, i:i + 1] for i in range(7)]
        halfs = sm[:, 16:32]
        ps7b = self.ps[7].bitcast(BF16)
        cnt = 0
        ocnt = 0
        for tb in range(NQB):
            L = (tb + 1) * 128
            qs = slice(tb * 128, (tb + 1) * 128)
            for j in range((L + 511) // 512):
                w = min(512, L - 512 * j)
                cs = slice(512 * j, 512 * j + w)
                for h in range(NIH):
                    pb = 64 * (h % 2)
                    zb = 2 + cnt % 2
                    k2 = cnt % 2
                    cnt += 1
                    Z = self.ps[zb]
                    P.op('pe', lambda e, Z=Z, h=h, pb=pb, cs=cs, w=w, qs=qs: e.matmul(
                        Z[:, 0:w], iq[h // 2][pb:pb + 64, qs], ikT[pb:pb + 64, cs], start=True, stop=True),
                        reads=[f'iq{h // 2}', 'ikT'], writes=[f'ps{zb}'])
                    if h == 0:
                        P.op('dve', lambda e, Z=Z, cs=cs, w=w, tb=tb: e.tensor_scalar(
                            out=sc32[:, cs], in0=Z[:, 0:w], scalar1=0.0, scalar2=wI[:, tb, 0:1],
                            op0=ALU.max, op1=ALU.mult), reads=[f'ps{zb}', 'wI'], writes=[f'sc_{j}'])
                    else:
                        P.op('dve', lambda e, Z=Z, w=w, tb=tb, h=h, k2=k2: e.tensor_scalar(
                            out=tt[k2][:, 0:w], in0=Z[:, 0:w], scalar1=0.0, scalar2=wI[:, tb, h:h + 1],
                            op0=ALU.max, op1=ALU.mult), reads=[f'ps{zb}', 'wI'], writes=[f'tt{k2}'])
                        P.op('pool', lambda e, cs=cs, w=w, k2=k2: e.tensor_tensor(
                            out=sc32[:, cs], in0=sc32[:, cs], in1=tt[k2][:, 0:w], op=ALU.add),
                            reads=[f'tt{k2}', f'sc_{j}'], writes=[f'sc_{j}'])
            scn = [f'sc_{j}' for j in range((L + 511) // 512)]
            if tb >= 2:
                P.op('dve', lambda e, L=L: e.tensor_reduce(out=mx, in_=sc32[:, 0:L], axis=mybir.AxisListType.X,
                                                           op=ALU.max), reads=scn, writes=['mx'])
                P.op('dve', lambda e, L=L: e.tensor_reduce(out=mn, in_=sc32[:, 0:L], axis=mybir.AxisListType.X,
                                                           op=ALU.min), reads=scn, writes=['mn'])
            P.op('pool', lambda e, L=L: e.memset(sc32[0:64, L - 64:L], -1e30), reads=scn + ['mx', 'mn'],
                 writes=[scn[-1]])
            if tb >= 2:
                P.op('dve', lambda e: e.tensor_tensor(out=d0, in0=mx, in1=mn, op=ALU.subtract),
                     reads=['mx', 'mn'], writes=['d0'])
                P.op('dve', lambda e: e.tensor_scalar(out=halfs, in0=self.pow2, scalar1=d0, scalar2=None,
                                                      op0=ALU.mult), reads=['d0'], writes=['halfs'])
                P.op('dve', lambda e: e.tensor_tensor(out=mid, in0=mn, in1=halfs[:, 0:1], op=ALU.add),
                     reads=['mn', 'halfs'], writes=['mid'])
                for k in range(16):
                    P.op('dve', lambda e, L=L: e.tensor_scalar(out=mask[:, 0:L], in0=sc32[:, 0:L], scalar1=mid,
                                                               scalar2=None, op0=ALU.is_ge, op1=ALU.add,
                                                               accum_out=cntc),
                         reads=scn + ['mid'], writes=['mask', 'cntc'])
                    P.op('dve', lambda e: e.tensor_scalar(out=tmp1, in0=cntc, scalar1=256.0, scalar2=0.5,
                                                          op0=ALU.is_ge, op1=ALU.subtract),
                         reads=['cntc'], writes=['tmp1'])
                    P.op('dve', lambda e, k=k: e.scalar_tensor_tensor(out=mid, in0=tmp1, scalar=halfs[:, k:k + 1],
                                                                      in1=mid, op0=ALU.mult, op1=ALU.add),
                         reads=['tmp1', 'halfs', 'mid'], writes=['mid'])
                P.op('dve', lambda e: e.scalar_tensor_tensor(out=theta, in0=halfs[:, 15:16], scalar=-0.5, in1=mid,
                                                             op0=ALU.mult, op1=ALU.add),
                     reads=['mid', 'halfs'], writes=['theta'])
            else:
                P.op('dve', lambda e: e.memset(theta, -1e29), writes=['theta'])
            P.op('dve', lambda e, L=L: e.tensor_scalar(out=mask[:, 0:L], in0=sc32[:, 0:L], scalar1=theta,
                                                       scalar2=None, op0=ALU.is_ge),
                 reads=scn + ['theta'], writes=['mask'])
            for g0 in range(0, tb + 1, 8):
                n = min(8, tb + 1 - g0)
                for i in range(n):
                    sc = g0 + i
                    P.op('pe', lambda e, i=i, sc=sc: e.transpose(ps7b[:, i * 128:(i + 1) * 128],
                                                                 mask[:, sc * 128:(sc + 1) * 128], self.ident),
                         reads=['mask'], writes=['ps7'])
                P.op('act', lambda e, g0=g0, n=n: e.copy(out=maskT[:, g0:g0 + n, :].rearrange("p a b -> p (a b)"),
                                                         in_=ps7b[:, 0:n * 128]),
                     reads=['ps7'], writes=['maskT'])
            for hc in range(NH_C):
                i = (hc + 1) // 2
                hh = (hc + 1) % 2
                pb = 64 * hh
                ob = 4 + ocnt % 2
                ocnt += 1
                O = self.ps[ob]
                for sc in range(tb + 1):
                    zb = 2 + cnt % 2
                    k = cnt % 4
                    cnt += 1
                    Z = self.ps[zb]
                    P.op('pe', lambda e, Z=Z, sc=sc, i=i, pb=pb, qs=qs: e.matmul(
                        Z[:, 0:128], kcT[pb:pb + 64, sc * 128:(sc + 1) * 128], qc[i][pb:pb + 64, qs],
                        start=True, stop=True), reads=['kcT', f'qc{i}'], writes=[f'ps{zb}'])
                    Ptk = Pt[k]
                    P.op('act', lambda e, Ptk=Ptk, Z=Z: e.activation(out=Ptk, in_=Z[:, 0:128], func=AF.Exp, scale=0.125),
                         reads=[f'ps{zb}'], writes=[f'dPt{k}'])
                    P.op('pool', lambda e, Ptk=Ptk, sc=sc: e.tensor_tensor(out=Ptk, in0=Ptk, in1=maskT[:, sc, :],
                                                                           op=ALU.mult),
                         reads=[f'dPt{k}', 'maskT'], writes=[f'dPt{k}'])
                    vs = slice(0, 65) if hh == 0 else slice(64, 192)
                    M = 65 if hh == 0 else 128
                    P.op('pe', lambda e, O=O, sc=sc, Ptk=Ptk, vs=vs, M=M, tb=tb: e.matmul(
                        O[0:M, 0:128], Vc[:, sc, vs], Ptk, start=(sc == 0), stop=(sc == tb)),
                        reads=['Vc_a', 'Vc_b', 'Vc_c', f'dPt{k}'], writes=[f'ps{ob}'])
                self.attn_finish(O, f'ps{ob}', hh, 5 + i, tb, True, rden, bcs, width=128)

    def merge(self, l, w_in):
        P = self.P
        A = self.A
        o = [OFF_A]

        def take(n):
            r = o[0]
            o[0] += n
            assert o[0] <= SB_BYTES, o[0]
            return r
        merged = A.carve(take(8 * S * 2), 128, [8, S], BF16)
        wg = [A.carve(take(6144), 128, [8, 384], BF16) for _ in range(2)]
        wu = [A.carve(take(9 * 256), 128, [9, 128], BF16) for _ in range(2)]
        wo = [A.carve(take(2048), 128, [8, 128], BF16) for _ in range(2)]
        sg = [A.carve(take(2048), 128, [512], F32) for _ in range(3)]
        mm = [A.carve(take(2048), 128, [512], F32) for _ in range(3)]
        ufox = self.w_up_fox[l].rearrange("(j p) n -> p j n", p=128)
        usb = self.w_up_sb[l]
        udsa = self.w_up_dsa[l]
        wout = self.w_out[l].rearrange("(kc p) n -> p kc n", p=128)
        for c in range(8):
            k = c % 2
            cs = slice(c * 128, (c + 1) * 128)
            for b in range(3):
                src = w_in[:, :, O_G + b * D + c * 128:O_G + b * D + (c + 1) * 128]
                dst = wg[k][:, :, b * 128:(b + 1) * 128]
                P.op('pool', lambda e, src=src, dst=dst: e.dma_start(out=dst, in_=src), writes=[f'wg{k}_{b}'], dma=True)
            W = wu[k]
            dl = [
                (W[:, 0:3, :], ufox[:, :, cs]),
                (W[:, 3:5, :], usb[0:256, :].rearrange("(j p) n -> p j n", p=128)[:, :, cs]),
                (W[0:64, 5, :], usb[256:320, cs]),
                (W[64:128, 6, :], udsa[0:64, cs]),
                (W[:, 7:9, :], udsa[64:320, :].rearrange("(j p) n -> p j n", p=128)[:, :, cs]),
            ]
            for i, (dst, src) in enumerate(dl):
                P.op('pool', lambda e, src=src, dst=dst: e.dma_start(out=dst, in_=src), writes=[f'wu{k}_{i}'], dma=True)
            wun = [f'wu{k}_{i}' for i in range(5)]
            for T in range(NTB):
                ts = slice(T * TB, (T + 1) * TB)
                yn = [nm for nm in self.P.bufs if nm.startswith('y') and nm.endswith(f'_{T}')]
                for b in range(3):
                    G = self.ps[b]
                    for kc in range(8):
                        P.op('pe', lambda e, G=G, kc=kc, b=b, ts=ts, k=k: e.matmul(
                            G, wg[k][:, kc, b * 128:(b + 1) * 128], self.H[:, kc, ts], start=(kc == 0), stop=(kc == 7)),
                            reads=[f'wg{k}_{b}', f'h_{T}'], writes=[f'ps{b}'])
                    U = self.ps[3 + b]
                    if b == 0:
                        terms = [(W[:, j, :], self.Y[:, j, ts]) for j in range(3)]
                    elif b == 1:
                        terms = [(W[:, 3, :], self.Y[:, 3, ts]), (W[:, 4, :], self.Y[:, 4, ts]),
                                 (W[0:64, 5, :], self.Y[0:64, 5, ts])]
                    else:
                        terms = [(W[64:128, 6, :], self.Y[64:128, 5, ts]), (W[:, 7, :], self.Y[:, 6, ts]),
                                 (W[:, 8, :], self.Y[:, 7, ts])]
                    for ti, (lh, rh) in enumerate(terms):
                        P.op('pe', lambda e, U=U, lh=lh, rh=rh, ti=ti: e.matmul(U, lh, rh, start=(ti == 0), stop=(ti == 2)),
                             reads=wun + ['yall'], writes=[f'ps{3 + b}'])
                    P.op('act', lambda e, G=G, b=b: e.activation(out=sg[b], in_=G, func=AF.Exp, scale=-1.0),
                         reads=[f'ps{b}'], writes=[f'sg{b}'])
                    P.op('dve', lambda e, b=b: e.tensor_scalar(out=sg[b], in0=sg[b], scalar1=1.0, scalar2=None, op0=ALU.add),
                         reads=[f'sg{b}'], writes=[f'sg{b}'])
                    P.op('dve', lambda e, b=b: e.reciprocal(out=sg[b], in_=sg[b]),
                         reads=[f'sg{b}'], writes=[f'sg{b}'])
                    if self.stage.rstrip('XYH') == 'mgd':
                        P.op('dve', lambda e, b=b, ts=ts: e.tensor_copy(out=self.X[:, b, ts], in_=sg[b]),
                             reads=[f'sg{b}'], writes=[f'x{b}_{T}'])
                        P.op('dve', lambda e, b=b, ts=ts, U=U: e.tensor_copy(out=self.X[:, 3 + b, ts], in_=U),
                             reads=[f'ps{3 + b}'], writes=[f'x{3 + b}_{T}'])
                    P.op('dve', lambda e, U=U, b=b: e.tensor_tensor(out=mm[b], in0=sg[b], in1=U, op=ALU.mult),
                         reads=[f'sg{b}', f'ps{3 + b}'], writes=[f'mm{b}'])
                P.op('pool', lambda e: e.tensor_tensor(out=mm[0], in0=mm[0], in1=mm[1], op=ALU.add),
                     reads=['mm0', 'mm1'], writes=['mm0'])
                P.op('pool', lambda e, c=c, ts=ts: e.tensor_tensor(out=merged[:, c, ts], in0=mm[0], in1=mm[2], op=ALU.add),
                     reads=['mm0', 'mm2'], writes=[f'mg_{T}'])
                if self.stage.rstrip('XYH') == 'mgd':
                    P.op('dve', lambda e, ts=ts: e.tensor_copy(out=self.X[:, 6, ts], in_=mm[0]), reads=['mm0'], writes=[f'x6_{T}'])
                    P.op('dve', lambda e, ts=ts, c=c: e.tensor_copy(out=self.X[:, 7, ts], in_=merged[:, c, ts]), reads=[f'mg_{T}'], writes=[f'x7_{T}'])
            self.chk('mgd')
        if self.stage.rstrip('XYH') == 'mg':
            for c in range(8):
                P.op('dve', lambda e, c=c: e.tensor_copy(out=self.X[:, c, :], in_=merged[:, c, :]),
                     reads=[f'mg_{T}' for T in range(NTB)], writes=[f'x{c}_{T}' for T in range(NTB)])
            self.chk('mg')
        for c2 in range(8):
            k = c2 % 2
            src = wout[:, :, c2 * 128:(c2 + 1) * 128]
            dst = wo[k]
            P.op('pool', lambda e, src=src, dst=dst: e.dma_start(out=dst, in_=src), writes=[f'wo{k}'], dma=True)
            for T in range(NTB):
                ts = slice(T * TB, (T + 1) * TB)
                pb_ = 6 + (c2 * NTB + T) % 2
                po = self.ps[pb_]
                for kc in range(8):
                    P.op('pe', lambda e, po=po, kc=kc, dst=dst, ts=ts: e.matmul(
                        po, dst[:, kc, :], merged[:, kc, ts], start=(kc == 0), stop=(kc == 7)),
                        reads=[f'wo{k}', f'mg_{T}'], writes=[f'ps{pb_}'])
                xd = self.X[:, c2, ts]
                P.op('dve', lambda e, xd=xd, po=po: e.tensor_tensor(out=xd, in0=xd, in1=po, op=ALU.add),
                     reads=[f'ps{pb_}', f'x{c2}_{T}'], writes=[f'x{c2}_{T}'])

    def final(self, s):
        P = self.P
        A = self.A
        gi = 3 * DEPTH
        o = OFF_A
        stg = [A.carve(o + k * 16384, 128, [8, TB], F32) for k in range(2)]; o += 32768
        tmp_off = o
        for t in range(NTB):
            k = t % 2
            if self.stage[-1] in 'XYH':
                SRC = {'X': self.X, 'Y': self.Y, 'H': self.H}[self.stage[-1]]
                for c in range(8):
                    P.op('dve', lambda e, c=c, t=t, k=k: e.tensor_copy(out=stg[k][:, c, :], in_=SRC[:, c, t * TB:(t + 1) * TB]),
                         reads=[f'x{c}_{t}'], writes=[f'stg{k}_{c}'])
            else:
                self.rmsnorm(gi, lambda c, t, k=k: (stg[k][:, c, :], f'stg{k}_{c}'), tmp_off, t_list=[t])
            for c in range(8):
                dst = self.outT[s, c * 128:(c + 1) * 128, t * TB:(t + 1) * TB]
                src = stg[k][:, c, :]
                P.op('sp', lambda e, src=src, dst=dst: e.dma_start(out=dst, in_=src),
                     reads=[f'stg{k}_{c}'], dma=True)


def pack_gains(inp):
    g = np.zeros((128, NGAIN), np.float32)
    for l in range(DEPTH):
        for k, name in enumerate(("g_ffn1", "g_mix", "g_ffn2")):
            g[:, (l * 3 + k) * 8:(l * 3 + k + 1) * 8] = np.asarray(inp[name][l], np.float32).reshape(8, 128).T
    g[:, 3 * DEPTH * 8:] = np.asarray(inp["g_final"], np.float32).reshape(8, 128).T
    return g


def rope_consts():
    pos = np.arange(S, dtype=np.float32)
    inv = (np.float32(500000.0) ** (-np.arange(0, 16, 2, dtype=np.float32) / np.float32(16))).astype(np.float32)
    ang = pos[None, :] * inv[:, None]
    cos, sin = np.cos(ang).astype(np.float32), np.sin(ang).astype(np.float32)
    r = np.zeros((128, 2, S), np.float32)
    r[:, 0, :] = 1.0
    for p in range(128):
        d = p % 64
        if d < 16:
            r[p, 0] = cos[d % 8]
            r[p, 1] = -sin[d % 8] if d < 8 else sin[d % 8]
    return r.reshape(128, 2 * S)


def const_tables():
    i = np.arange(128)
    cbf = np.zeros((128, NCBF), np.float32)
    cbf[:, 0:128] = (i[:, None] <= i[None, :])
    for k in range(4):
        m = np.zeros((128, 4, 128), np.float32)
        m[:, k, :] = (i[:, None] < i[None, :])
        m[:, k + 1:, :] = 1.0
        cbf[:, 128 + 512 * k:128 + 512 * (k + 1)] = m.reshape(128, 512)
    cbf[:, 2176:2304] = -(i[:, None] >= i[None, :]).astype(np.float32)
    cbf[:, 2304:2432] = -1.0
    cbf[:, 2432:2560] = np.eye(128, dtype=np.float32)
    cbf[:, 2560:2688] = 1.0 / 1024
    cbf[:, 2688:2816] = 1.0 / 128
    cbf[:, 2816:2944] = 1.0 / 64
    cf = np.zeros((128, NCF32), np.float32)
    cf[:, 0:128] = (i[:, None] <= i[None, :])
    cf[:, 128:256] = 1.0
    cf[:, 256:272] = 2.0 ** -(np.arange(16) + 1.0)
    return cbf, cf


def make_in_maps(inp, n_cores, n_seq):
    x = np.asarray(inp["x"], np.float32)
    gains = pack_gains(inp)
    smallp = np.zeros((128, 256), np.float32)
    for l in range(DEPTH):
        smallp[:, l * 96:(l + 1) * 96] = np.tile(np.asarray(inp["b_forget"][l], np.float32), 16)[None, :]
        smallp[:, 192 + l] = np.asarray(inp["g_kv_latent"][l], np.float32)
        smallp[:64, 194 + l] = np.asarray(inp["g_idx_k"][l], np.float32)
        smallp[64:, 194 + l] = np.asarray(inp["g_idx_k"][l], np.float32)
        gsw = np.asarray(inp["g_idx_k"][l], np.float32).copy()
        gsw[0:8], gsw[8:16] = gsw[8:16].copy(), gsw[0:8].copy()
        smallp[:64, 196 + l] = gsw
        smallp[64:, 196 + l] = gsw
    rope = rope_consts()
    cbf, cf32 = const_tables()
    maps = []
    shared = {
        "w_ffn1_gu": np.asarray(inp["w_ffn1_gu"], np.float32), "w_ffn2_gu": np.asarray(inp["w_ffn2_gu"], np.float32),
        "w_ffn1_down": np.asarray(inp["w_ffn1_down"], np.float32),
        "w_ffn2_down": np.asarray(inp["w_ffn2_down"], np.float32),
        "w_in": np.asarray(inp["w_in"], np.float32), "w_kv_up": np.asarray(inp["w_kv_up"], np.float32),
        "w_up_fox": np.asarray(inp["w_up_fox"], np.float32), "w_up_sb": np.asarray(inp["w_up_sb"], np.float32),
        "w_up_dsa": np.asarray(inp["w_up_dsa"], np.float32), "w_out": np.asarray(inp["w_out"], np.float32),
        "gains": gains, "smallp": smallp, "rope": rope, "cbf": cbf, "cf32": cf32,
    }
    for cidx in range(n_cores):
        xs = x[cidx * n_seq:(cidx + 1) * n_seq]
        m = dict(shared)
        m["xT"] = np.ascontiguousarray(xs.transpose(0, 2, 1))
        maps.append(m)
    return maps


def kernel(**inputs):
    n_cores, n_seq = 8, 2
    mdl = Model(n_seq=n_seq)
    nc = mdl.build()
    maps = make_in_maps(inputs, n_cores, n_seq)
    res = run_bass_kernel_spmd(nc, maps, core_ids=list(range(n_cores)))
    outs = [r["outT"].transpose(0, 2, 1) for r in res.results]
    return np.ascontiguousarray(np.concatenate(outs, axis=0)).astype(np.float32)
```

```python
import numpy as np
import concourse.bass as bass
import concourse.mybir as mybir
from concourse.bass_utils import run_bass_kernel_spmd

F32 = mybir.dt.float32
BF16 = mybir.dt.bfloat16
U8 = mybir.dt.uint8
AF = mybir.ActivationFunctionType
ALU = mybir.AluOpType

D = 1024
S = 2048
DEPTH = 2
DFF = 2816
NFF = DFF // 128
HD = 64
NH_A, NH_B, NH_C = 6, 5, 5
W_A, W_B, W_C = 384, 320, 320
KVL = 128
NIH = 4
D_IN = 5962
EPS = 1e-6
TB = 512
NTB = S // TB
NQB = S // 128

O_QA = 0
O_KA = O_QA + W_A
O_VA = O_KA + W_A
O_FA = O_VA + W_A
O_QB = O_FA + NH_A
O_KB = O_QB + W_B
O_VB = O_KB + W_B
O_QC = O_VB + W_B
O_CKV = O_QC + W_C
O_QI = O_CKV + KVL
O_KI = O_QI + NIH * 64
O_WI = O_KI + 64
O_G = O_WI + NIH
assert O_G + 3 * D == D_IN

EPOCH = 4096
ENGS = ['pe', 'act', 'dve', 'pool', 'sp']


class Buf:
    __slots__ = ('name', 'lw', 'rd')

    def __init__(self, name):
        self.name = name
        self.lw = None
        self.rd = {}


class Prog:
    NRING = 8

    def __init__(self):
        self.ops = {e: [] for e in ENGS}
        self.waited = {e: {} for e in ENGS}
        self.ndma = {e: 0 for e in ENGS}
        self.bufs = {}

    def buf(self, name):
        b = self.bufs.get(name)
        if b is None:
            b = Buf(name)
            self.bufs[name] = b
        return b

    def _tok(self, names):
        return [self.buf(n) if isinstance(n, str) else n for n in names]

    def op(self, eng, emit, reads=(), writes=(), dma=False):
        reads = self._tok(reads)
        writes = self._tok(writes)
        idx = len(self.ops[eng])
        deps = set()
        for b in reads:
            if b.lw is not None:
                deps.add(b.lw)
        for b in writes:
            if b.lw is not None:
                deps.add(b.lw)
            for t in b.rd.values():
                deps.add(t)
        if dma:
            d = self.ndma[eng]
            self.ndma[eng] += 1
            tok = ('d', eng, d)
            if d >= self.NRING:
                deps.add(('d', eng, d - self.NRING))
        else:
            tok = ('c', eng, idx)
        waits = []
        w = self.waited[eng]
        for t in deps:
            if t[0] == 'c':
                _, e, i = t
                if e == eng and eng == 'pe':
                    continue
                if w.get(('c', e), -1) >= i:
                    continue
                w[('c', e)] = i
                self.ops[e][i][2] = True
                waits.append(t)
            else:
                _, q, d0 = t
                key = ('d', q, d0 % self.NRING)
                if w.get(key, -1) >= d0:
                    continue
                w[key] = d0
                waits.append(t)
        self.ops[eng].append([emit, waits, False, tok if dma else None])
        rkey = (tok[0], tok[1]) if tok[0] == 'c' else (tok[0], tok[1], tok[2] % self.NRING)
        for b in reads:
            b.rd[rkey] = tok
        for b in writes:
            b.lw = tok
            b.rd = {}
        return tok

    def barrier(self):
        last = {}
        for e in ENGS:
            for i in range(len(self.ops[e]) - 1, -1, -1):
                o = self.ops[e][i]
                if o[0] is not None and o[3] is None:
                    last[e] = i
                    break
        for eng in ENGS:
            waits = []
            w = self.waited[eng]
            for e, i in last.items():
                if e == eng:
                    continue
                if w.get(('c', e), -1) >= i:
                    continue
                w[('c', e)] = i
                self.ops[e][i][2] = True
                waits.append(('c', e, i))
            for q in ENGS:
                n = self.ndma[q]
                for d0 in range(max(0, n - self.NRING), n):
                    key = ('d', q, d0 % self.NRING)
                    if w.get(key, -1) >= d0:
                        continue
                    w[key] = d0
                    waits.append(('d', q, d0))
            self.ops[eng].append([None, waits, False, None])
        for b in self.bufs.values():
            b.lw = None
            b.rd = {}

    def wait_all_dma(self, eng):
        waits = []
        for q in ENGS:
            n = self.ndma[q]
            for d in range(max(0, n - self.NRING), n):
                waits.append(('d', q, d))
        self.ops[eng].append([None, waits, False, None])

    def emit(self, nc, block_cm):
        cnt = {}
        nsem = {}
        for e in ENGS:
            c = 0
            arr = []
            for o in self.ops[e]:
                if o[2]:
                    c += 1
                arr.append(c)
            cnt[e] = arr
            nsem[e] = (c + EPOCH - 1) // EPOCH
        csem = {e: [nc.alloc_semaphore(name=f"c_{e}_{k}") for k in range(nsem[e])] for e in ENGS}
        dsem = {e: [nc.alloc_semaphore(name=f"d_{e}_{k}") for k in range(self.NRING)]
                for e in ENGS if self.ndma[e] > 0}

        def resolve(t):
            if t[0] == 'c':
                _, e, i = t
                c = cnt[e][i]
                return csem[e][(c - 1) // EPOCH], (c - 1) % EPOCH + 1
            _, q, d0 = t
            return dsem[q][d0 % self.NRING], 16 * (d0 // self.NRING + 1)

        prog = self

        def run(e, eng):
            for k, (emit, waits, marked, dtok) in enumerate(prog.ops[e]):
                for t in waits:
                    s, v = resolve(t)
                    eng.wait_ge(s, v)
                if emit is None:
                    continue
                ins = emit(eng)
                if dtok is not None:
                    s, _ = resolve(dtok)
                    ins.then_inc(s, 16)
                elif marked:
                    c = cnt[e][k]
                    ins.then_inc(csem[e][(c - 1) // EPOCH], 1)

        with block_cm as block:
            @block.tensor
            def _(eng):
                run('pe', eng)

            @block.scalar
            def _(eng):
                run('act', eng)

            @block.vector
            def _(eng):
                run('dve', eng)

            @block.gpsimd
            def _(eng):
                run('pool', eng)

            @block.sync
            def _(eng):
                run('sp', eng)


class Arena:
    def __init__(self, ap_u8, nbytes):
        self.ap = ap_u8
        self.nbytes = nbytes

    def carve(self, off, parts, free_shape, dtype, pbase=0):
        esz = 4 if dtype == F32 else 2
        n = 1
        for s in free_shape:
            n *= s
        assert off % 4 == 0 and off + n * esz <= self.nbytes, (off, n * esz, self.nbytes)
        a = self.ap[pbase:pbase + parts, off:off + n * esz].bitcast(dtype)
        if len(free_shape) == 2:
            a = a.rearrange('p (a b) -> p a b', a=free_shape[0])
        elif len(free_shape) == 3:
            a = a.rearrange('p (a b c) -> p a b c', a=free_shape[0], b=free_shape[1])
        return a


OFF_X = 0
OFF_H = OFF_X + 8 * S * 4
OFF_C = OFF_H + 8 * S * 2
OFF_Y = OFF_C + 10240
OFF_A = OFF_Y + 8 * S * 2
SB_BYTES = 212800
ARENA_BYTES = SB_BYTES - OFF_A
NGAIN = 8 * (3 * DEPTH + 1)
NCBF = 2944
NCF32 = 272


class _Stop(Exception):
    pass


class Model:
    def __init__(self, n_seq=2, depth=DEPTH, stage='full'):
        self.n_seq = n_seq
        self.depth = depth
        self.stage = stage
        nc = bass.Bass("TRN2", target_bir_lowering=False)
        self.nc = nc
        dt = nc.dram_tensor
        self.xT = dt("xT", [n_seq, D, S], F32, kind="ExternalInput").ap()
        self.outT = dt("outT", [n_seq, D, S], F32, kind="ExternalOutput").ap()
        self.w_gu = [dt(f"w_ffn{i}_gu", [DEPTH, D, 2 * DFF], F32, kind="ExternalInput").ap() for i in (1, 2)]
        self.w_dn = [dt(f"w_ffn{i}_down", [DEPTH, DFF, D], F32, kind="ExternalInput").ap() for i in (1, 2)]
        self.w_in = dt("w_in", [DEPTH, D, D_IN], F32, kind="ExternalInput").ap()
        self.w_kv_up = dt("w_kv_up", [DEPTH, KVL, 2 * HD], F32, kind="ExternalInput").ap()
        self.w_up_fox = dt("w_up_fox", [DEPTH, W_A, D], F32, kind="ExternalInput").ap()
        self.w_up_sb = dt("w_up_sb", [DEPTH, W_B, D], F32, kind="ExternalInput").ap()
        self.w_up_dsa = dt("w_up_dsa", [DEPTH, W_C, D], F32, kind="ExternalInput").ap()
        self.w_out = dt("w_out", [DEPTH, D, D], F32, kind="ExternalInput").ap()
        self.gains = dt("gains", [128, NGAIN], F32, kind="ExternalInput").ap()
        self.smallp = dt("smallp", [128, 256], F32, kind="ExternalInput").ap()
        self.rope = dt("rope", [128, 2 * S], F32, kind="ExternalInput").ap()
        self.cbf = dt("cbf", [128, NCBF], F32, kind="ExternalInput").ap()
        self.cf32 = dt("cf32", [128, NCF32], F32, kind="ExternalInput").ap()
        self.P = Prog()

    def build(self):
        nc = self.nc
        P = self.P
        with nc.sbuf_tensor("sb", [128, SB_BYTES], U8) as sb:
            self.psum_cms = [nc.psum_tensor(f"ps{k}", [128, 512], F32) for k in range(8)]
            self.ps = [cm.__enter__()[:] for cm in self.psum_cms]
            A = Arena(sb, SB_BYTES)
            self.A = A
            self.X = A.carve(OFF_X, 128, [8, S], F32)
            self.H = A.carve(OFF_H, 128, [8, S], BF16)
            self.Y = A.carve(OFF_Y, 128, [8, S], BF16)
            o = OFF_C
            self.gain_sb = A.carve(o, 128, [NGAIN], F32); o += NGAIN * 4
            self.small_sb = A.carve(o, 128, [256], F32); o += 1024
            self.cbf_sb = A.carve(o, 128, [NCBF], BF16); o += NCBF * 2
            self.cf32_sb = A.carve(o, 128, [NCF32], F32); o += NCF32 * 4
            cb = self.cbf_sb
            self.tri_incl = cb[:, 0:128]
            self.smask = [cb[:, 128 + 512 * k: 128 + 512 * (k + 1)] for k in range(4)]
            self.negtri = cb[:, 2176:2304]
            self.negones = cb[:, 2304:2432]
            self.ident = cb[:, 2432:2560]
            self.ones_mean = cb[:, 2560:2688]
            self.ones_128th = cb[:, 2688:2816]
            self.ones_64th = cb[:, 2816:2944]
            self.tri_f32 = self.cf32_sb[:, 0:128]
            self.ones_f32 = self.cf32_sb[:, 128:256]
            self.pow2 = self.cf32_sb[:, 256:272]
            self.cst = A.carve(o, 128, [16], F32); o += 64
            self.c_off = o
            assert o <= OFF_C + 10240, o
            self.consts()
            for s in range(self.n_seq):
                self.load_x(s)
                try:
                    for l in range(self.depth):
                        self.ffn(l, 0)
                        if self.stage == 'ffn1':
                            break
                        self.mixer(l)
                        self.ffn(l, 1)
                except _Stop:
                    pass
                P.barrier()
                self.final(s)
                P.barrier()
            P.wait_all_dma('sp')
            P.emit(nc, nc.Block())
            for cm in reversed(self.psum_cms):
                cm.__exit__(None, None, None)
        return nc

    def consts(self):
        P = self.P
        P.op('pool', lambda e: e.dma_start(out=self.cbf_sb, in_=self.cbf), writes=['ones_mean'], dma=True)
        P.op('sp', lambda e: e.dma_start(out=self.cf32_sb, in_=self.cf32), writes=['cf32'], dma=True)
        P.op('pool', lambda e: e.memset(self.cst[:, 0:1], EPS), writes=['cst'])
        P.op('pool', lambda e: e.memset(self.cst[:, 1:2], 1.0), writes=['cst'])
        P.op('pool', lambda e: e.memset(self.cst[:, 2:3], 0.0), writes=['cst'])
        g = self.gain_sb
        P.op('sp', lambda e: e.dma_start(out=g, in_=self.gains), writes=['gains'], dma=True)
        sm = self.small_sb
        P.op('sp', lambda e: e.dma_start(out=sm, in_=self.smallp), writes=['smallp'], dma=True)

    def load_x(self, s):
        P = self.P
        for c in range(8):
            src = self.xT[s, c * 128:(c + 1) * 128, :]
            dst = self.X[:, c, :]
            P.op('sp', lambda e, src=src, dst=dst: e.dma_start(out=dst, in_=src),
                 writes=[f'x{c}_{t}' for t in range(NTB)], dma=True)

    def rmsnorm(self, gi, out_fn, tmp_off, t_list=None):
        P = self.P
        A = self.A
        sq = A.carve(tmp_off, 128, [2, 8, TB], BF16)
        rstd = A.carve(tmp_off + 2 * 8 * TB * 2, 128, [2, TB], F32)
        for t in (range(NTB) if t_list is None else t_list):
            k = t % 2
            ts = slice(t * TB, (t + 1) * TB)
            xin = self.X[:, :, ts]
            sqk = sq[:, k]
            P.op('pool', lambda e, xin=xin, sqk=sqk: e.tensor_tensor(out=sqk, in0=xin, in1=xin, op=ALU.mult),
                 reads=[f'x{c}_{t}' for c in range(8)], writes=[f'sq{k}'])
            ps = self.ps[6 + k]
            for c in range(8):
                P.op('pe', lambda e, ps=ps, c=c, sqk=sqk: e.matmul(ps, self.ones_mean, sqk[:, c, :],
                                                                   start=(c == 0), stop=(c == 7)),
                     reads=[f'sq{k}', 'ones_mean'], writes=[f'ps{6 + k}'])
            rk = rstd[:, k]
            P.op('act', lambda e, ps=ps, rk=rk: e.activation(out=rk, in_=ps, func=AF.Ln, bias=self.cst[:, 0:1]),
                 reads=[f'ps{6 + k}', 'cst'], writes=[f'rstd{k}'])
            P.op('act', lambda e, rk=rk: e.activation(out=rk, in_=rk, func=AF.Exp, scale=-0.5),
                 reads=[f'rstd{k}'], writes=[f'rstd{k}'])
            for c in range(8):
                dst, bname = out_fn(c, t)
                xin_c = self.X[:, c, ts]
                gcol = self.gain_sb[:, gi * 8 + c: gi * 8 + c + 1]
                eng = 'dve'
                P.op(eng, lambda e, dst=dst, xin_c=xin_c, gcol=gcol, rk=rk:
                     e.scalar_tensor_tensor(out=dst, in0=xin_c, scalar=gcol, in1=rk, op0=ALU.mult, op1=ALU.mult),
                     reads=[f'x{c}_{t}', f'rstd{k}', 'gains'], writes=[bname])

    def norm_to_H(self, gi, tmp_off):
        self.rmsnorm(gi, lambda c, t: (self.H[:, c, t * TB:(t + 1) * TB], f'h_{t}'), tmp_off)

    def ffn(self, l, which):
        P = self.P
        A = self.A
        gi = l * 3 + (0 if which == 0 else 2)
        o = OFF_A
        actT = A.carve(o, 128, [NFF // 2, S], BF16); o += (NFF // 2) * S * 2
        wgu = [A.carve(o + k * 4096, 128, [8, 2, 128], BF16) for k in range(2)]; o += 8192
        wdn = [A.carve(o + k * 2816, 128, [NFF // 2, 128], BF16) for k in range(2)]; o += 2 * 2816
        sg = [A.carve(o + k * 2048, 128, [TB], F32) for k in range(2)]; o += 4096
        assert o <= SB_BYTES, o
        P.barrier()
        self.norm_to_H(gi, OFF_A)
        P.barrier()
        self.chk(f'f{which}norm')
        w_gu = self.w_gu[which][l].rearrange("(kc p) (two n) -> p kc two n", p=128, two=2)
        w_dn = self.w_dn[which][l].rearrange("(j p) n -> p j n", p=128)
        NH = NFF // 2
        cnt = 0
        for half in range(2):
            for jj in range(NH):
                j = half * NH + jj
                wb = cnt % 2
                dst = wgu[wb]
                for two in range(2):
                    src = w_gu[:, :, two, j * 128:(j + 1) * 128]
                    dd = dst[:, :, two, :]
                    P.op('pool', lambda e, src=src, dd=dd: e.dma_start(out=dd, in_=src),
                         writes=[f'wgu{wb}_{two}'], dma=True)
                for t in range(NTB):
                    pb = (cnt * NTB + t) % 2
                    ts = slice(t * TB, (t + 1) * TB)
                    pg, pu = self.ps[pb], self.ps[2 + pb]
                    for kc in range(8):
                        P.op('pe', lambda e, pg=pg, dst=dst, kc=kc, ts=ts: e.matmul(
                            pg, dst[:, kc, 0, :], self.H[:, kc, ts], start=(kc == 0), stop=(kc == 7)),
                            reads=[f'wgu{wb}_0', f'h_{t}'], writes=[f'ps{pb}'])
                    for kc in range(8):
                        P.op('pe', lambda e, pu=pu, dst=dst, kc=kc, ts=ts: e.matmul(
                            pu, dst[:, kc, 1, :], self.H[:, kc, ts], start=(kc == 0), stop=(kc == 7)),
                            reads=[f'wgu{wb}_1', f'h_{t}'], writes=[f'ps{2 + pb}'])
                    sgk = sg[pb]
                    P.op('act', lambda e, sgk=sgk, pg=pg: e.activation(out=sgk, in_=pg, func=AF.Silu),
                         reads=[f'ps{pb}'], writes=[f'sg{pb}'])
                    adst = actT[:, jj, ts]
                    P.op('dve', lambda e, adst=adst, sgk=sgk, pu=pu: e.tensor_tensor(
                        out=adst, in0=sgk, in1=pu, op=ALU.mult),
                        reads=[f'sg{pb}', f'ps{2 + pb}'], writes=[f'act{jj}_{t}'])
                cnt += 1
            for dc in range(8):
                db = dc % 2
                src = w_dn[:, half * NH:(half + 1) * NH, dc * 128:(dc + 1) * 128]
                dst = wdn[db]
                P.op('pool', lambda e, src=src, dst=dst: e.dma_start(out=dst, in_=src),
                     writes=[f'wdn{db}'], dma=True)
                for t in range(NTB):
                    pb = 4 + (dc * NTB + t) % 2
                    ts = slice(t * TB, (t + 1) * TB)
                    po = self.ps[pb]
                    for jj in range(NH):
                        P.op('pe', lambda e, po=po, dst=dst, jj=jj, ts=ts: e.matmul(
                            po, dst[:, jj, :], actT[:, jj, ts], start=(jj == 0), stop=(jj == NH - 1)),
                            reads=[f'wdn{db}', f'act{jj}_{t}'], writes=[f'ps{pb}'])
                    xd = self.X[:, dc, ts]
                    P.op('dve', lambda e, xd=xd, po=po: e.scalar_tensor_tensor(
                        out=xd, in0=po, scalar=0.5, in1=xd, op0=ALU.mult, op1=ALU.add),
                        reads=[f'ps{pb}', f'x{dc}_{t}'], writes=[f'x{dc}_{t}'])
        self.chk(f'f{which}end')

    def chk(self, name):
        if self.stage.rstrip('XYH') == name:
            raise _Stop()

    def load_w(self, slot, src, ncols, name, dcol=0):
        dst = slot[:, :, dcol:dcol + ncols]
        self.P.op('pool', lambda e, src=src, dst=dst: e.dma_start(out=dst, in_=src), writes=[name], dma=True)

    def proj_T(self, slot, wname, M, evac, banks=(0, 1)):
        P = self.P
        for T in range(NTB):
            b = banks[T % 2]
            ps = self.ps[b]
            ts = slice(T * TB, (T + 1) * TB)
            for kc in range(8):
                P.op('pe', lambda e, ps=ps, kc=kc, ts=ts: e.matmul(ps[0:M, :], slot[:, kc, 0:M], self.H[:, kc, ts],
                                                                  start=(kc == 0), stop=(kc == 7)),
                     reads=[wname, f'h_{T}'], writes=[f'ps{b}'])
            evac(T, ps, f'ps{b}')

    def proj_tok(self, slot, wname, N, evac, banks=(0, 1)):
        P = self.P
        for g in range(4):
            b = banks[g % 2]
            ps = self.ps[b]
            for cc in range(4):
                ch = 4 * g + cc
                for kc in range(8):
                    P.op('pe', lambda e, ps=ps, kc=kc, ch=ch, cc=cc: e.matmul(
                        ps[:, cc * 128:cc * 128 + N], self.H[:, kc, ch * 128:(ch + 1) * 128], slot[:, kc, 0:N],
                        start=(kc == 0), stop=(kc == 7)),
                        reads=[wname, f'h_{ch // 4}'], writes=[f'ps{b}'])
            self.chk('tok_mm')
            evac(g, ps.rearrange("p (a b) -> p a b", a=4), f'ps{b}')
            self.chk('tok_ev')

    def attn_finish(self, O, oname, hh, ychunk, T, normalize, rden, bcs, width=TB):
        P = self.P
        pb = 64 * hh
        ts = slice(T * width, (T + 1) * width)
        ydst = self.Y[pb:pb + 64, ychunk, ts]
        yname = f'y{ychunk}_{hh}_{T}_{width}'
        O = O[:, 0:width]
        rden = rden[:, 0:width]
        bcs = bcs[:, 0:width]
        if not normalize:
            P.op('act', lambda e: e.copy(out=ydst, in_=O[pb:pb + 64, :]), reads=[oname], writes=[yname])
            return
        p = 64 if hh == 0 else 0
        P.op('dve', lambda e: e.reciprocal(out=rden[p:p + 1, :], in_=O[p:p + 1, :]), reads=[oname], writes=['rden'])
        BC = self.ps[6][:, 0:width]
        P.op('pe', lambda e: e.matmul(BC, self.ones_f32[p:p + 1, :], rden[p:p + 1, :], start=True, stop=True),
             reads=['rden'], writes=['ps6'])
        P.op('act', lambda e: e.copy(out=bcs[pb:pb + 64, :], in_=BC[pb:pb + 64, :]), reads=['ps6'], writes=['bcs'])
        P.op('dve', lambda e: e.tensor_tensor(out=ydst, in0=O[pb:pb + 64, :], in1=bcs[pb:pb + 64, :], op=ALU.mult),
             reads=[oname, 'bcs'], writes=[yname])

    def mixer(self, l):
        P = self.P
        A = self.A
        P.barrier()
        self.norm_to_H(l * 3 + 1, OFF_A)
        P.barrier()
        self.chk('norm')
        w_in = self.w_in[l].rearrange("(kc p) n -> p kc n", p=128)
        o = [OFF_A]

        def take(n):
            r = o[0]
            o[0] += n
            assert o[0] <= SB_BYTES, o[0]
            return r
        WSL = [A.carve(take(2048), 128, [8, 128], BF16) for _ in range(6)]
        qT = A.carve(take(4096), 128, [S], BF16)
        kT = A.carve(take(4096), 128, [S], BF16)
        Vp = A.carve(take(16 * 192 * 2), 128, [16, 192], BF16)
        Pt = [A.carve(take(1024), 128, [512], BF16) for _ in range(4)]
        rden = A.carve(take(2048), 128, [512], F32)
        bcs = A.carve(take(2048), 128, [512], F32)
        base_common = o[0]
        self.cnt = 0
        self.ocnt = 0

        P.op('pool', lambda e: e.memset(Vp[:, :, 64:65], 1.0), writes=['Vp_c'])
        P.op('pool', lambda e: e.memset(Vp[:, :, 65:128], 0.0), writes=['Vp_c'])

        def proj_qkv(qcol, kcol, vcol, nc_, wi):
            s0, s1, s2 = WSL[wi], WSL[wi + 1], WSL[wi + 2]
            self.load_w(s0, w_in[:, :, qcol:qcol + nc_], nc_, f'wsl{wi}')
            self.load_w(s1, w_in[:, :, kcol:kcol + nc_], nc_, f'wsl{wi + 1}')
            self.load_w(s2, w_in[:, :, vcol:vcol + nc_], nc_, f'wsl{wi + 2}')
            self.proj_T(s0, f'wsl{wi}', nc_, lambda T, ps, pn: P.op(
                'dve', lambda e: e.tensor_scalar(out=qT[0:nc_, T * TB:(T + 1) * TB], in0=ps[0:nc_, :], scalar1=0.125,
                                                 scalar2=None, op0=ALU.mult),
                reads=[pn], writes=[f'qT_{T}']))
            self.chk('projq')
            self.proj_T(s1, f'wsl{wi + 1}', nc_, lambda T, ps, pn: P.op(
                'dve', lambda e: e.tensor_copy(out=kT[0:nc_, T * TB:(T + 1) * TB], in_=ps[0:nc_, :]),
                reads=[pn], writes=[f'kT_{T}']))
            self.chk('projk')

            import os
            VAR = os.environ.get('EVVAR', 'ab')

            def ev(g, ps3, pn):
                if 'a' in VAR:
                  P.op('dve', lambda e: e.tensor_copy(out=Vp[:, 4 * g:4 * g + 4, 0:64], in_=ps3[:, :, 0:64]),
                     reads=[pn], writes=[f'Vp_{g}a'])
                if nc_ > 64 and 'b' in VAR:
                    P.op('dve', lambda e: e.tensor_copy(out=Vp[:, 4 * g:4 * g + 4, 128:192], in_=ps3[:, :, 64:128]),
                         reads=[pn], writes=[f'Vp_{g}b'])
            self.proj_tok(s2, f'wsl{wi + 2}', nc_, ev)
            self.chk('proj')

        ls = A.carve(take(384), 128, [16, 6], F32)
        tot = A.carve(take(384), 128, [16, 6], F32)
        pre = A.carve(take(17 * 24), 128, [17, 6], F32)
        cpos = A.carve(take(384), 128, [16, 6], F32)
        Btab = A.carve(take(6 * 256 * 4), 128, [6, 16, 16], F32)
        base_fox = o[0]
        self.load_w(WSL[5], w_in[:, :, O_FA:O_FA + 6], 6, 'wsl5')
        ps7 = self.ps[7]
        for ch in range(16):
            for kc in range(8):
                P.op('pe', lambda e, ch=ch, kc=kc: e.matmul(ps7[:, ch * 6:(ch + 1) * 6],
                                                              self.H[:, kc, ch * 128:(ch + 1) * 128],
                                                              WSL[5][:, kc, 0:6], start=(kc == 0), stop=(kc == 7)),
                     reads=['wsl5', f'h_{ch // 4}'], writes=['ps7'])
        lsf = ls.rearrange("p a b -> p (a b)")
        P.op('dve', lambda e: e.tensor_tensor(out=lsf, in0=ps7[:, 0:96], in1=self.small_sb[:, l * 96:(l + 1) * 96],
                                              op=ALU.add), reads=['ps7'], writes=['ls'])
        P.op('act', lambda e: e.activation(out=lsf, in_=lsf, func=AF.Exp, scale=-1.0), reads=['ls'], writes=['ls'])
        P.op('act', lambda e: e.activation(out=lsf, in_=lsf, func=AF.Ln, bias=self.cst[:, 1:2]),
             reads=['ls'], writes=['ls'])
        ps6 = self.ps[6]
        P.op('pe', lambda e: e.matmul(ps6[:, 0:96], self.tri_f32, lsf, start=True, stop=True),
             reads=['ls'], writes=['ps6'])
        P.op('pe', lambda e: e.matmul(ps6[:, 128:224], self.ones_f32, lsf, start=True, stop=True),
             reads=['ls'], writes=['ps6'])
        P.op('dve', lambda e: e.tensor_copy(out=tot.rearrange("p a b -> p (a b)"), in_=ps6[:, 128:224]),
             reads=['ps6'], writes=['tot'])
        P.op('dve', lambda e: e.memset(pre[:, 0, :], 0.0), writes=['pre'])
        for ch in range(1, 17):
            P.op('dve', lambda e, ch=ch: e.tensor_tensor(out=pre[:, ch, :], in0=pre[:, ch - 1, :],
                                                         in1=tot[:, ch - 1, :], op=ALU.add),
                 reads=['tot', 'pre'], writes=['pre'])
        P.op('dve', lambda e: e.tensor_tensor(out=cpos.rearrange("p a b -> p (a b)"), in0=ps6[:, 0:96],
                                              in1=pre[:, 0:16, :].rearrange("p a b -> p (a b)"), op=ALU.add),
             reads=['ps6', 'pre'], writes=['cpos'])
        for h in range(6):
            for tb in range(16):
                P.op('dve', lambda e, h=h, tb=tb: e.tensor_scalar(
                    out=Btab[:, h, tb, :], in0=cpos[:, :, h], scalar1=pre[:, tb + 1, h:h + 1], scalar2=None,
                    op0=ALU.subtract), reads=['cpos', 'pre'], writes=['Btab'])

        self.chk('pre')
        def softmax_attn(hh, ychunk, bias_fn, mask_fn):
            pb = 64 * hh
            for T in range(NTB):
                nsc = 4 * T + 4
                ob = 4 + (self.ocnt % 2)
                self.ocnt += 1
                O = self.ps[ob]
                for sc in range(nsc):
                    zb = 2 + (self.cnt % 2)
                    k = self.cnt % 4
                    self.cnt += 1
                    Z = self.ps[zb]
                    P.op('pe', lambda e, Z=Z, sc=sc, T=T: e.matmul(
                        Z, kT[pb:pb + 64, sc * 128:(sc + 1) * 128], qT[pb:pb + 64, T * TB:(T + 1) * TB],
                        start=True, stop=True), reads=[f'kT_{sc // 4}', f'qT_{T}'], writes=[f'ps{zb}'])
                    Ptk = Pt[k]
                    for tl in range(4):
                        tb = 4 * T + tl
                        cs = slice(tl * 128, (tl + 1) * 128)
                        pn = f'Pt{k}_{tl}'
                        if tb < sc:
                            P.op('pool', lambda e, Ptk=Ptk, cs=cs: e.memset(Ptk[:, cs], 0.0), writes=[pn])
                            continue
                        bias = bias_fn(sc, tb)
                        P.op('act', lambda e, Ptk=Ptk, cs=cs, Z=Z, bias=bias: e.activation(
                            out=Ptk[:, cs], in_=Z[:, cs], func=AF.Exp, bias=bias),
                            reads=[f'ps{zb}', 'Btab'], writes=[pn])
                        mask_fn(sc, tb, Ptk[:, cs], pn)
                    vs = slice(0, 65) if hh == 0 else slice(64, 192)
                    M = 65 if hh == 0 else 128
                    P.op('pe', lambda e, O=O, sc=sc, Ptk=Ptk, vs=vs, M=M, nsc=nsc: e.matmul(
                        O[0:M, :], Vp[:, sc, vs], Ptk, start=(sc == 0), stop=(sc == nsc - 1)),
                        reads=[f'Vp_{sc // 4}a', f'Vp_{sc // 4}b', 'Vp_c'] + [f'Pt{k}_{tl}' for tl in range(4)], writes=[f'ps{ob}'])
                    if sc == 0:
                        self.chk('fox_sc0')
                self.chk('fox_T0n')
                self.attn_finish(O, f'ps{ob}', hh, ychunk, T, True, rden, bcs)
                self.chk('fox_T0')

        def fox_mask(sc, tb, ap, pn):
            if sc == tb:
                P.op('pool', lambda e: e.tensor_tensor(out=ap, in0=ap, in1=self.tri_incl, op=ALU.mult),
                     reads=[pn], writes=[pn])

        if True:
            for hp in range(3):
                proj_qkv(O_QA + hp * 128, O_KA + hp * 128, O_VA + hp * 128, 128, 3 * (hp % 2))
                for hh in range(2):
                    h = 2 * hp + hh
                    softmax_attn(hh, hp, lambda sc, tb, h=h: Btab[:, h, tb, sc:sc + 1], fox_mask)
        P.barrier()

        o[0] = base_common
        e32 = A.carve(take(2048), 128, [512], F32)
        spb = [A.carve(take(1024), 128, [512], BF16) for _ in range(2)]
        Ab = [A.carve(take(1024), 128, [512], BF16) for _ in range(2)]
        R = A.carve(take(2048), 128, [512], F32)
        Rb = [A.carve(take(1024), 128, [512], BF16) for _ in range(2)]

        def sb_attn(hh, ychunk):
            pb = 64 * hh
            for T in range(NTB):
                nsc = 4 * T + 4
                ob = 6 + (T % 2)
                O = self.ps[ob]
                rcount = 0
                for sc in reversed(range(nsc)):
                    first = (sc == nsc - 1)
                    c2 = self.cnt % 2
                    self.cnt += 1
                    zb, lb = 2 + c2, 4 + c2
                    Z, L = self.ps[zb], self.ps[lb]
                    kk = kT[pb:pb + 64, sc * 128:(sc + 1) * 128]
                    qq = qT[pb:pb + 64, T * TB:(T + 1) * TB]
                    P.op('pe', lambda e, Z=Z, kk=kk, qq=qq: e.matmul(Z, kk, qq, start=True, stop=True),
                         reads=[f'kT_{sc // 4}', f'qT_{T}'], writes=[f'ps{zb}'])
                    P.op('act', lambda e, Z=Z: e.activation(out=e32, in_=Z, func=AF.Exp),
                         reads=[f'ps{zb}'], writes=['e32'])
                    sp = spb[c2]
                    P.op('act', lambda e, sp=sp: e.activation(out=sp, in_=e32, func=AF.Ln, bias=self.cst[:, 1:2]),
                         reads=['e32'], writes=[f'spb{c2}'])
                    if sc >= 4 * T:
                        m = self.smask[sc - 4 * T]
                        P.op('pool', lambda e, sp=sp, m=m: e.tensor_tensor(out=sp, in0=sp, in1=m, op=ALU.mult),
                             reads=[f'spb{c2}'], writes=[f'spb{c2}'])
                    P.op('pe', lambda e, L=L, kk=kk, qq=qq: e.matmul(L, kk, qq, start=True, stop=False),
                         reads=[f'kT_{sc // 4}', f'qT_{T}'], writes=[f'ps{lb}'])
                    P.op('pe', lambda e, L=L, sp=sp, first=first: e.matmul(L, self.negtri, sp, start=False, stop=first),
                         reads=[f'spb{c2}'], writes=[f'ps{lb}'])
                    if not first:
                        rb = Rb[(rcount - 1) % 2]
                        P.op('pe', lambda e, L=L, rb=rb: e.matmul(L, self.negones, rb, start=False, stop=True),
                             reads=[f'Rb{(rcount - 1) % 2}'], writes=[f'ps{lb}'])
                    ab = Ab[c2]
                    P.op('act', lambda e, ab=ab, L=L: e.activation(out=ab, in_=L, func=AF.Exp),
                         reads=[f'ps{lb}'], writes=[f'Ab{c2}'])
                    if sc >= 4 * T:
                        m = self.smask[sc - 4 * T]
                        P.op('pool', lambda e, ab=ab, m=m: e.tensor_tensor(out=ab, in0=ab, in1=m, op=ALU.mult),
                             reads=[f'Ab{c2}'], writes=[f'Ab{c2}'])
                    vs = slice(0, 64) if hh == 0 else slice(64, 192)
                    M = 64 if hh == 0 else 128
                    P.op('pe', lambda e, O=O, sc=sc, ab=ab, vs=vs, M=M, first=first: e.matmul(
                        O[0:M, :], Vp[:, sc, vs], ab, start=first, stop=(sc == 0)),
                        reads=[f'Vp_{sc // 4}a', f'Vp_{sc // 4}b', 'Vp_c', f'Ab{c2}'], writes=[f'ps{ob}'])
                    if sc > 0:
                        if first:
                            P.op('dve', lambda e, sp=sp: e.tensor_copy(out=R, in_=sp), reads=[f'spb{c2}'], writes=['R'])
                        else:
                            P.op('dve', lambda e, sp=sp: e.tensor_tensor(out=R, in0=R, in1=sp, op=ALU.add),
                                 reads=[f'spb{c2}', 'R'], writes=['R'])
                        rb = Rb[rcount % 2]
                        P.op('dve', lambda e, rb=rb: e.tensor_copy(out=rb, in_=R), reads=['R'],
                             writes=[f'Rb{rcount % 2}'])
                        rcount += 1
                self.attn_finish(O, f'ps{ob}', hh, ychunk, T, False, rden, bcs)

        self.chk('fox')
        if True:
            for hp in range(3):
                nc_ = 128 if hp < 2 else 64
                proj_qkv(O_QB + hp * 128, O_KB + hp * 128, O_VB + hp * 128, nc_, 3 * (hp % 2))
                for hh in range(2 if hp < 2 else 1):
                    sb_attn(hh, 3 + hp)
        P.barrier()
        self.chk('sb')
        self.dsa(l, w_in)
        P.barrier()
        self.chk('dsa')
        self.merge(l, w_in)
        P.barrier()
        self.chk('merge')

    def dsa(self, l, w_in):
        P = self.P
        A = self.A
        o = [OFF_A]

        def take(n):
            r = o[0]
            o[0] += n
            assert o[0] <= SB_BYTES, o[0]
            return r
        qc = [A.carve(take(4096), 128, [S], BF16) for _ in range(3)]
        kcT = A.carve(take(4096), 128, [S], BF16)
        Vc = A.carve(take(16 * 192 * 2), 128, [16, 192], BF16)
        iq = [A.carve(take(4096), 128, [S], BF16) for _ in range(2)]
        ikT = A.carve(take(4096), 128, [S], BF16)
        wI = A.carve(take(256), 128, [16, 4], F32)
        base_d2 = o[0]
        WSL = [A.carve(take(2048), 128, [8, 128], BF16) for _ in range(4)]
        ROPE = A.carve(take(16384), 128, [2, S], F32)
        ckvn = A.carve(take(4096), 128, [S], BF16)
        t32 = [A.carve(take(2048), 128, [512], F32) for _ in range(3)]
        sqb = A.carve(take(1024), 128, [512], BF16)
        COS, SIN = ROPE[:, 0, :], ROPE[:, 1, :]
        P.op('sp', lambda e: e.dma_start(out=ROPE.rearrange("p a b -> p (a b)"), in_=self.rope),
             writes=['rope'], dma=True)
        P.op('pool', lambda e: e.memset(Vc[:, :, 64:65], 1.0), writes=['Vc_c'])
        P.op('pool', lambda e: e.memset(Vc[:, :, 65:128], 0.0), writes=['Vc_c'])
        gkv = self.small_sb[:, 192 + l:193 + l]
        gidx = self.small_sb[:, 194 + l:195 + l]
        gidx_sw = self.small_sb[:, 196 + l:197 + l]

        def rstd_of(src32, T):
            P.op('pool', lambda e: e.tensor_tensor(out=sqb, in0=src32, in1=src32, op=ALU.mult),
                 reads=['t32_0'], writes=['sqb'])
            P.op('pe', lambda e: e.matmul(self.ps[6], self.ones_128th, sqb, start=True, stop=True),
                 reads=['sqb'], writes=['ps6'])
            P.op('act', lambda e: e.activation(out=t32[1], in_=self.ps[6], func=AF.Ln, bias=self.cst[:, 0:1]),
                 reads=['ps6'], writes=['t32_1'])
            P.op('act', lambda e: e.activation(out=t32[1], in_=t32[1], func=AF.Exp, scale=-0.5),
                 reads=['t32_1'], writes=['t32_1'])

        self.load_w(WSL[0], w_in[:, :, O_CKV:O_CKV + 128], 128, 'dw0')

        def ev_ckv(T, ps, pn):
            ts = slice(T * TB, (T + 1) * TB)
            P.op('act', lambda e: e.copy(out=t32[0], in_=ps), reads=[pn], writes=['t32_0'])
            rstd_of(t32[0], T)
            P.op('dve', lambda e: e.scalar_tensor_tensor(out=ckvn[:, ts], in0=t32[0], scalar=gkv, in1=t32[1],
                                                         op0=ALU.mult, op1=ALU.mult),
                 reads=['t32_0', 't32_1'], writes=[f'ckvn_{T}'])
        self.proj_T(WSL[0], 'dw0', 128, ev_ckv)

        wkv = WSL[1]
        src_kv = self.w_kv_up[l]
        P.op('pool', lambda e: e.dma_start(out=wkv[:, 0, :], in_=src_kv), writes=['dw1a'], dma=True)
        P.op('pool', lambda e: e.dma_start(out=wkv[:, 2, 0:64], in_=src_kv[:, 0:64]), writes=['dw1b'], dma=True)
        P.op('pool', lambda e: e.dma_start(out=wkv[:, 2, 64:128], in_=src_kv[:, 0:64]), writes=['dw1c'], dma=True)

        def make_swapped(dst, src, names):
            d4 = dst.rearrange("p k (h d) -> p k h d", h=2)
            s4 = src.rearrange("p k (h d) -> p k h d", h=2)
            P.op('pool', lambda e: e.tensor_copy(out=dst, in_=src), reads=names, writes=['swp'])
            P.op('pool', lambda e: e.tensor_copy(out=d4[:, :, :, 0:8], in_=s4[:, :, :, 8:16]), reads=names, writes=['swp'])
            P.op('pool', lambda e: e.tensor_copy(out=d4[:, :, :, 8:16], in_=s4[:, :, :, 0:8]), reads=names, writes=['swp'])
        make_swapped(wkv[:, 3:4, :], wkv[:, 2:3, :], ['dw1b', 'dw1c'])

        def rope_combine(T, psn, pss, names, dst, dname, pre_n=None, pre_s=None):
            ts = slice(T * TB, (T + 1) * TB)
            P.op('dve', lambda e: e.tensor_tensor(out=t32[0], in0=psn, in1=COS[:, ts], op=ALU.mult),
                 reads=[names[0], 'rope'], writes=['t32_0'])
            P.op('dve', lambda e: e.tensor_tensor(out=t32[2], in0=pss, in1=SIN[:, ts], op=ALU.mult),
                 reads=[names[1], 'rope'], writes=['t32_2'])
            P.op('pool', lambda e: e.tensor_tensor(out=dst[:, ts], in0=t32[0], in1=t32[2], op=ALU.add),
                 reads=['t32_0', 't32_2'], writes=[dname])

        for T in range(NTB):
            ts = slice(T * TB, (T + 1) * TB)
            P.op('pe', lambda e, ts=ts: e.matmul(self.ps[0], wkv[:, 2, :], ckvn[:, ts], start=True, stop=True),
                 reads=['dw1b', 'dw1c', f'ckvn_{T}'], writes=['ps0'])
            P.op('pe', lambda e, ts=ts: e.matmul(self.ps[1], wkv[:, 3, :], ckvn[:, ts], start=True, stop=True),
                 reads=['swp', f'ckvn_{T}'], writes=['ps1'])
            rope_combine(T, self.ps[0], self.ps[1], ['ps0', 'ps1'], kcT, 'kcT')
        for g in range(4):
            b = 2 + g % 2
            ps = self.ps[b]
            for cc in range(4):
                ch = 4 * g + cc
                P.op('pe', lambda e, ps=ps, cc=cc, ch=ch: e.matmul(ps[:, cc * 128:cc * 128 + 64],
                                                                  ckvn[:, ch * 128:(ch + 1) * 128], wkv[:, 0, 64:128],
                                                                  start=True, stop=True),
                     reads=['dw1a', f'ckvn_{g}'], writes=[f'ps{b}'])
            ps3 = ps.rearrange("p (a b) -> p a b", a=4)
            P.op('dve', lambda e, ps3=ps3, g=g: e.tensor_copy(out=Vc[:, 4 * g:4 * g + 4, 0:64], in_=ps3[:, :, 0:64]),
                 reads=[f'ps{b}'], writes=['Vc_a'])
            P.op('dve', lambda e, ps3=ps3, g=g: e.tensor_copy(out=Vc[:, 4 * g:4 * g + 4, 128:192], in_=ps3[:, :, 0:64]),
                 reads=[f'ps{b}'], writes=['Vc_b'])

        def rope_pair(col_lo, col_hi, dst, dname):
            wn, ws = WSL[2], WSL[3]
            if col_hi == col_lo + 64:
                self.load_w(wn, w_in[:, :, col_lo:col_lo + 128], 128, 'dw2a')
                nm = ['dw2a']
            else:
                self.load_w(wn, w_in[:, :, col_lo:col_lo + 64], 64, 'dw2a')
                self.load_w(wn, w_in[:, :, col_hi:col_hi + 64], 64, 'dw2b', dcol=64)
                nm = ['dw2a', 'dw2b']
            make_swapped(ws, wn, nm)
            for T in range(NTB):
                ts = slice(T * TB, (T + 1) * TB)
                for kc in range(8):
                    P.op('pe', lambda e, kc=kc, ts=ts: e.matmul(self.ps[0], wn[:, kc, :], self.H[:, kc, ts],
                                                                start=(kc == 0), stop=(kc == 7)),
                         reads=nm + [f'h_{T}'], writes=['ps0'])
                for kc in range(8):
                    P.op('pe', lambda e, kc=kc, ts=ts: e.matmul(self.ps[1], ws[:, kc, :], self.H[:, kc, ts],
                                                                start=(kc == 0), stop=(kc == 7)),
                         reads=['swp', f'h_{T}'], writes=['ps1'])
                rope_combine(T, self.ps[0], self.ps[1], ['ps0', 'ps1'], dst, dname)

        rope_pair(O_QC, O_QC, qc[0], 'qc0')
        rope_pair(O_QC + 64, O_QC + 128, qc[1], 'qc1')
        rope_pair(O_QC + 192, O_QC + 256, qc[2], 'qc2')
        rope_pair(O_QI, O_QI + 64, iq[0], 'iq0')
        rope_pair(O_QI + 128, O_QI + 192, iq[1], 'iq1')

        wn, ws = WSL[2], WSL[3]
        self.load_w(wn, w_in[:, :, O_KI:O_KI + 64], 64, 'dw2a')
        self.load_w(wn, w_in[:, :, O_KI:O_KI + 64], 64, 'dw2b', dcol=64)
        make_swapped(ws, wn, ['dw2a', 'dw2b'])
        for T in range(NTB):
            ts = slice(T * TB, (T + 1) * TB)
            for kc in range(8):
                P.op('pe', lambda e, kc=kc, ts=ts: e.matmul(self.ps[0], wn[:, kc, :], self.H[:, kc, ts],
                                                            start=(kc == 0), stop=(kc == 7)),
                     reads=['dw2a', 'dw2b', f'h_{T}'], writes=['ps0'])
            for kc in range(8):
                P.op('pe', lambda e, kc=kc, ts=ts: e.matmul(self.ps[1], ws[:, kc, :], self.H[:, kc, ts],
                                                            start=(kc == 0), stop=(kc == 7)),
                     reads=['swp', f'h_{T}'], writes=['ps1'])
            P.op('act', lambda e: e.copy(out=t32[0], in_=self.ps[0]), reads=['ps0'], writes=['t32_0'])
            rstd_of(t32[0], T)
            P.op('dve', lambda e: e.scalar_tensor_tensor(out=t32[0], in0=t32[0], scalar=gidx, in1=t32[1],
                                                         op0=ALU.mult, op1=ALU.mult),
                 reads=['t32_0', 't32_1'], writes=['t32_0'])
            P.op('dve', lambda e: e.scalar_tensor_tensor(out=t32[2], in0=self.ps[1], scalar=gidx_sw, in1=t32[1],
                                                         op0=ALU.mult, op1=ALU.mult),
                 reads=['ps1', 't32_1'], writes=['t32_2'])
            P.op('dve', lambda e, ts=ts: e.tensor_tensor(out=t32[0], in0=t32[0], in1=COS[:, ts], op=ALU.mult),
                 reads=['t32_0', 'rope'], writes=['t32_0'])
            P.op('dve', lambda e, ts=ts: e.tensor_tensor(out=t32[2], in0=t32[2], in1=SIN[:, ts], op=ALU.mult),
                 reads=['t32_2', 'rope'], writes=['t32_2'])
            P.op('pool', lambda e, ts=ts: e.tensor_tensor(out=ikT[:, ts], in0=t32[0], in1=t32[2], op=ALU.add),
                 reads=['t32_0', 't32_2'], writes=['ikT'])

        self.load_w(WSL[0], w_in[:, :, O_WI:O_WI + 4], 4, 'dw0')
        self.proj_tok(WSL[0], 'dw0', 4, lambda g, ps3, pn: P.op(
            'dve', lambda e: e.tensor_copy(out=wI[:, 4 * g:4 * g + 4, :], in_=ps3[:, :, 0:4]),
            reads=[pn], writes=['wI']), banks=(2, 3))
        P.barrier()

        o[0] = base_d2
        sc32 = A.carve(take(8192), 128, [S], F32)
        mask = A.carve(take(4096), 128, [S], BF16)
        maskT = A.carve(take(4096), 128, [16, 128], BF16)
        Pt = [A.carve(take(256), 128, [128], BF16) for _ in range(4)]
        tt = [A.carve(take(2048), 128, [512], F32) for _ in range(2)]
        sm = A.carve(take(256), 128, [64], F32)
        mx, mn, d0, mid, cntc, tmp1, theta = [sm[:, i:i + 1] for i in range(7)]
        halfs = sm[:, 16:32]
        ps7b = self.ps[7].bitcast(BF16)
        maskT2 = [maskT, A.carve(take(4096), 128, [16, 128], BF16)]
        fin = [[A.carve(take(512), 128, [128], F32) for _ in range(3)] for _ in range(6)]
        st = {'cnt': 0, 'ocnt': 0, 'icnt': 0}

        def A1(tb):
            L = (tb + 1) * 128
            qs = slice(tb * 128, (tb + 1) * 128)
            for j in range((L + 511) // 512):
                w = min(512, L - 512 * j)
                cs = slice(512 * j, 512 * j + w)
                for h in range(NIH):
                    pb = 64 * (h % 2)
                    zb = st['icnt'] % 2
                    k2 = st['icnt'] % 2
                    st['icnt'] += 1
                    Z = self.ps[zb]
                    P.op('pe', lambda e, Z=Z, h=h, pb=pb, cs=cs, w=w, qs=qs: e.matmul(
                        Z[:, 0:w], iq[h // 2][pb:pb + 64, qs], ikT[pb:pb + 64, cs], start=True, stop=True),
                        reads=[f'iq{h // 2}', 'ikT'], writes=[f'ps{zb}'])
                    if h == 0:
                        P.op('dve', lambda e, Z=Z, cs=cs, w=w, tb=tb: e.tensor_scalar(
                            out=sc32[:, cs], in0=Z[:, 0:w], scalar1=0.0, scalar2=wI[:, tb, 0:1],
                            op0=ALU.max, op1=ALU.mult), reads=[f'ps{zb}', 'wI'], writes=[f'sc_{j}'])
                    else:
                        P.op('dve', lambda e, Z=Z, w=w, tb=tb, h=h, k2=k2: e.tensor_scalar(
                            out=tt[k2][:, 0:w], in0=Z[:, 0:w], scalar1=0.0, scalar2=wI[:, tb, h:h + 1],
                            op0=ALU.max, op1=ALU.mult), reads=[f'ps{zb}', 'wI'], writes=[f'tt{k2}'])
                        P.op('pool', lambda e, cs=cs, w=w, k2=k2: e.tensor_tensor(
                            out=sc32[:, cs], in0=sc32[:, cs], in1=tt[k2][:, 0:w], op=ALU.add),
                            reads=[f'tt{k2}', f'sc_{j}'], writes=[f'sc_{j}'])
            scn = [f'sc_{j}' for j in range((L + 511) // 512)]
            if tb >= 2:
                P.op('dve', lambda e, L=L: e.tensor_reduce(out=mx, in_=sc32[:, 0:L], axis=mybir.AxisListType.X,
                                                           op=ALU.max), reads=scn, writes=['mx'])
                P.op('dve', lambda e, L=L: e.tensor_reduce(out=mn, in_=sc32[:, 0:L], axis=mybir.AxisListType.X,
                                                           op=ALU.min), reads=scn, writes=['mn'])
            P.op('pool', lambda e, L=L: e.memset(sc32[0:64, L - 64:L], -1e30), reads=scn + ['mx', 'mn'],
                 writes=[scn[-1]])
            return scn

        def Abis(tb, scn):
            L = (tb + 1) * 128
            if tb >= 2:
                P.op('dve', lambda e: e.tensor_tensor(out=d0, in0=mx, in1=mn, op=ALU.subtract),
                     reads=['mx', 'mn'], writes=['d0'])
                P.op('dve', lambda e: e.tensor_scalar(out=halfs, in0=self.pow2, scalar1=d0, scalar2=None,
                                                      op0=ALU.mult), reads=['d0'], writes=['halfs'])
                P.op('dve', lambda e: e.tensor_tensor(out=mid, in0=mn, in1=halfs[:, 0:1], op=ALU.add),
                     reads=['mn', 'halfs'], writes=['mid'])
                for k in range(16):
                    P.op('dve', lambda e, L=L: e.tensor_scalar(out=mask[:, 0:L], in0=sc32[:, 0:L], scalar1=mid,
                                                               scalar2=None, op0=ALU.is_ge, op1=ALU.add,
                                                               accum_out=cntc),
                         reads=scn + ['mid'], writes=['mask', 'cntc'])
                    P.op('dve', lambda e: e.tensor_scalar(out=tmp1, in0=cntc, scalar1=256.0, scalar2=0.5,
                                                          op0=ALU.is_ge, op1=ALU.subtract),
                         reads=['cntc'], writes=['tmp1'])
                    P.op('dve', lambda e, k=k: e.scalar_tensor_tensor(out=mid, in0=tmp1, scalar=halfs[:, k:k + 1],
                                                                      in1=mid, op0=ALU.mult, op1=ALU.add),
                         reads=['tmp1', 'halfs', 'mid'], writes=['mid'])
                P.op('dve', lambda e: e.scalar_tensor_tensor(out=theta, in0=halfs[:, 15:16], scalar=-0.5, in1=mid,
                                                             op0=ALU.mult, op1=ALU.add),
                     reads=['mid', 'halfs'], writes=['theta'])
            else:
                P.op('dve', lambda e: e.memset(theta, -1e29), writes=['theta'])

        def A2(tb, scn):
            L = (tb + 1) * 128
            mT = maskT2[tb % 2]
            P.op('dve', lambda e, L=L: e.tensor_scalar(out=mask[:, 0:L], in0=sc32[:, 0:L], scalar1=theta,
                                                       scalar2=None, op0=ALU.is_ge),
                 reads=scn + ['theta'], writes=['mask'])
            for g0 in range(0, tb + 1, 8):
                n = min(8, tb + 1 - g0)
                for i in range(n):
                    sc = g0 + i
                    P.op('pe', lambda e, i=i, sc=sc: e.transpose(ps7b[:, i * 128:(i + 1) * 128],
                                                                 mask[:, sc * 128:(sc + 1) * 128], self.ident),
                         reads=['mask'], writes=['ps7'])
                P.op('act', lambda e, g0=g0, n=n, mT=mT: e.copy(out=mT[:, g0:g0 + n, :].rearrange("p a b -> p (a b)"),
                                                                in_=ps7b[:, 0:n * 128]),
                     reads=['ps7'], writes=[f'maskT{tb % 2}'])

        def Battn(tb):
            qs = slice(tb * 128, (tb + 1) * 128)
            mT = maskT2[tb % 2]
            for hc in range(NH_C):
                i = (hc + 1) // 2
                hh = (hc + 1) % 2
                pb = 64 * hh
                ob = 4 + st['ocnt'] % 2
                fb = st['ocnt'] % 6
                st['ocnt'] += 1
                O = self.ps[ob]
                for sc in range(tb + 1):
                    zb = 2 + st['cnt'] % 2
                    k = st['cnt'] % 4
                    st['cnt'] += 1
                    Z = self.ps[zb]
                    P.op('pe', lambda e, Z=Z, sc=sc, i=i, pb=pb, qs=qs: e.matmul(
                        Z[:, 0:128], kcT[pb:pb + 64, sc * 128:(sc + 1) * 128], qc[i][pb:pb + 64, qs],
                        start=True, stop=True), reads=['kcT', f'qc{i}'], writes=[f'ps{zb}'])
                    Ptk = Pt[k]
                    P.op('act', lambda e, Ptk=Ptk, Z=Z: e.activation(out=Ptk, in_=Z[:, 0:128], func=AF.Exp, scale=0.125),
                         reads=[f'ps{zb}'], writes=[f'dPt{k}'])
                    P.op('pool', lambda e, Ptk=Ptk, sc=sc, mT=mT: e.tensor_tensor(out=Ptk, in0=Ptk, in1=mT[:, sc, :],
                                                                                  op=ALU.mult),
                         reads=[f'dPt{k}', f'maskT{tb % 2}'], writes=[f'dPt{k}'])
                    vs = slice(0, 65) if hh == 0 else slice(64, 192)
                    M = 65 if hh == 0 else 128
                    P.op('pe', lambda e, O=O, sc=sc, Ptk=Ptk, vs=vs, M=M, tb=tb: e.matmul(
                        O[0:M, 0:128], Vc[:, sc, vs], Ptk, start=(sc == 0), stop=(sc == tb)),
                        reads=['Vc_a', 'Vc_b', 'Vc_c', f'dPt{k}'], writes=[f'ps{ob}'])
                rd, bc_, osb = fin[fb]
                p = 64 if hh == 0 else 0
                P.op('act', lambda e, rd=rd, O=O, p=p: e.activation(out=rd[:, :], in_=O[:, 0:128], func=AF.Ln),
                     reads=[f'ps{ob}'], writes=[f'frd{fb}'])
                P.op('act', lambda e, rd=rd, p=p: e.activation(out=rd[:, :], in_=rd[:, :], func=AF.Exp, scale=-1.0),
                     reads=[f'frd{fb}'], writes=[f'frd{fb}'])
                BC = self.ps[6][:, 0:128]
                P.op('pe', lambda e, rd=rd, p=p, BC=BC: e.matmul(BC, self.ones_f32[p:p + 1, :], rd[p:p + 1, :],
                                                                 start=True, stop=True),
                     reads=[f'frd{fb}'], writes=['ps6'])
                P.op('act', lambda e, bc_=bc_, pb=pb, BC=BC: e.copy(out=bc_[pb:pb + 64, :], in_=BC[pb:pb + 64, :]),
                     reads=['ps6'], writes=[f'fbc{fb}'])
                P.op('act', lambda e, osb=osb, pb=pb, O=O: e.copy(out=osb[pb:pb + 64, :], in_=O[pb:pb + 64, 0:128]),
                     reads=[f'ps{ob}'], writes=[f'fos{fb}'])
                ydst = self.Y[pb:pb + 64, 5 + i, qs]
                P.op('dve', lambda e, ydst=ydst, osb=osb, bc_=bc_, pb=pb: e.tensor_tensor(
                    out=ydst, in0=osb[pb:pb + 64, :], in1=bc_[pb:pb + 64, :], op=ALU.mult),
                    reads=[f'fos{fb}', f'fbc{fb}'], writes=[f'y{5 + i}_{hh}_{tb}_128'])

        scn_cur = A1(0)
        Abis(0, scn_cur)
        A2(0, scn_cur)
        for tb in range(NQB):
            if tb + 1 < NQB:
                scn_nxt = A1(tb + 1)
                Abis(tb + 1, scn_nxt)
            Battn(tb)
            if tb + 1 < NQB:
                A2(tb + 1, scn_nxt)

    def merge(self, l, w_in):
        P = self.P
        A = self.A
        o = [OFF_A]

        def take(n):
            r = o[0]
            o[0] += n
            assert o[0] <= SB_BYTES, o[0]
            return r
        merged = A.carve(take(8 * S * 2), 128, [8, S], BF16)
        wg = [A.carve(take(6144), 128, [8, 384], BF16) for _ in range(2)]
        wu = [A.carve(take(9 * 256), 128, [9, 128], BF16) for _ in range(2)]
        wo = [A.carve(take(2048), 128, [8, 128], BF16) for _ in range(2)]
        sg = [A.carve(take(2048), 128, [512], F32) for _ in range(3)]
        mm = [A.carve(take(2048), 128, [512], F32) for _ in range(3)]
        ufox = self.w_up_fox[l].rearrange("(j p) n -> p j n", p=128)
        usb = self.w_up_sb[l]
        udsa = self.w_up_dsa[l]
        wout = self.w_out[l].rearrange("(kc p) n -> p kc n", p=128)
        for c in range(8):
            k = c % 2
            cs = slice(c * 128, (c + 1) * 128)
            for b in range(3):
                src = w_in[:, :, O_G + b * D + c * 128:O_G + b * D + (c + 1) * 128]
                dst = wg[k][:, :, b * 128:(b + 1) * 128]
                P.op('pool', lambda e, src=src, dst=dst: e.dma_start(out=dst, in_=src), writes=[f'wg{k}_{b}'], dma=True)
            W = wu[k]
            dl = [
                (W[:, 0:3, :], ufox[:, :, cs]),
                (W[:, 3:5, :], usb[0:256, :].rearrange("(j p) n -> p j n", p=128)[:, :, cs]),
                (W[0:64, 5, :], usb[256:320, cs]),
                (W[64:128, 6, :], udsa[0:64, cs]),
                (W[:, 7:9, :], udsa[64:320, :].rearrange("(j p) n -> p j n", p=128)[:, :, cs]),
            ]
            for i, (dst, src) in enumerate(dl):
                P.op('pool', lambda e, src=src, dst=dst: e.dma_start(out=dst, in_=src), writes=[f'wu{k}_{i}'], dma=True)
            wun = [f'wu{k}_{i}' for i in range(5)]
            for T in range(NTB):
                ts = slice(T * TB, (T + 1) * TB)
                yn = [nm for nm in self.P.bufs if nm.startswith('y') and nm.endswith(f'_{T}')]
                for b in range(3):
                    G = self.ps[b]
                    for kc in range(8):
                        P.op('pe', lambda e, G=G, kc=kc, b=b, ts=ts, k=k: e.matmul(
                            G, wg[k][:, kc, b * 128:(b + 1) * 128], self.H[:, kc, ts], start=(kc == 0), stop=(kc == 7)),
                            reads=[f'wg{k}_{b}', f'h_{T}'], writes=[f'ps{b}'])
                    U = self.ps[3 + b]
                    if b == 0:
                        terms = [(W[:, j, :], self.Y[:, j, ts]) for j in range(3)]
                    elif b == 1:
                        terms = [(W[:, 3, :], self.Y[:, 3, ts]), (W[:, 4, :], self.Y[:, 4, ts]),
                                 (W[0:64, 5, :], self.Y[0:64, 5, ts])]
                    else:
                        terms = [(W[64:128, 6, :], self.Y[64:128, 5, ts]), (W[:, 7, :], self.Y[:, 6, ts]),
                                 (W[:, 8, :], self.Y[:, 7, ts])]
                    for ti, (lh, rh) in enumerate(terms):
                        P.op('pe', lambda e, U=U, lh=lh, rh=rh, ti=ti: e.matmul(U, lh, rh, start=(ti == 0), stop=(ti == 2)),
                             reads=wun + ['yall'], writes=[f'ps{3 + b}'])
                    P.op('act', lambda e, G=G, b=b: e.activation(out=sg[b], in_=G, func=AF.Sigmoid),
                         reads=[f'ps{b}'], writes=[f'sg{b}'])
                    if self.stage.rstrip('XYH') == 'mgd':
                        P.op('dve', lambda e, b=b, ts=ts: e.tensor_copy(out=self.X[:, b, ts], in_=sg[b]),
                             reads=[f'sg{b}'], writes=[f'x{b}_{T}'])
                        P.op('dve', lambda e, b=b, ts=ts, U=U: e.tensor_copy(out=self.X[:, 3 + b, ts], in_=U),
                             reads=[f'ps{3 + b}'], writes=[f'x{3 + b}_{T}'])
                    P.op('dve', lambda e, U=U, b=b: e.tensor_tensor(out=mm[b], in0=sg[b], in1=U, op=ALU.mult),
                         reads=[f'sg{b}', f'ps{3 + b}'], writes=[f'mm{b}'])
                P.op('pool', lambda e: e.tensor_tensor(out=mm[0], in0=mm[0], in1=mm[1], op=ALU.add),
                     reads=['mm0', 'mm1'], writes=['mm0'])
                P.op('pool', lambda e, c=c, ts=ts: e.tensor_tensor(out=merged[:, c, ts], in0=mm[0], in1=mm[2], op=ALU.add),
                     reads=['mm0', 'mm2'], writes=[f'mg_{T}'])
                if self.stage.rstrip('XYH') == 'mgd':
                    P.op('dve', lambda e, ts=ts: e.tensor_copy(out=self.X[:, 6, ts], in_=mm[0]), reads=['mm0'], writes=[f'x6_{T}'])
                    P.op('dve', lambda e, ts=ts, c=c: e.tensor_copy(out=self.X[:, 7, ts], in_=merged[:, c, ts]), reads=[f'mg_{T}'], writes=[f'x7_{T}'])
            self.chk('mgd')
        if self.stage.rstrip('XYH') == 'mg':
            for c in range(8):
                P.op('dve', lambda e, c=c: e.tensor_copy(out=self.X[:, c, :], in_=merged[:, c, :]),
                     reads=[f'mg_{T}' for T in range(NTB)], writes=[f'x{c}_{T}' for T in range(NTB)])
            self.chk('mg')
        for c2 in range(8):
            k = c2 % 2
            src = wout[:, :, c2 * 128:(c2 + 1) * 128]
            dst = wo[k]
            P.op('pool', lambda e, src=src, dst=dst: e.dma_start(out=dst, in_=src), writes=[f'wo{k}'], dma=True)
            for T in range(NTB):
                ts = slice(T * TB, (T + 1) * TB)
                pb_ = 6 + (c2 * NTB + T) % 2
                po = self.ps[pb_]
                for kc in range(8):
                    P.op('pe', lambda e, po=po, kc=kc, dst=dst, ts=ts: e.matmul(
                        po, dst[:, kc, :], merged[:, kc, ts], start=(kc == 0), stop=(kc == 7)),
                        reads=[f'wo{k}', f'mg_{T}'], writes=[f'ps{pb_}'])
                xd = self.X[:, c2, ts]
                P.op('dve', lambda e, xd=xd, po=po: e.tensor_tensor(out=xd, in0=xd, in1=po, op=ALU.add),
                     reads=[f'ps{pb_}', f'x{c2}_{T}'], writes=[f'x{c2}_{T}'])

    def final(self, s):
        P = self.P
        A = self.A
        gi = 3 * DEPTH
        o = OFF_A
        stg = [A.carve(o + k * 16384, 128, [8, TB], F32) for k in range(2)]; o += 32768
        tmp_off = o
        for t in range(NTB):
            k = t % 2
            if self.stage[-1] in 'XYH':
                SRC = {'X': self.X, 'Y': self.Y, 'H': self.H}[self.stage[-1]]
                for c in range(8):
                    P.op('dve', lambda e, c=c, t=t, k=k: e.tensor_copy(out=stg[k][:, c, :], in_=SRC[:, c, t * TB:(t + 1) * TB]),
                         reads=[f'x{c}_{t}'], writes=[f'stg{k}_{c}'])
            else:
                self.rmsnorm(gi, lambda c, t, k=k: (stg[k][:, c, :], f'stg{k}_{c}'), tmp_off, t_list=[t])
            for c in range(8):
                dst = self.outT[s, c * 128:(c + 1) * 128, t * TB:(t + 1) * TB]
                src = stg[k][:, c, :]
                P.op('sp', lambda e, src=src, dst=dst: e.dma_start(out=dst, in_=src),
                     reads=[f'stg{k}_{c}'], dma=True)


def pack_gains(inp):
    g = np.zeros((128, NGAIN), np.float32)
    for l in range(DEPTH):
        for k, name in enumerate(("g_ffn1", "g_mix", "g_ffn2")):
            g[:, (l * 3 + k) * 8:(l * 3 + k + 1) * 8] = np.asarray(inp[name][l], np.float32).reshape(8, 128).T
    g[:, 3 * DEPTH * 8:] = np.asarray(inp["g_final"], np.float32).reshape(8, 128).T
    return g


def rope_consts():
    pos = np.arange(S, dtype=np.float32)
    inv = (np.float32(500000.0) ** (-np.arange(0, 16, 2, dtype=np.float32) / np.float32(16))).astype(np.float32)
    ang = pos[None, :] * inv[:, None]
    cos, sin = np.cos(ang).astype(np.float32), np.sin(ang).astype(np.float32)
    r = np.zeros((128, 2, S), np.float32)
    r[:, 0, :] = 1.0
    for p in range(128):
        d = p % 64
        if d < 16:
            r[p, 0] = cos[d % 8]
            r[p, 1] = -sin[d % 8] if d < 8 else sin[d % 8]
    return r.reshape(128, 2 * S)


def const_tables():
    i = np.arange(128)
    cbf = np.zeros((128, NCBF), np.float32)
    cbf[:, 0:128] = (i[:, None] <= i[None, :])
    for k in range(4):
        m = np.zeros((128, 4, 128), np.float32)
        m[:, k, :] = (i[:, None] < i[None, :])
        m[:, k + 1:, :] = 1.0
        cbf[:, 128 + 512 * k:128 + 512 * (k + 1)] = m.reshape(128, 512)
    cbf[:, 2176:2304] = -(i[:, None] >= i[None, :]).astype(np.float32)
    cbf[:, 2304:2432] = -1.0
    cbf[:, 2432:2560] = np.eye(128, dtype=np.float32)
    cbf[:, 2560:2688] = 1.0 / 1024
    cbf[:, 2688:2816] = 1.0 / 128
    cbf[:, 2816:2944] = 1.0 / 64
    cf = np.zeros((128, NCF32), np.float32)
    cf[:, 0:128] = (i[:, None] <= i[None, :])
    cf[:, 128:256] = 1.0
    cf[:, 256:272] = 2.0 ** -(np.arange(16) + 1.0)
    return cbf, cf


def make_in_maps(inp, n_cores, n_seq):
    x = np.asarray(inp["x"], np.float32)
    gains = pack_gains(inp)
    smallp = np.zeros((128, 256), np.float32)
    for l in range(DEPTH):
        smallp[:, l * 96:(l + 1) * 96] = np.tile(np.asarray(inp["b_forget"][l], np.float32), 16)[None, :]
        smallp[:, 192 + l] = np.asarray(inp["g_kv_latent"][l], np.float32)
        smallp[:64, 194 + l] = np.asarray(inp["g_idx_k"][l], np.float32)
        smallp[64:, 194 + l] = np.asarray(inp["g_idx_k"][l], np.float32)
        gsw = np.asarray(inp["g_idx_k"][l], np.float32).copy()
        gsw[0:8], gsw[8:16] = gsw[8:16].copy(), gsw[0:8].copy()
        smallp[:64, 196 + l] = gsw
        smallp[64:, 196 + l] = gsw
    rope = rope_consts()
    cbf, cf32 = const_tables()
    maps = []
    shared = {
        "w_ffn1_gu": np.asarray(inp["w_ffn1_gu"], np.float32), "w_ffn2_gu": np.asarray(inp["w_ffn2_gu"], np.float32),
        "w_ffn1_down": np.asarray(inp["w_ffn1_down"], np.float32),
        "w_ffn2_down": np.asarray(inp["w_ffn2_down"], np.float32),
        "w_in": np.asarray(inp["w_in"], np.float32), "w_kv_up": np.asarray(inp["w_kv_up"], np.float32),
        "w_up_fox": np.asarray(inp["w_up_fox"], np.float32), "w_up_sb": np.asarray(inp["w_up_sb"], np.float32),
        "w_up_dsa": np.asarray(inp["w_up_dsa"], np.float32), "w_out": np.asarray(inp["w_out"], np.float32),
        "gains": gains, "smallp": smallp, "rope": rope, "cbf": cbf, "cf32": cf32,
    }
    for cidx in range(n_cores):
        xs = x[cidx * n_seq:(cidx + 1) * n_seq]
        m = dict(shared)
        m["xT"] = np.ascontiguousarray(xs.transpose(0, 2, 1))
        maps.append(m)
    return maps


def kernel(**inputs):
    n_cores, n_seq = 8, 2
    mdl = Model(n_seq=n_seq)
    nc = mdl.build()
    maps = make_in_maps(inputs, n_cores, n_seq)
    res = run_bass_kernel_spmd(nc, maps, core_ids=list(range(n_cores)))
    outs = [r["outT"].transpose(0, 2, 1) for r in res.results]
    return np.ascontiguousarray(np.concatenate(outs, axis=0)).astype(np.float32)
```

```python
import numpy as np
import concourse.bass as bass
import concourse.mybir as mybir
from concourse.bass_utils import run_bass_kernel_spmd

F32 = mybir.dt.float32
BF16 = mybir.dt.bfloat16
U8 = mybir.dt.uint8
AF = mybir.ActivationFunctionType
ALU = mybir.AluOpType

D = 1024
S = 2048
DEPTH = 2
DFF = 2816
NFF = DFF // 128
HD = 64
NH_A, NH_B, NH_C = 6, 5, 5
W_A, W_B, W_C = 384, 320, 320
KVL = 128
NIH = 4
D_IN = 5962
EPS = 1e-6
TB = 512
NTB = S // TB
NQB = S // 128

O_QA = 0
O_KA = O_QA + W_A
O_VA = O_KA + W_A
O_FA = O_VA + W_A
O_QB = O_FA + NH_A
O_KB = O_QB + W_B
O_VB = O_KB + W_B
O_QC = O_VB + W_B
O_CKV = O_QC + W_C
O_QI = O_CKV + KVL
O_KI = O_QI + NIH * 64
O_WI = O_KI + 64
O_G = O_WI + NIH
assert O_G + 3 * D == D_IN

EPOCH = 4096
ENGS = ['pe', 'act', 'dve', 'pool', 'sp']


class Buf:
    __slots__ = ('name', 'lw', 'rd')

    def __init__(self, name):
        self.name = name
        self.lw = None
        self.rd = {}


class Prog:
    NRING = 8

    def __init__(self):
        self.ops = {e: [] for e in ENGS}
        self.waited = {e: {} for e in ENGS}
        self.ndma = {e: 0 for e in ENGS}
        self.bufs = {}

    def buf(self, name):
        b = self.bufs.get(name)
        if b is None:
            b = Buf(name)
            self.bufs[name] = b
        return b

    def _tok(self, names):
        return [self.buf(n) if isinstance(n, str) else n for n in names]

    def op(self, eng, emit, reads=(), writes=(), dma=False):
        reads = self._tok(reads)
        writes = self._tok(writes)
        idx = len(self.ops[eng])
        deps = set()
        for b in reads:
            if b.lw is not None:
                deps.add(b.lw)
        for b in writes:
            if b.lw is not None:
                deps.add(b.lw)
            for t in b.rd.values():
                deps.add(t)
        if dma:
            d = self.ndma[eng]
            self.ndma[eng] += 1
            tok = ('d', eng, d)
            if d >= self.NRING:
                deps.add(('d', eng, d - self.NRING))
        else:
            tok = ('c', eng, idx)
        waits = []
        w = self.waited[eng]
        for t in deps:
            if t[0] == 'c':
                _, e, i = t
                if e == eng and eng == 'pe':
                    continue
                if w.get(('c', e), -1) >= i:
                    continue
                w[('c', e)] = i
                self.ops[e][i][2] = True
                waits.append(t)
            else:
                _, q, d0 = t
                key = ('d', q, d0 % self.NRING)
                if w.get(key, -1) >= d0:
                    continue
                w[key] = d0
                waits.append(t)
        self.ops[eng].append([emit, waits, False, tok if dma else None])
        rkey = (tok[0], tok[1]) if tok[0] == 'c' else (tok[0], tok[1], tok[2] % self.NRING)
        for b in reads:
            b.rd[rkey] = tok
        for b in writes:
            b.lw = tok
            b.rd = {}
        return tok

    def barrier(self):
        last = {}
        for e in ENGS:
            for i in range(len(self.ops[e]) - 1, -1, -1):
                o = self.ops[e][i]
                if o[0] is not None and o[3] is None:
                    last[e] = i
                    break
        for eng in ENGS:
            waits = []
            w = self.waited[eng]
            for e, i in last.items():
                if e == eng:
                    continue
                if w.get(('c', e), -1) >= i:
                    continue
                w[('c', e)] = i
                self.ops[e][i][2] = True
                waits.append(('c', e, i))
            for q in ENGS:
                n = self.ndma[q]
                for d0 in range(max(0, n - self.NRING), n):
                    key = ('d', q, d0 % self.NRING)
                    if w.get(key, -1) >= d0:
                        continue
                    w[key] = d0
                    waits.append(('d', q, d0))
            self.ops[eng].append([None, waits, False, None])
        for b in self.bufs.values():
            b.lw = None
            b.rd = {}

    def wait_all_dma(self, eng):
        waits = []
        for q in ENGS:
            n = self.ndma[q]
            for d in range(max(0, n - self.NRING), n):
                waits.append(('d', q, d))
        self.ops[eng].append([None, waits, False, None])

    def emit(self, nc, block_cm):
        cnt = {}
        nsem = {}
        for e in ENGS:
            c = 0
            arr = []
            for o in self.ops[e]:
                if o[2]:
                    c += 1
                arr.append(c)
            cnt[e] = arr
            nsem[e] = (c + EPOCH - 1) // EPOCH
        csem = {e: [nc.alloc_semaphore(name=f"c_{e}_{k}") for k in range(nsem[e])] for e in ENGS}
        dsem = {e: [nc.alloc_semaphore(name=f"d_{e}_{k}") for k in range(self.NRING)]
                for e in ENGS if self.ndma[e] > 0}

        def resolve(t):
            if t[0] == 'c':
                _, e, i = t
                c = cnt[e][i]
                return csem[e][(c - 1) // EPOCH], (c - 1) % EPOCH + 1
            _, q, d0 = t
            return dsem[q][d0 % self.NRING], 16 * (d0 // self.NRING + 1)

        prog = self

        def run(e, eng):
            for k, (emit, waits, marked, dtok) in enumerate(prog.ops[e]):
                for t in waits:
                    s, v = resolve(t)
                    eng.wait_ge(s, v)
                if emit is None:
                    continue
                ins = emit(eng)
                if dtok is not None:
                    s, _ = resolve(dtok)
                    ins.then_inc(s, 16)
                elif marked:
                    c = cnt[e][k]
                    ins.then_inc(csem[e][(c - 1) // EPOCH], 1)

        with block_cm as block:
            @block.tensor
            def _(eng):
                run('pe', eng)

            @block.scalar
            def _(eng):
                run('act', eng)

            @block.vector
            def _(eng):
                run('dve', eng)

            @block.gpsimd
            def _(eng):
                run('pool', eng)

            @block.sync
            def _(eng):
                run('sp', eng)


class Arena:
    def __init__(self, ap_u8, nbytes):
        self.ap = ap_u8
        self.nbytes = nbytes

    def carve(self, off, parts, free_shape, dtype, pbase=0):
        esz = 4 if dtype == F32 else 2
        n = 1
        for s in free_shape:
            n *= s
        assert off % 4 == 0 and off + n * esz <= self.nbytes, (off, n * esz, self.nbytes)
        a = self.ap[pbase:pbase + parts, off:off + n * esz].bitcast(dtype)
        if len(free_shape) == 2:
            a = a.rearrange('p (a b) -> p a b', a=free_shape[0])
        elif len(free_shape) == 3:
            a = a.rearrange('p (a b c) -> p a b c', a=free_shape[0], b=free_shape[1])
        return a


OFF_X = 0
OFF_H = OFF_X + 8 * S * 4
OFF_C = OFF_H + 8 * S * 2
OFF_Y = OFF_C + 10240
OFF_A = OFF_Y + 8 * S * 2
SB_BYTES = 212800
ARENA_BYTES = SB_BYTES - OFF_A
NGAIN = 8 * (3 * DEPTH + 1)
NCBF = 2944
NCF32 = 272


class _Stop(Exception):
    pass


class Model:
    def __init__(self, n_seq=2, depth=DEPTH, stage='full'):
        self.n_seq = n_seq
        self.depth = depth
        self.stage = stage
        nc = bass.Bass("TRN2", target_bir_lowering=False)
        self.nc = nc
        dt = nc.dram_tensor
        self.xT = dt("xT", [n_seq, D, S], F32, kind="ExternalInput").ap()
        self.outT = dt("outT", [n_seq, D, S], F32, kind="ExternalOutput").ap()
        self.w_gu = [dt(f"w_ffn{i}_gu", [DEPTH, D, 2 * DFF], F32, kind="ExternalInput").ap() for i in (1, 2)]
        self.w_dn = [dt(f"w_ffn{i}_down", [DEPTH, DFF, D], F32, kind="ExternalInput").ap() for i in (1, 2)]
        self.w_in = dt("w_in", [DEPTH, D, D_IN], F32, kind="ExternalInput").ap()
        self.w_kv_up = dt("w_kv_up", [DEPTH, KVL, 2 * HD], F32, kind="ExternalInput").ap()
        self.w_up_fox = dt("w_up_fox", [DEPTH, W_A, D], F32, kind="ExternalInput").ap()
        self.w_up_sb = dt("w_up_sb", [DEPTH, W_B, D], F32, kind="ExternalInput").ap()
        self.w_up_dsa = dt("w_up_dsa", [DEPTH, W_C, D], F32, kind="ExternalInput").ap()
        self.w_out = dt("w_out", [DEPTH, D, D], F32, kind="ExternalInput").ap()
        self.gains = dt("gains", [128, NGAIN], F32, kind="ExternalInput").ap()
        self.smallp = dt("smallp", [128, 256], F32, kind="ExternalInput").ap()
        self.rope = dt("rope", [128, 2 * S], F32, kind="ExternalInput").ap()
        self.cbf = dt("cbf", [128, NCBF], F32, kind="ExternalInput").ap()
        self.cf32 = dt("cf32", [128, NCF32], F32, kind="ExternalInput").ap()
        self.P = Prog()

    def build(self):
        nc = self.nc
        P = self.P
        with nc.sbuf_tensor("sb", [128, SB_BYTES], U8) as sb:
            self.psum_cms = [nc.psum_tensor(f"ps{k}", [128, 512], F32) for k in range(8)]
            self.ps = [cm.__enter__()[:] for cm in self.psum_cms]
            A = Arena(sb, SB_BYTES)
            self.A = A
            self.X = A.carve(OFF_X, 128, [8, S], F32)
            self.H = A.carve(OFF_H, 128, [8, S], BF16)
            self.Y = A.carve(OFF_Y, 128, [8, S], BF16)
            o = OFF_C
            self.gain_sb = A.carve(o, 128, [NGAIN], F32); o += NGAIN * 4
            self.small_sb = A.carve(o, 128, [256], F32); o += 1024
            self.cbf_sb = A.carve(o, 128, [NCBF], BF16); o += NCBF * 2
            self.cf32_sb = A.carve(o, 128, [NCF32], F32); o += NCF32 * 4
            cb = self.cbf_sb
            self.tri_incl = cb[:, 0:128]
            self.smask = [cb[:, 128 + 512 * k: 128 + 512 * (k + 1)] for k in range(4)]
            self.negtri = cb[:, 2176:2304]
            self.negones = cb[:, 2304:2432]
            self.ident = cb[:, 2432:2560]
            self.ones_mean = cb[:, 2560:2688]
            self.ones_128th = cb[:, 2688:2816]
            self.ones_64th = cb[:, 2816:2944]
            self.tri_f32 = self.cf32_sb[:, 0:128]
            self.ones_f32 = self.cf32_sb[:, 128:256]
            self.pow2 = self.cf32_sb[:, 256:272]
            self.cst = A.carve(o, 128, [16], F32); o += 64
            self.c_off = o
            assert o <= OFF_C + 10240, o
            self.consts()
            for s in range(self.n_seq):
                self.load_x(s)
                try:
                    for l in range(self.depth):
                        self.ffn(l, 0)
                        if self.stage == 'ffn1':
                            break
                        self.mixer(l)
                        self.ffn(l, 1)
                except _Stop:
                    pass
                P.barrier()
                self.final(s)
                P.barrier()
            P.wait_all_dma('sp')
            P.emit(nc, nc.Block())
            for cm in reversed(self.psum_cms):
                cm.__exit__(None, None, None)
        return nc

    def consts(self):
        P = self.P
        P.op('pool', lambda e: e.dma_start(out=self.cbf_sb, in_=self.cbf), writes=['ones_mean'], dma=True)
        P.op('sp', lambda e: e.dma_start(out=self.cf32_sb, in_=self.cf32), writes=['cf32'], dma=True)
        P.op('pool', lambda e: e.memset(self.cst[:, 0:1], EPS), writes=['cst'])
        P.op('pool', lambda e: e.memset(self.cst[:, 1:2], 1.0), writes=['cst'])
        P.op('pool', lambda e: e.memset(self.cst[:, 2:3], 0.0), writes=['cst'])
        g = self.gain_sb
        P.op('sp', lambda e: e.dma_start(out=g, in_=self.gains), writes=['gains'], dma=True)
        sm = self.small_sb
        P.op('sp', lambda e: e.dma_start(out=sm, in_=self.smallp), writes=['smallp'], dma=True)

    def load_x(self, s):
        P = self.P
        for c in range(8):
            src = self.xT[s, c * 128:(c + 1) * 128, :]
            dst = self.X[:, c, :]
            P.op('sp', lambda e, src=src, dst=dst: e.dma_start(out=dst, in_=src),
                 writes=[f'x{c}_{t}' for t in range(NTB)], dma=True)

    def rmsnorm(self, gi, out_fn, tmp_off, t_list=None):
        P = self.P
        A = self.A
        sq = A.carve(tmp_off, 128, [2, 8, TB], BF16)
        rstd = A.carve(tmp_off + 2 * 8 * TB * 2, 128, [2, TB], F32)
        for t in (range(NTB) if t_list is None else t_list):
            k = t % 2
            ts = slice(t * TB, (t + 1) * TB)
            xin = self.X[:, :, ts]
            sqk = sq[:, k]
            P.op('pool', lambda e, xin=xin, sqk=sqk: e.tensor_tensor(out=sqk, in0=xin, in1=xin, op=ALU.mult),
                 reads=[f'x{c}_{t}' for c in range(8)], writes=[f'sq{k}'])
            ps = self.ps[6 + k]
            for c in range(8):
                P.op('pe', lambda e, ps=ps, c=c, sqk=sqk: e.matmul(ps, self.ones_mean, sqk[:, c, :],
                                                                   start=(c == 0), stop=(c == 7)),
                     reads=[f'sq{k}', 'ones_mean'], writes=[f'ps{6 + k}'])
            rk = rstd[:, k]
            P.op('act', lambda e, ps=ps, rk=rk: e.activation(out=rk, in_=ps, func=AF.Ln, bias=self.cst[:, 0:1]),
                 reads=[f'ps{6 + k}', 'cst'], writes=[f'rstd{k}'])
            P.op('act', lambda e, rk=rk: e.activation(out=rk, in_=rk, func=AF.Exp, scale=-0.5),
                 reads=[f'rstd{k}'], writes=[f'rstd{k}'])
            for c in range(8):
                dst, bname = out_fn(c, t)
                xin_c = self.X[:, c, ts]
                gcol = self.gain_sb[:, gi * 8 + c: gi * 8 + c + 1]
                eng = 'dve'
                P.op(eng, lambda e, dst=dst, xin_c=xin_c, gcol=gcol, rk=rk:
                     e.scalar_tensor_tensor(out=dst, in0=xin_c, scalar=gcol, in1=rk, op0=ALU.mult, op1=ALU.mult),
                     reads=[f'x{c}_{t}', f'rstd{k}', 'gains'], writes=[bname])

    def norm_to_H(self, gi, tmp_off):
        self.rmsnorm(gi, lambda c, t: (self.H[:, c, t * TB:(t + 1) * TB], f'h_{t}'), tmp_off)

    def ffn(self, l, which):
        P = self.P
        A = self.A
        gi = l * 3 + (0 if which == 0 else 2)
        o = OFF_A
        actT = A.carve(o, 128, [NFF // 2, S], BF16); o += (NFF // 2) * S * 2
        wgu = [A.carve(o + k * 4096, 128, [8, 2, 128], BF16) for k in range(2)]; o += 8192
        wdn = [A.carve(o + k * 2816, 128, [NFF // 2, 128], BF16) for k in range(2)]; o += 2 * 2816
        sg = [A.carve(o + k * 2048, 128, [TB], F32) for k in range(2)]; o += 4096
        assert o <= SB_BYTES, o
        P.barrier()
        self.norm_to_H(gi, OFF_A)
        P.barrier()
        self.chk(f'f{which}norm')
        w_gu = self.w_gu[which][l].rearrange("(kc p) (two n) -> p kc two n", p=128, two=2)
        w_dn = self.w_dn[which][l].rearrange("(j p) n -> p j n", p=128)
        NH = NFF // 2
        cnt = 0
        for half in range(2):
            for jj in range(NH):
                j = half * NH + jj
                wb = cnt % 2
                dst = wgu[wb]
                for two in range(2):
                    src = w_gu[:, :, two, j * 128:(j + 1) * 128]
                    dd = dst[:, :, two, :]
                    P.op('pool', lambda e, src=src, dd=dd: e.dma_start(out=dd, in_=src),
                         writes=[f'wgu{wb}_{two}'], dma=True)
                for t in range(NTB):
                    pb = (cnt * NTB + t) % 2
                    ts = slice(t * TB, (t + 1) * TB)
                    pg, pu = self.ps[pb], self.ps[2 + pb]
                    for kc in range(8):
                        P.op('pe', lambda e, pg=pg, dst=dst, kc=kc, ts=ts: e.matmul(
                            pg, dst[:, kc, 0, :], self.H[:, kc, ts], start=(kc == 0), stop=(kc == 7)),
                            reads=[f'wgu{wb}_0', f'h_{t}'], writes=[f'ps{pb}'])
                    for kc in range(8):
                        P.op('pe', lambda e, pu=pu, dst=dst, kc=kc, ts=ts: e.matmul(
                            pu, dst[:, kc, 1, :], self.H[:, kc, ts], start=(kc == 0), stop=(kc == 7)),
                            reads=[f'wgu{wb}_1', f'h_{t}'], writes=[f'ps{2 + pb}'])
                    sgk = sg[pb]
                    P.op('act', lambda e, sgk=sgk, pg=pg: e.activation(out=sgk, in_=pg, func=AF.Silu),
                         reads=[f'ps{pb}'], writes=[f'sg{pb}'])
                    adst = actT[:, jj, ts]
                    P.op('dve', lambda e, adst=adst, sgk=sgk, pu=pu: e.tensor_tensor(
                        out=adst, in0=sgk, in1=pu, op=ALU.mult),
                        reads=[f'sg{pb}', f'ps{2 + pb}'], writes=[f'act{jj}_{t}'])
                cnt += 1
            for dc in range(8):
                db = dc % 2
                src = w_dn[:, half * NH:(half + 1) * NH, dc * 128:(dc + 1) * 128]
                dst = wdn[db]
                P.op('pool', lambda e, src=src, dst=dst: e.dma_start(out=dst, in_=src),
                     writes=[f'wdn{db}'], dma=True)
                for t in range(NTB):
                    pb = 4 + (dc * NTB + t) % 2
                    ts = slice(t * TB, (t + 1) * TB)
                    po = self.ps[pb]
                    for jj in range(NH):
                        P.op('pe', lambda e, po=po, dst=dst, jj=jj, ts=ts: e.matmul(
                            po, dst[:, jj, :], actT[:, jj, ts], start=(jj == 0), stop=(jj == NH - 1)),
                            reads=[f'wdn{db}', f'act{jj}_{t}'], writes=[f'ps{pb}'])
                    xd = self.X[:, dc, ts]
                    P.op('dve', lambda e, xd=xd, po=po: e.scalar_tensor_tensor(
                        out=xd, in0=po, scalar=0.5, in1=xd, op0=ALU.mult, op1=ALU.add),
                        reads=[f'ps{pb}', f'x{dc}_{t}'], writes=[f'x{dc}_{t}'])
        self.chk(f'f{which}end')

    def chk(self, name):
        if self.stage.rstrip('XYH') == name:
            raise _Stop()

    def load_w(self, slot, src, ncols, name, dcol=0):
        dst = slot[:, :, dcol:dcol + ncols]
        self.P.op('pool', lambda e, src=src, dst=dst: e.dma_start(out=dst, in_=src), writes=[name], dma=True)

    def proj_T(self, slot, wname, M, evac, banks=(0, 1)):
        P = self.P
        for T in range(NTB):
            b = banks[T % 2]
            ps = self.ps[b]
            ts = slice(T * TB, (T + 1) * TB)
            for kc in range(8):
                P.op('pe', lambda e, ps=ps, kc=kc, ts=ts: e.matmul(ps[0:M, :], slot[:, kc, 0:M], self.H[:, kc, ts],
                                                                  start=(kc == 0), stop=(kc == 7)),
                     reads=[wname, f'h_{T}'], writes=[f'ps{b}'])
            evac(T, ps, f'ps{b}')

    def proj_tok(self, slot, wname, N, evac, banks=(0, 1)):
        P = self.P
        for g in range(4):
            b = banks[g % 2]
            ps = self.ps[b]
            for cc in range(4):
                ch = 4 * g + cc
                for kc in range(8):
                    P.op('pe', lambda e, ps=ps, kc=kc, ch=ch, cc=cc: e.matmul(
                        ps[:, cc * 128:cc * 128 + N], self.H[:, kc, ch * 128:(ch + 1) * 128], slot[:, kc, 0:N],
                        start=(kc == 0), stop=(kc == 7)),
                        reads=[wname, f'h_{ch // 4}'], writes=[f'ps{b}'])
            self.chk('tok_mm')
            evac(g, ps.rearrange("p (a b) -> p a b", a=4), f'ps{b}')
            self.chk('tok_ev')

    def attn_finish(self, O, oname, hh, ychunk, T, normalize, rden, bcs, width=TB):
        P = self.P
        pb = 64 * hh
        ts = slice(T * width, (T + 1) * width)
        ydst = self.Y[pb:pb + 64, ychunk, ts]
        yname = f'y{ychunk}_{hh}_{T}_{width}'
        O = O[:, 0:width]
        rden = rden[:, 0:width]
        bcs = bcs[:, 0:width]
        if not normalize:
            P.op('act', lambda e: e.copy(out=ydst, in_=O[pb:pb + 64, :]), reads=[oname], writes=[yname])
            return
        p = 64 if hh == 0 else 0
        P.op('dve', lambda e: e.reciprocal(out=rden[p:p + 1, :], in_=O[p:p + 1, :]), reads=[oname], writes=['rden'])
        BC = self.ps[6][:, 0:width]
        P.op('pe', lambda e: e.matmul(BC, self.ones_f32[p:p + 1, :], rden[p:p + 1, :], start=True, stop=True),
             reads=['rden'], writes=['ps6'])
        P.op('act', lambda e: e.copy(out=bcs[pb:pb + 64, :], in_=BC[pb:pb + 64, :]), reads=['ps6'], writes=['bcs'])
        P.op('dve', lambda e: e.tensor_tensor(out=ydst, in0=O[pb:pb + 64, :], in1=bcs[pb:pb + 64, :], op=ALU.mult),
             reads=[oname, 'bcs'], writes=[yname])

    def mixer(self, l):
        P = self.P
        A = self.A
        P.barrier()
        self.norm_to_H(l * 3 + 1, OFF_A)
        P.barrier()
        self.chk('norm')
        w_in = self.w_in[l].rearrange("(kc p) n -> p kc n", p=128)
        o = [OFF_A]

        def take(n):
            r = o[0]
            o[0] += n
            assert o[0] <= SB_BYTES, o[0]
            return r
        WSL = [A.carve(take(2048), 128, [8, 128], BF16) for _ in range(6)]
        qT = A.carve(take(4096), 128, [S], BF16)
        kT = A.carve(take(4096), 128, [S], BF16)
        Vp = A.carve(take(16 * 192 * 2), 128, [16, 192], BF16)
        Pt = [A.carve(take(1024), 128, [512], BF16) for _ in range(4)]
        rden = A.carve(take(2048), 128, [512], F32)
        bcs = A.carve(take(2048), 128, [512], F32)
        base_common = o[0]
        self.cnt = 0
        self.ocnt = 0

        P.op('pool', lambda e: e.memset(Vp[:, :, 64:65], 1.0), writes=['Vp_c'])
        P.op('pool', lambda e: e.memset(Vp[:, :, 65:128], 0.0), writes=['Vp_c'])

        def proj_qkv(qcol, kcol, vcol, nc_, wi):
            s0, s1, s2 = WSL[wi], WSL[wi + 1], WSL[wi + 2]
            self.load_w(s0, w_in[:, :, qcol:qcol + nc_], nc_, f'wsl{wi}')
            self.load_w(s1, w_in[:, :, kcol:kcol + nc_], nc_, f'wsl{wi + 1}')
            self.load_w(s2, w_in[:, :, vcol:vcol + nc_], nc_, f'wsl{wi + 2}')
            self.proj_T(s0, f'wsl{wi}', nc_, lambda T, ps, pn: P.op(
                'dve', lambda e: e.tensor_scalar(out=qT[0:nc_, T * TB:(T + 1) * TB], in0=ps[0:nc_, :], scalar1=0.125,
                                                 scalar2=None, op0=ALU.mult),
                reads=[pn], writes=[f'qT_{T}']))
            self.chk('projq')
            self.proj_T(s1, f'wsl{wi + 1}', nc_, lambda T, ps, pn: P.op(
                'dve', lambda e: e.tensor_copy(out=kT[0:nc_, T * TB:(T + 1) * TB], in_=ps[0:nc_, :]),
                reads=[pn], writes=[f'kT_{T}']))
            self.chk('projk')

            import os
            VAR = os.environ.get('EVVAR', 'ab')

            def ev(g, ps3, pn):
                if 'a' in VAR:
                  P.op('dve', lambda e: e.tensor_copy(out=Vp[:, 4 * g:4 * g + 4, 0:64], in_=ps3[:, :, 0:64]),
                     reads=[pn], writes=[f'Vp_{g}a'])
                if nc_ > 64 and 'b' in VAR:
                    P.op('dve', lambda e: e.tensor_copy(out=Vp[:, 4 * g:4 * g + 4, 128:192], in_=ps3[:, :, 64:128]),
                         reads=[pn], writes=[f'Vp_{g}b'])
            self.proj_tok(s2, f'wsl{wi + 2}', nc_, ev)
            self.chk('proj')

        ls = A.carve(take(384), 128, [16, 6], F32)
        tot = A.carve(take(384), 128, [16, 6], F32)
        pre = A.carve(take(17 * 24), 128, [17, 6], F32)
        cpos = A.carve(take(384), 128, [16, 6], F32)
        Btab = A.carve(take(6 * 256 * 4), 128, [6, 16, 16], F32)
        base_fox = o[0]
        self.load_w(WSL[5], w_in[:, :, O_FA:O_FA + 6], 6, 'wsl5')
        ps7 = self.ps[7]
        for ch in range(16):
            for kc in range(8):
                P.op('pe', lambda e, ch=ch, kc=kc: e.matmul(ps7[:, ch * 6:(ch + 1) * 6],
                                                              self.H[:, kc, ch * 128:(ch + 1) * 128],
                                                              WSL[5][:, kc, 0:6], start=(kc == 0), stop=(kc == 7)),
                     reads=['wsl5', f'h_{ch // 4}'], writes=['ps7'])
        lsf = ls.rearrange("p a b -> p (a b)")
        P.op('dve', lambda e: e.tensor_tensor(out=lsf, in0=ps7[:, 0:96], in1=self.small_sb[:, l * 96:(l + 1) * 96],
                                              op=ALU.add), reads=['ps7'], writes=['ls'])
        P.op('act', lambda e: e.activation(out=lsf, in_=lsf, func=AF.Exp, scale=-1.0), reads=['ls'], writes=['ls'])
        P.op('act', lambda e: e.activation(out=lsf, in_=lsf, func=AF.Ln, bias=self.cst[:, 1:2]),
             reads=['ls'], writes=['ls'])
        ps6 = self.ps[6]
        P.op('pe', lambda e: e.matmul(ps6[:, 0:96], self.tri_f32, lsf, start=True, stop=True),
             reads=['ls'], writes=['ps6'])
        P.op('pe', lambda e: e.matmul(ps6[:, 128:224], self.ones_f32, lsf, start=True, stop=True),
             reads=['ls'], writes=['ps6'])
        P.op('dve', lambda e: e.tensor_copy(out=tot.rearrange("p a b -> p (a b)"), in_=ps6[:, 128:224]),
             reads=['ps6'], writes=['tot'])
        P.op('dve', lambda e: e.memset(pre[:, 0, :], 0.0), writes=['pre'])
        for ch in range(1, 17):
            P.op('dve', lambda e, ch=ch: e.tensor_tensor(out=pre[:, ch, :], in0=pre[:, ch - 1, :],
                                                         in1=tot[:, ch - 1, :], op=ALU.add),
                 reads=['tot', 'pre'], writes=['pre'])
        P.op('dve', lambda e: e.tensor_tensor(out=cpos.rearrange("p a b -> p (a b)"), in0=ps6[:, 0:96],
                                              in1=pre[:, 0:16, :].rearrange("p a b -> p (a b)"), op=ALU.add),
             reads=['ps6', 'pre'], writes=['cpos'])
        for h in range(6):
            for tb in range(16):
                P.op('dve', lambda e, h=h, tb=tb: e.tensor_scalar(
                    out=Btab[:, h, tb, :], in0=cpos[:, :, h], scalar1=pre[:, tb + 1, h:h + 1], scalar2=None,
                    op0=ALU.subtract), reads=['cpos', 'pre'], writes=['Btab'])

        self.chk('pre')
        def softmax_attn(hh, ychunk, bias_fn, mask_fn):
            pb = 64 * hh
            for T in range(NTB):
                nsc = 4 * T + 4
                ob = 4 + (self.ocnt % 2)
                self.ocnt += 1
                O = self.ps[ob]
                tinfo = {}

                def S1(sc):
                    zb = 2 + (self.cnt % 2)
                    k = self.cnt % 4
                    self.cnt += 1
                    tinfo[sc] = k
                    Z = self.ps[zb]
                    P.op('pe', lambda e, Z=Z, sc=sc, T=T: e.matmul(
                        Z, kT[pb:pb + 64, sc * 128:(sc + 1) * 128], qT[pb:pb + 64, T * TB:(T + 1) * TB],
                        start=True, stop=True), reads=[f'kT_{sc // 4}', f'qT_{T}'], writes=[f'ps{zb}'])
                    Ptk = Pt[k]
                    for tl in range(4):
                        tb = 4 * T + tl
                        cs = slice(tl * 128, (tl + 1) * 128)
                        pn = f'Pt{k}_{tl}'
                        if tb < sc:
                            P.op('pool', lambda e, Ptk=Ptk, cs=cs: e.memset(Ptk[:, cs], 0.0), writes=[pn])
                            continue
                        bias = bias_fn(sc, tb)
                        P.op('act', lambda e, Ptk=Ptk, cs=cs, Z=Z, bias=bias: e.activation(
                            out=Ptk[:, cs], in_=Z[:, cs], func=AF.Exp, bias=bias),
                            reads=[f'ps{zb}', 'Btab'], writes=[pn])
                        mask_fn(sc, tb, Ptk[:, cs], pn)

                def S2(sc):
                    k = tinfo[sc]
                    Ptk = Pt[k]
                    vs = slice(0, 65) if hh == 0 else slice(64, 192)
                    M = 65 if hh == 0 else 128
                    P.op('pe', lambda e, O=O, sc=sc, Ptk=Ptk, vs=vs, M=M, nsc=nsc: e.matmul(
                        O[0:M, :], Vp[:, sc, vs], Ptk, start=(sc == 0), stop=(sc == nsc - 1)),
                        reads=[f'Vp_{sc // 4}a', f'Vp_{sc // 4}b', 'Vp_c'] + [f'Pt{k}_{tl}' for tl in range(4)], writes=[f'ps{ob}'])

                S1(0)
                for sc in range(nsc):
                    if sc + 1 < nsc:
                        S1(sc + 1)
                    S2(sc)
                    if sc == 0:
                        self.chk('fox_sc0')
                self.chk('fox_T0n')
                self.attn_finish(O, f'ps{ob}', hh, ychunk, T, True, rden, bcs)
                self.chk('fox_T0')

        def fox_mask(sc, tb, ap, pn):
            if sc == tb:
                P.op('pool', lambda e: e.tensor_tensor(out=ap, in0=ap, in1=self.tri_incl, op=ALU.mult),
                     reads=[pn], writes=[pn])

        if True:
            for hp in range(3):
                proj_qkv(O_QA + hp * 128, O_KA + hp * 128, O_VA + hp * 128, 128, 3 * (hp % 2))
                for hh in range(2):
                    h = 2 * hp + hh
                    softmax_attn(hh, hp, lambda sc, tb, h=h: Btab[:, h, tb, sc:sc + 1], fox_mask)
        P.barrier()

        o[0] = base_common
        e32 = A.carve(take(2048), 128, [512], F32)
        spb = [A.carve(take(1024), 128, [512], BF16) for _ in range(2)]
        Ab = [A.carve(take(1024), 128, [512], BF16) for _ in range(2)]
        R = A.carve(take(2048), 128, [512], F32)
        Rb = [A.carve(take(1024), 128, [512], BF16) for _ in range(2)]

        Rb3 = Rb + [A.carve(take(1024), 128, [512], BF16)]

        def sb_attn(hh, ychunk):
            pb = 64 * hh
            for T in range(NTB):
                nsc = 4 * T + 4
                ob = 6 + (T % 2)
                O = self.ps[ob]
                order = list(reversed(range(nsc)))
                info = {}

                def S1(i):
                    sc = order[i]
                    c2 = self.cnt % 2
                    self.cnt += 1
                    info[i] = c2
                    zb = 2 + c2
                    Z = self.ps[zb]
                    kk = kT[pb:pb + 64, sc * 128:(sc + 1) * 128]
                    qq = qT[pb:pb + 64, T * TB:(T + 1) * TB]
                    P.op('pe', lambda e, Z=Z, kk=kk, qq=qq: e.matmul(Z, kk, qq, start=True, stop=True),
                         reads=[f'kT_{sc // 4}', f'qT_{T}'], writes=[f'ps{zb}'])
                    P.op('act', lambda e, Z=Z: e.activation(out=e32, in_=Z, func=AF.Exp),
                         reads=[f'ps{zb}'], writes=['e32'])
                    sp = spb[c2]
                    P.op('act', lambda e, sp=sp: e.activation(out=sp, in_=e32, func=AF.Ln, bias=self.cst[:, 1:2]),
                         reads=['e32'], writes=[f'spb{c2}'])
                    if sc >= 4 * T:
                        m = self.smask[sc - 4 * T]
                        P.op('pool', lambda e, sp=sp, m=m: e.tensor_tensor(out=sp, in0=sp, in1=m, op=ALU.mult),
                             reads=[f'spb{c2}'], writes=[f'spb{c2}'])
                    if sc > 0:
                        if i == 0:
                            P.op('dve', lambda e, sp=sp: e.tensor_copy(out=R, in_=sp), reads=[f'spb{c2}'], writes=['R'])
                        else:
                            P.op('dve', lambda e, sp=sp: e.tensor_tensor(out=R, in0=R, in1=sp, op=ALU.add),
                                 reads=[f'spb{c2}', 'R'], writes=['R'])
                        rb = Rb3[i % 3]
                        P.op('dve', lambda e, rb=rb: e.tensor_copy(out=rb, in_=R), reads=['R'],
                             writes=[f'Rb{i % 3}'])

                def S2(i):
                    sc = order[i]
                    c2 = info[i]
                    first = (i == 0)
                    lb = 4 + c2
                    L = self.ps[lb]
                    kk = kT[pb:pb + 64, sc * 128:(sc + 1) * 128]
                    qq = qT[pb:pb + 64, T * TB:(T + 1) * TB]
                    sp = spb[c2]
                    P.op('pe', lambda e, L=L, kk=kk, qq=qq: e.matmul(L, kk, qq, start=True, stop=False),
                         reads=[f'kT_{sc // 4}', f'qT_{T}'], writes=[f'ps{lb}'])
                    P.op('pe', lambda e, L=L, sp=sp, first=first: e.matmul(L, self.negtri, sp, start=False, stop=first),
                         reads=[f'spb{c2}'], writes=[f'ps{lb}'])
                    if not first:
                        rb = Rb3[(i - 1) % 3]
                        P.op('pe', lambda e, L=L, rb=rb: e.matmul(L, self.negones, rb, start=False, stop=True),
                             reads=[f'Rb{(i - 1) % 3}'], writes=[f'ps{lb}'])
                    ab = Ab[c2]
                    P.op('act', lambda e, ab=ab, L=L: e.activation(out=ab, in_=L, func=AF.Exp),
                         reads=[f'ps{lb}'], writes=[f'Ab{c2}'])
                    if sc >= 4 * T:
                        m = self.smask[sc - 4 * T]
                        P.op('pool', lambda e, ab=ab, m=m: e.tensor_tensor(out=ab, in0=ab, in1=m, op=ALU.mult),
                             reads=[f'Ab{c2}'], writes=[f'Ab{c2}'])
                    vs = slice(0, 64) if hh == 0 else slice(64, 192)
                    M = 64 if hh == 0 else 128
                    P.op('pe', lambda e, O=O, sc=sc, ab=ab, vs=vs, M=M, first=first: e.matmul(
                        O[0:M, :], Vp[:, sc, vs], ab, start=first, stop=(sc == 0)),
                        reads=[f'Vp_{sc // 4}a', f'Vp_{sc // 4}b', 'Vp_c', f'Ab{c2}'], writes=[f'ps{ob}'])

                S1(0)
                for i in range(nsc):
                    if i + 1 < nsc:
                        S1(i + 1)
                    S2(i)
                self.attn_finish(O, f'ps{ob}', hh, ychunk, T, False, rden, bcs)

        self.chk('fox')
        if True:
            for hp in range(3):
                nc_ = 128 if hp < 2 else 64
                proj_qkv(O_QB + hp * 128, O_KB + hp * 128, O_VB + hp * 128, nc_, 3 * (hp % 2))
                for hh in range(2 if hp < 2 else 1):
                    sb_attn(hh, 3 + hp)
        P.barrier()
        self.chk('sb')
        self.dsa(l, w_in)
        P.barrier()
        self.chk('dsa')
        self.merge(l, w_in)
        P.barrier()
        self.chk('merge')

    def dsa(self, l, w_in):
        P = self.P
        A = self.A
        o = [OFF_A]

        def take(n):
            r = o[0]
            o[0] += n
            assert o[0] <= SB_BYTES, o[0]
            return r
        qc = [A.carve(take(4096), 128, [S], BF16) for _ in range(3)]
        kcT = A.carve(take(4096), 128, [S], BF16)
        Vc = A.carve(take(16 * 192 * 2), 128, [16, 192], BF16)
        iq = [A.carve(take(4096), 128, [S], BF16) for _ in range(2)]
        ikT = A.carve(take(4096), 128, [S], BF16)
        wI = A.carve(take(256), 128, [16, 4], F32)
        base_d2 = o[0]
        WSL = [A.carve(take(2048), 128, [8, 128], BF16) for _ in range(4)]
        ROPE = A.carve(take(16384), 128, [2, S], F32)
        ckvn = A.carve(take(4096), 128, [S], BF16)
        t32 = [A.carve(take(2048), 128, [512], F32) for _ in range(3)]
        sqb = A.carve(take(1024), 128, [512], BF16)
        COS, SIN = ROPE[:, 0, :], ROPE[:, 1, :]
        P.op('sp', lambda e: e.dma_start(out=ROPE.rearrange("p a b -> p (a b)"), in_=self.rope),
             writes=['rope'], dma=True)
        P.op('pool', lambda e: e.memset(Vc[:, :, 64:65], 1.0), writes=['Vc_c'])
        P.op('pool', lambda e: e.memset(Vc[:, :, 65:128], 0.0), writes=['Vc_c'])
        gkv = self.small_sb[:, 192 + l:193 + l]
        gidx = self.small_sb[:, 194 + l:195 + l]
        gidx_sw = self.small_sb[:, 196 + l:197 + l]

        def rstd_of(src32, T):
            P.op('pool', lambda e: e.tensor_tensor(out=sqb, in0=src32, in1=src32, op=ALU.mult),
                 reads=['t32_0'], writes=['sqb'])
            P.op('pe', lambda e: e.matmul(self.ps[6], self.ones_128th, sqb, start=True, stop=True),
                 reads=['sqb'], writes=['ps6'])
            P.op('act', lambda e: e.activation(out=t32[1], in_=self.ps[6], func=AF.Ln, bias=self.cst[:, 0:1]),
                 reads=['ps6'], writes=['t32_1'])
            P.op('act', lambda e: e.activation(out=t32[1], in_=t32[1], func=AF.Exp, scale=-0.5),
                 reads=['t32_1'], writes=['t32_1'])

        self.load_w(WSL[0], w_in[:, :, O_CKV:O_CKV + 128], 128, 'dw0')

        def ev_ckv(T, ps, pn):
            ts = slice(T * TB, (T + 1) * TB)
            P.op('act', lambda e: e.copy(out=t32[0], in_=ps), reads=[pn], writes=['t32_0'])
            rstd_of(t32[0], T)
            P.op('dve', lambda e: e.scalar_tensor_tensor(out=ckvn[:, ts], in0=t32[0], scalar=gkv, in1=t32[1],
                                                         op0=ALU.mult, op1=ALU.mult),
                 reads=['t32_0', 't32_1'], writes=[f'ckvn_{T}'])
        self.proj_T(WSL[0], 'dw0', 128, ev_ckv)

        wkv = WSL[1]
        src_kv = self.w_kv_up[l]
        P.op('pool', lambda e: e.dma_start(out=wkv[:, 0, :], in_=src_kv), writes=['dw1a'], dma=True)
        P.op('pool', lambda e: e.dma_start(out=wkv[:, 2, 0:64], in_=src_kv[:, 0:64]), writes=['dw1b'], dma=True)
        P.op('pool', lambda e: e.dma_start(out=wkv[:, 2, 64:128], in_=src_kv[:, 0:64]), writes=['dw1c'], dma=True)

        def make_swapped(dst, src, names):
            d4 = dst.rearrange("p k (h d) -> p k h d", h=2)
            s4 = src.rearrange("p k (h d) -> p k h d", h=2)
            P.op('pool', lambda e: e.tensor_copy(out=dst, in_=src), reads=names, writes=['swp'])
            P.op('pool', lambda e: e.tensor_copy(out=d4[:, :, :, 0:8], in_=s4[:, :, :, 8:16]), reads=names, writes=['swp'])
            P.op('pool', lambda e: e.tensor_copy(out=d4[:, :, :, 8:16], in_=s4[:, :, :, 0:8]), reads=names, writes=['swp'])
        make_swapped(wkv[:, 3:4, :], wkv[:, 2:3, :], ['dw1b', 'dw1c'])

        def rope_combine(T, psn, pss, names, dst, dname, pre_n=None, pre_s=None):
            ts = slice(T * TB, (T + 1) * TB)
            P.op('dve', lambda e: e.tensor_tensor(out=t32[0], in0=psn, in1=COS[:, ts], op=ALU.mult),
                 reads=[names[0], 'rope'], writes=['t32_0'])
            P.op('dve', lambda e: e.tensor_tensor(out=t32[2], in0=pss, in1=SIN[:, ts], op=ALU.mult),
                 reads=[names[1], 'rope'], writes=['t32_2'])
            P.op('pool', lambda e: e.tensor_tensor(out=dst[:, ts], in0=t32[0], in1=t32[2], op=ALU.add),
                 reads=['t32_0', 't32_2'], writes=[dname])

        for T in range(NTB):
            ts = slice(T * TB, (T + 1) * TB)
            P.op('pe', lambda e, ts=ts: e.matmul(self.ps[0], wkv[:, 2, :], ckvn[:, ts], start=True, stop=True),
                 reads=['dw1b', 'dw1c', f'ckvn_{T}'], writes=['ps0'])
            P.op('pe', lambda e, ts=ts: e.matmul(self.ps[1], wkv[:, 3, :], ckvn[:, ts], start=True, stop=True),
                 reads=['swp', f'ckvn_{T}'], writes=['ps1'])
            rope_combine(T, self.ps[0], self.ps[1], ['ps0', 'ps1'], kcT, 'kcT')
        for g in range(4):
            b = 2 + g % 2
            ps = self.ps[b]
            for cc in range(4):
                ch = 4 * g + cc
                P.op('pe', lambda e, ps=ps, cc=cc, ch=ch: e.matmul(ps[:, cc * 128:cc * 128 + 64],
                                                                  ckvn[:, ch * 128:(ch + 1) * 128], wkv[:, 0, 64:128],
                                                                  start=True, stop=True),
                     reads=['dw1a', f'ckvn_{g}'], writes=[f'ps{b}'])
            ps3 = ps.rearrange("p (a b) -> p a b", a=4)
            P.op('dve', lambda e, ps3=ps3, g=g: e.tensor_copy(out=Vc[:, 4 * g:4 * g + 4, 0:64], in_=ps3[:, :, 0:64]),
                 reads=[f'ps{b}'], writes=['Vc_a'])
            P.op('dve', lambda e, ps3=ps3, g=g: e.tensor_copy(out=Vc[:, 4 * g:4 * g + 4, 128:192], in_=ps3[:, :, 0:64]),
                 reads=[f'ps{b}'], writes=['Vc_b'])

        def rope_pair(col_lo, col_hi, dst, dname):
            wn, ws = WSL[2], WSL[3]
            if col_hi == col_lo + 64:
                self.load_w(wn, w_in[:, :, col_lo:col_lo + 128], 128, 'dw2a')
                nm = ['dw2a']
            else:
                self.load_w(wn, w_in[:, :, col_lo:col_lo + 64], 64, 'dw2a')
                self.load_w(wn, w_in[:, :, col_hi:col_hi + 64], 64, 'dw2b', dcol=64)
                nm = ['dw2a', 'dw2b']
            make_swapped(ws, wn, nm)
            for T in range(NTB):
                ts = slice(T * TB, (T + 1) * TB)
                for kc in range(8):
                    P.op('pe', lambda e, kc=kc, ts=ts: e.matmul(self.ps[0], wn[:, kc, :], self.H[:, kc, ts],
                                                                start=(kc == 0), stop=(kc == 7)),
                         reads=nm + [f'h_{T}'], writes=['ps0'])
                for kc in range(8):
                    P.op('pe', lambda e, kc=kc, ts=ts: e.matmul(self.ps[1], ws[:, kc, :], self.H[:, kc, ts],
                                                                start=(kc == 0), stop=(kc == 7)),
                         reads=['swp', f'h_{T}'], writes=['ps1'])
                rope_combine(T, self.ps[0], self.ps[1], ['ps0', 'ps1'], dst, dname)

        rope_pair(O_QC, O_QC, qc[0], 'qc0')
        rope_pair(O_QC + 64, O_QC + 128, qc[1], 'qc1')
        rope_pair(O_QC + 192, O_QC + 256, qc[2], 'qc2')
        rope_pair(O_QI, O_QI + 64, iq[0], 'iq0')
        rope_pair(O_QI + 128, O_QI + 192, iq[1], 'iq1')

        wn, ws = WSL[2], WSL[3]
        self.load_w(wn, w_in[:, :, O_KI:O_KI + 64], 64, 'dw2a')
        self.load_w(wn, w_in[:, :, O_KI:O_KI + 64], 64, 'dw2b', dcol=64)
        make_swapped(ws, wn, ['dw2a', 'dw2b'])
        for T in range(NTB):
            ts = slice(T * TB, (T + 1) * TB)
            for kc in range(8):
                P.op('pe', lambda e, kc=kc, ts=ts: e.matmul(self.ps[0], wn[:, kc, :], self.H[:, kc, ts],
                                                            start=(kc == 0), stop=(kc == 7)),
                     reads=['dw2a', 'dw2b', f'h_{T}'], writes=['ps0'])
            for kc in range(8):
                P.op('pe', lambda e, kc=kc, ts=ts: e.matmul(self.ps[1], ws[:, kc, :], self.H[:, kc, ts],
                                                            start=(kc == 0), stop=(kc == 7)),
                     reads=['swp', f'h_{T}'], writes=['ps1'])
            P.op('act', lambda e: e.copy(out=t32[0], in_=self.ps[0]), reads=['ps0'], writes=['t32_0'])
            rstd_of(t32[0], T)
            P.op('dve', lambda e: e.scalar_tensor_tensor(out=t32[0], in0=t32[0], scalar=gidx, in1=t32[1],
                                                         op0=ALU.mult, op1=ALU.mult),
                 reads=['t32_0', 't32_1'], writes=['t32_0'])
            P.op('dve', lambda e: e.scalar_tensor_tensor(out=t32[2], in0=self.ps[1], scalar=gidx_sw, in1=t32[1],
                                                         op0=ALU.mult, op1=ALU.mult),
                 reads=['ps1', 't32_1'], writes=['t32_2'])
            P.op('dve', lambda e, ts=ts: e.tensor_tensor(out=t32[0], in0=t32[0], in1=COS[:, ts], op=ALU.mult),
                 reads=['t32_0', 'rope'], writes=['t32_0'])
            P.op('dve', lambda e, ts=ts: e.tensor_tensor(out=t32[2], in0=t32[2], in1=SIN[:, ts], op=ALU.mult),
                 reads=['t32_2', 'rope'], writes=['t32_2'])
            P.op('pool', lambda e, ts=ts: e.tensor_tensor(out=ikT[:, ts], in0=t32[0], in1=t32[2], op=ALU.add),
                 reads=['t32_0', 't32_2'], writes=['ikT'])

        self.load_w(WSL[0], w_in[:, :, O_WI:O_WI + 4], 4, 'dw0')
        self.proj_tok(WSL[0], 'dw0', 4, lambda g, ps3, pn: P.op(
            'dve', lambda e: e.tensor_copy(out=wI[:, 4 * g:4 * g + 4, :], in_=ps3[:, :, 0:4]),
            reads=[pn], writes=['wI']), banks=(2, 3))
        P.barrier()

        o[0] = base_d2
        sc32 = A.carve(take(8192), 128, [S], F32)
        mask = A.carve(take(4096), 128, [S], BF16)
        maskT = A.carve(take(4096), 128, [16, 128], BF16)
        Pt = [A.carve(take(256), 128, [128], BF16) for _ in range(4)]
        tt = [A.carve(take(2048), 128, [512], F32) for _ in range(2)]
        sm = A.carve(take(256), 128, [64], F32)
        mx, mn, d0, mid, cntc, tmp1, theta = [sm[:, i:i + 1] for i in range(7)]
        halfs = sm[:, 16:32]
        ps7b = self.ps[7].bitcast(BF16)
        maskT2 = [maskT, A.carve(take(4096), 128, [16, 128], BF16)]
        fin = [[A.carve(take(512), 128, [128], F32) for _ in range(3)] for _ in range(6)]
        st = {'cnt': 0, 'ocnt': 0, 'icnt': 0}

        def A1(tb):
            L = (tb + 1) * 128
            qs = slice(tb * 128, (tb + 1) * 128)
            for j in range((L + 511) // 512):
                w = min(512, L - 512 * j)
                cs = slice(512 * j, 512 * j + w)
                for h in range(NIH):
                    pb = 64 * (h % 2)
                    zb = st['icnt'] % 2
                    k2 = st['icnt'] % 2
                    st['icnt'] += 1
                    Z = self.ps[zb]
                    P.op('pe', lambda e, Z=Z, h=h, pb=pb, cs=cs, w=w, qs=qs: e.matmul(
                        Z[:, 0:w], iq[h // 2][pb:pb + 64, qs], ikT[pb:pb + 64, cs], start=True, stop=True),
                        reads=[f'iq{h // 2}', 'ikT'], writes=[f'ps{zb}'])
                    if h == 0:
                        P.op('dve', lambda e, Z=Z, cs=cs, w=w, tb=tb: e.tensor_scalar(
                            out=sc32[:, cs], in0=Z[:, 0:w], scalar1=0.0, scalar2=wI[:, tb, 0:1],
                            op0=ALU.max, op1=ALU.mult), reads=[f'ps{zb}', 'wI'], writes=[f'sc_{j}'])
                    else:
                        P.op('dve', lambda e, Z=Z, w=w, tb=tb, h=h, k2=k2: e.tensor_scalar(
                            out=tt[k2][:, 0:w], in0=Z[:, 0:w], scalar1=0.0, scalar2=wI[:, tb, h:h + 1],
                            op0=ALU.max, op1=ALU.mult), reads=[f'ps{zb}', 'wI'], writes=[f'tt{k2}'])
                        P.op('pool', lambda e, cs=cs, w=w, k2=k2: e.tensor_tensor(
                            out=sc32[:, cs], in0=sc32[:, cs], in1=tt[k2][:, 0:w], op=ALU.add),
                            reads=[f'tt{k2}', f'sc_{j}'], writes=[f'sc_{j}'])
            scn = [f'sc_{j}' for j in range((L + 511) // 512)]
            if tb >= 2:
                P.op('dve', lambda e, L=L: e.tensor_reduce(out=mx, in_=sc32[:, 0:L], axis=mybir.AxisListType.X,
                                                           op=ALU.max), reads=scn, writes=['mx'])
                P.op('dve', lambda e, L=L: e.tensor_reduce(out=mn, in_=sc32[:, 0:L], axis=mybir.AxisListType.X,
                                                           op=ALU.min), reads=scn, writes=['mn'])
            P.op('pool', lambda e, L=L: e.memset(sc32[0:64, L - 64:L], -1e30), reads=scn + ['mx', 'mn'],
                 writes=[scn[-1]])
            return scn

        def Abis(tb, scn):
            L = (tb + 1) * 128
            if tb >= 2:
                P.op('dve', lambda e: e.tensor_tensor(out=d0, in0=mx, in1=mn, op=ALU.subtract),
                     reads=['mx', 'mn'], writes=['d0'])
                P.op('dve', lambda e: e.tensor_scalar(out=halfs, in0=self.pow2, scalar1=d0, scalar2=None,
                                                      op0=ALU.mult), reads=['d0'], writes=['halfs'])
                P.op('dve', lambda e: e.tensor_tensor(out=mid, in0=mn, in1=halfs[:, 0:1], op=ALU.add),
                     reads=['mn', 'halfs'], writes=['mid'])
                for k in range(16):
                    P.op('dve', lambda e, L=L: e.tensor_scalar(out=mask[:, 0:L], in0=sc32[:, 0:L], scalar1=mid,
                                                               scalar2=None, op0=ALU.is_ge, op1=ALU.add,
                                                               accum_out=cntc),
                         reads=scn + ['mid'], writes=['mask', 'cntc'])
                    P.op('dve', lambda e: e.tensor_scalar(out=tmp1, in0=cntc, scalar1=256.0, scalar2=0.5,
                                                          op0=ALU.is_ge, op1=ALU.subtract),
                         reads=['cntc'], writes=['tmp1'])
                    P.op('dve', lambda e, k=k: e.scalar_tensor_tensor(out=mid, in0=tmp1, scalar=halfs[:, k:k + 1],
                                                                      in1=mid, op0=ALU.mult, op1=ALU.add),
                         reads=['tmp1', 'halfs', 'mid'], writes=['mid'])
                P.op('dve', lambda e: e.scalar_tensor_tensor(out=theta, in0=halfs[:, 15:16], scalar=-0.5, in1=mid,
                                                             op0=ALU.mult, op1=ALU.add),
                     reads=['mid', 'halfs'], writes=['theta'])
            else:
                P.op('dve', lambda e: e.memset(theta, -1e29), writes=['theta'])

        def A2(tb, scn):
            L = (tb + 1) * 128
            mT = maskT2[tb % 2]
            P.op('dve', lambda e, L=L: e.tensor_scalar(out=mask[:, 0:L], in0=sc32[:, 0:L], scalar1=theta,
                                                       scalar2=None, op0=ALU.is_ge),
                 reads=scn + ['theta'], writes=['mask'])
            for g0 in range(0, tb + 1, 8):
                n = min(8, tb + 1 - g0)
                for i in range(n):
                    sc = g0 + i
                    P.op('pe', lambda e, i=i, sc=sc: e.transpose(ps7b[:, i * 128:(i + 1) * 128],
                                                                 mask[:, sc * 128:(sc + 1) * 128], self.ident),
                         reads=['mask'], writes=['ps7'])
                P.op('act', lambda e, g0=g0, n=n, mT=mT: e.copy(out=mT[:, g0:g0 + n, :].rearrange("p a b -> p (a b)"),
                                                                in_=ps7b[:, 0:n * 128]),
                     reads=['ps7'], writes=[f'maskT{tb % 2}'])

        def Battn(tb):
            qs = slice(tb * 128, (tb + 1) * 128)
            mT = maskT2[tb % 2]
            for hc in range(NH_C):
                i = (hc + 1) // 2
                hh = (hc + 1) % 2
                pb = 64 * hh
                ob = 4 + st['ocnt'] % 2
                fb = st['ocnt'] % 6
                st['ocnt'] += 1
                O = self.ps[ob]
                dinfo = {}

                def D1(sc):
                    zb = 2 + st['cnt'] % 2
                    k = st['cnt'] % 4
                    st['cnt'] += 1
                    dinfo[sc] = k
                    Z = self.ps[zb]
                    P.op('pe', lambda e, Z=Z, sc=sc, i=i, pb=pb, qs=qs: e.matmul(
                        Z[:, 0:128], kcT[pb:pb + 64, sc * 128:(sc + 1) * 128], qc[i][pb:pb + 64, qs],
                        start=True, stop=True), reads=['kcT', f'qc{i}'], writes=[f'ps{zb}'])
                    Ptk = Pt[k]
                    P.op('act', lambda e, Ptk=Ptk, Z=Z: e.activation(out=Ptk, in_=Z[:, 0:128], func=AF.Exp, scale=0.125),
                         reads=[f'ps{zb}'], writes=[f'dPt{k}'])
                    P.op('pool', lambda e, Ptk=Ptk, sc=sc, mT=mT: e.tensor_tensor(out=Ptk, in0=Ptk, in1=mT[:, sc, :],
                                                                                  op=ALU.mult),
                         reads=[f'dPt{k}', f'maskT{tb % 2}'], writes=[f'dPt{k}'])

                def D2(sc):
                    k = dinfo[sc]
                    Ptk = Pt[k]
                    vs = slice(0, 65) if hh == 0 else slice(64, 192)
                    M = 65 if hh == 0 else 128
                    P.op('pe', lambda e, O=O, sc=sc, Ptk=Ptk, vs=vs, M=M, tb=tb: e.matmul(
                        O[0:M, 0:128], Vc[:, sc, vs], Ptk, start=(sc == 0), stop=(sc == tb)),
                        reads=['Vc_a', 'Vc_b', 'Vc_c', f'dPt{k}'], writes=[f'ps{ob}'])

                D1(0)
                for sc in range(tb + 1):
                    if sc + 1 < tb + 1:
                        D1(sc + 1)
                    D2(sc)
                rd, bc_, osb = fin[fb]
                p = 64 if hh == 0 else 0
                P.op('act', lambda e, rd=rd, O=O, p=p: e.activation(out=rd[:, :], in_=O[:, 0:128], func=AF.Ln),
                     reads=[f'ps{ob}'], writes=[f'frd{fb}'])
                P.op('act', lambda e, rd=rd, p=p: e.activation(out=rd[:, :], in_=rd[:, :], func=AF.Exp, scale=-1.0),
                     reads=[f'frd{fb}'], writes=[f'frd{fb}'])
                BC = self.ps[6][:, 0:128]
                P.op('pe', lambda e, rd=rd, p=p, BC=BC: e.matmul(BC, self.ones_f32[p:p + 1, :], rd[p:p + 1, :],
                                                                 start=True, stop=True),
                     reads=[f'frd{fb}'], writes=['ps6'])
                P.op('act', lambda e, bc_=bc_, pb=pb, BC=BC: e.copy(out=bc_[pb:pb + 64, :], in_=BC[pb:pb + 64, :]),
                     reads=['ps6'], writes=[f'fbc{fb}'])
                P.op('act', lambda e, osb=osb, pb=pb, O=O: e.copy(out=osb[pb:pb + 64, :], in_=O[pb:pb + 64, 0:128]),
                     reads=[f'ps{ob}'], writes=[f'fos{fb}'])
                ydst = self.Y[pb:pb + 64, 5 + i, qs]
                P.op('dve', lambda e, ydst=ydst, osb=osb, bc_=bc_, pb=pb: e.tensor_tensor(
                    out=ydst, in0=osb[pb:pb + 64, :], in1=bc_[pb:pb + 64, :], op=ALU.mult),
                    reads=[f'fos{fb}', f'fbc{fb}'], writes=[f'y{5 + i}_{hh}_{tb}_128'])

        scn_cur = A1(0)
        Abis(0, scn_cur)
        A2(0, scn_cur)
        for tb in range(NQB):
            if tb + 1 < NQB:
                scn_nxt = A1(tb + 1)
                Abis(tb + 1, scn_nxt)
            Battn(tb)
            if tb + 1 < NQB:
                A2(tb + 1, scn_nxt)

    def merge(self, l, w_in):
        P = self.P
        A = self.A
        o = [OFF_A]

        def take(n):
            r = o[0]
            o[0] += n
            assert o[0] <= SB_BYTES, o[0]
            return r
        merged = A.carve(take(8 * S * 2), 128, [8, S], BF16)
        wg = [A.carve(take(6144), 128, [8, 384], BF16) for _ in range(2)]
        wu = [A.carve(take(9 * 256), 128, [9, 128], BF16) for _ in range(2)]
        wo = [A.carve(take(2048), 128, [8, 128], BF16) for _ in range(2)]
        sg = [A.carve(take(2048), 128, [512], F32) for _ in range(3)]
        mm = [A.carve(take(2048), 128, [512], F32) for _ in range(3)]
        ufox = self.w_up_fox[l].rearrange("(j p) n -> p j n", p=128)
        usb = self.w_up_sb[l]
        udsa = self.w_up_dsa[l]
        wout = self.w_out[l].rearrange("(kc p) n -> p kc n", p=128)
        for c in range(8):
            k = c % 2
            cs = slice(c * 128, (c + 1) * 128)
            for b in range(3):
                src = w_in[:, :, O_G + b * D + c * 128:O_G + b * D + (c + 1) * 128]
                dst = wg[k][:, :, b * 128:(b + 1) * 128]
                P.op('pool', lambda e, src=src, dst=dst: e.dma_start(out=dst, in_=src), writes=[f'wg{k}_{b}'], dma=True)
            W = wu[k]
            dl = [
                (W[:, 0:3, :], ufox[:, :, cs]),
                (W[:, 3:5, :], usb[0:256, :].rearrange("(j p) n -> p j n", p=128)[:, :, cs]),
                (W[0:64, 5, :], usb[256:320, cs]),
                (W[64:128, 6, :], udsa[0:64, cs]),
                (W[:, 7:9, :], udsa[64:320, :].rearrange("(j p) n -> p j n", p=128)[:, :, cs]),
            ]
            for i, (dst, src) in enumerate(dl):
                P.op('pool', lambda e, src=src, dst=dst: e.dma_start(out=dst, in_=src), writes=[f'wu{k}_{i}'], dma=True)
            wun = [f'wu{k}_{i}' for i in range(5)]
            for T in range(NTB):
                ts = slice(T * TB, (T + 1) * TB)
                yn = [nm for nm in self.P.bufs if nm.startswith('y') and nm.endswith(f'_{T}')]
                for b in range(3):
                    G = self.ps[b]
                    for kc in range(8):
                        P.op('pe', lambda e, G=G, kc=kc, b=b, ts=ts, k=k: e.matmul(
                            G, wg[k][:, kc, b * 128:(b + 1) * 128], self.H[:, kc, ts], start=(kc == 0), stop=(kc == 7)),
                            reads=[f'wg{k}_{b}', f'h_{T}'], writes=[f'ps{b}'])
                    U = self.ps[3 + b]
                    if b == 0:
                        terms = [(W[:, j, :], self.Y[:, j, ts]) for j in range(3)]
                    elif b == 1:
                        terms = [(W[:, 3, :], self.Y[:, 3, ts]), (W[:, 4, :], self.Y[:, 4, ts]),
                                 (W[0:64, 5, :], self.Y[0:64, 5, ts])]
                    else:
                        terms = [(W[64:128, 6, :], self.Y[64:128, 5, ts]), (W[:, 7, :], self.Y[:, 6, ts]),
                                 (W[:, 8, :], self.Y[:, 7, ts])]
                    for ti, (lh, rh) in enumerate(terms):
                        P.op('pe', lambda e, U=U, lh=lh, rh=rh, ti=ti: e.matmul(U, lh, rh, start=(ti == 0), stop=(ti == 2)),
                             reads=wun + ['yall'], writes=[f'ps{3 + b}'])
                    P.op('act', lambda e, G=G, b=b: e.activation(out=sg[b], in_=G, func=AF.Sigmoid),
                         reads=[f'ps{b}'], writes=[f'sg{b}'])
                    if self.stage.rstrip('XYH') == 'mgd':
                        P.op('dve', lambda e, b=b, ts=ts: e.tensor_copy(out=self.X[:, b, ts], in_=sg[b]),
                             reads=[f'sg{b}'], writes=[f'x{b}_{T}'])
                        P.op('dve', lambda e, b=b, ts=ts, U=U: e.tensor_copy(out=self.X[:, 3 + b, ts], in_=U),
                             reads=[f'ps{3 + b}'], writes=[f'x{3 + b}_{T}'])
                    P.op('dve', lambda e, U=U, b=b: e.tensor_tensor(out=mm[b], in0=sg[b], in1=U, op=ALU.mult),
                         reads=[f'sg{b}', f'ps{3 + b}'], writes=[f'mm{b}'])
                P.op('pool', lambda e: e.tensor_tensor(out=mm[0], in0=mm[0], in1=mm[1], op=ALU.add),
                     reads=['mm0', 'mm1'], writes=['mm0'])
                P.op('pool', lambda e, c=c, ts=ts: e.tensor_tensor(out=merged[:, c, ts], in0=mm[0], in1=mm[2], op=ALU.add),
                     reads=['mm0', 'mm2'], writes=[f'mg_{T}'])
                if self.stage.rstrip('XYH') == 'mgd':
                    P.op('dve', lambda e, ts=ts: e.tensor_copy(out=self.X[:, 6, ts], in_=mm[0]), reads=['mm0'], writes=[f'x6_{T}'])
                    P.op('dve', lambda e, ts=ts, c=c: e.tensor_copy(out=self.X[:, 7, ts], in_=merged[:, c, ts]), reads=[f'mg_{T}'], writes=[f'x7_{T}'])
            self.chk('mgd')
        if self.stage.rstrip('XYH') == 'mg':
            for c in range(8):
                P.op('dve', lambda e, c=c: e.tensor_copy(out=self.X[:, c, :], in_=merged[:, c, :]),
                     reads=[f'mg_{T}' for T in range(NTB)], writes=[f'x{c}_{T}' for T in range(NTB)])
            self.chk('mg')
        for c2 in range(8):
            k = c2 % 2
            src = wout[:, :, c2 * 128:(c2 + 1) * 128]
            dst = wo[k]
            P.op('pool', lambda e, src=src, dst=dst: e.dma_start(out=dst, in_=src), writes=[f'wo{k}'], dma=True)
            for T in range(NTB):
                ts = slice(T * TB, (T + 1) * TB)
                pb_ = 6 + (c2 * NTB + T) % 2
                po = self.ps[pb_]
                for kc in range(8):
                    P.op('pe', lambda e, po=po, kc=kc, dst=dst, ts=ts: e.matmul(
                        po, dst[:, kc, :], merged[:, kc, ts], start=(kc == 0), stop=(kc == 7)),
                        reads=[f'wo{k}', f'mg_{T}'], writes=[f'ps{pb_}'])
                xd = self.X[:, c2, ts]
                P.op('dve', lambda e, xd=xd, po=po: e.tensor_tensor(out=xd, in0=xd, in1=po, op=ALU.add),
                     reads=[f'ps{pb_}', f'x{c2}_{T}'], writes=[f'x{c2}_{T}'])

    def final(self, s):
        P = self.P
        A = self.A
        gi = 3 * DEPTH
        o = OFF_A
        stg = [A.carve(o + k * 16384, 128, [8, TB], F32) for k in range(2)]; o += 32768
        tmp_off = o
        for t in range(NTB):
            k = t % 2
            if self.stage[-1] in 'XYH':
                SRC = {'X': self.X, 'Y': self.Y, 'H': self.H}[self.stage[-1]]
                for c in range(8):
                    P.op('dve', lambda e, c=c, t=t, k=k: e.tensor_copy(out=stg[k][:, c, :], in_=SRC[:, c, t * TB:(t + 1) * TB]),
                         reads=[f'x{c}_{t}'], writes=[f'stg{k}_{c}'])
            else:
                self.rmsnorm(gi, lambda c, t, k=k: (stg[k][:, c, :], f'stg{k}_{c}'), tmp_off, t_list=[t])
            for c in range(8):
                dst = self.outT[s, c * 128:(c + 1) * 128, t * TB:(t + 1) * TB]
                src = stg[k][:, c, :]
                P.op('sp', lambda e, src=src, dst=dst: e.dma_start(out=dst, in_=src),
                     reads=[f'stg{k}_{c}'], dma=True)


def pack_gains(inp):
    g = np.zeros((128, NGAIN), np.float32)
    for l in range(DEPTH):
        for k, name in enumerate(("g_ffn1", "g_mix", "g_ffn2")):
            g[:, (l * 3 + k) * 8:(l * 3 + k + 1) * 8] = np.asarray(inp[name][l], np.float32).reshape(8, 128).T
    g[:, 3 * DEPTH * 8:] = np.asarray(inp["g_final"], np.float32).reshape(8, 128).T
    return g


def rope_consts():
    pos = np.arange(S, dtype=np.float32)
    inv = (np.float32(500000.0) ** (-np.arange(0, 16, 2, dtype=np.float32) / np.float32(16))).astype(np.float32)
    ang = pos[None, :] * inv[:, None]
    cos, sin = np.cos(ang).astype(np.float32), np.sin(ang).astype(np.float32)
    r = np.zeros((128, 2, S), np.float32)
    r[:, 0, :] = 1.0
    for p in range(128):
        d = p % 64
        if d < 16:
            r[p, 0] = cos[d % 8]
            r[p, 1] = -sin[d % 8] if d < 8 else sin[d % 8]
    return r.reshape(128, 2 * S)


def const_tables():
    i = np.arange(128)
    cbf = np.zeros((128, NCBF), np.float32)
    cbf[:, 0:128] = (i[:, None] <= i[None, :])
    for k in range(4):
        m = np.zeros((128, 4, 128), np.float32)
        m[:, k, :] = (i[:, None] < i[None, :])
        m[:, k + 1:, :] = 1.0
        cbf[:, 128 + 512 * k:128 + 512 * (k + 1)] = m.reshape(128, 512)
    cbf[:, 2176:2304] = -(i[:, None] >= i[None, :]).astype(np.float32)
    cbf[:, 2304:2432] = -1.0
    cbf[:, 2432:2560] = np.eye(128, dtype=np.float32)
    cbf[:, 2560:2688] = 1.0 / 1024
    cbf[:, 2688:2816] = 1.0 / 128
    cbf[:, 2816:2944] = 1.0 / 64
    cf = np.zeros((128, NCF32), np.float32)
    cf[:, 0:128] = (i[:, None] <= i[None, :])
    cf[:, 128:256] = 1.0
    cf[:, 256:272] = 2.0 ** -(np.arange(16) + 1.0)
    return cbf, cf


def make_in_maps(inp, n_cores, n_seq):
    x = np.asarray(inp["x"], np.float32)
    gains = pack_gains(inp)
    smallp = np.zeros((128, 256), np.float32)
    for l in range(DEPTH):
        smallp[:, l * 96:(l + 1) * 96] = np.tile(np.asarray(inp["b_forget"][l], np.float32), 16)[None, :]
        smallp[:, 192 + l] = np.asarray(inp["g_kv_latent"][l], np.float32)
        smallp[:64, 194 + l] = np.asarray(inp["g_idx_k"][l], np.float32)
        smallp[64:, 194 + l] = np.asarray(inp["g_idx_k"][l], np.float32)
        gsw = np.asarray(inp["g_idx_k"][l], np.float32).copy()
        gsw[0:8], gsw[8:16] = gsw[8:16].copy(), gsw[0:8].copy()
        smallp[:64, 196 + l] = gsw
        smallp[64:, 196 + l] = gsw
    rope = rope_consts()
    cbf, cf32 = const_tables()
    maps = []
    shared = {
        "w_ffn1_gu": np.asarray(inp["w_ffn1_gu"], np.float32), "w_ffn2_gu": np.asarray(inp["w_ffn2_gu"], np.float32),
        "w_ffn1_down": np.asarray(inp["w_ffn1_down"], np.float32),
        "w_ffn2_down": np.asarray(inp["w_ffn2_down"], np.float32),
        "w_in": np.asarray(inp["w_in"], np.float32), "w_kv_up": np.asarray(inp["w_kv_up"], np.float32),
        "w_up_fox": np.asarray(inp["w_up_fox"], np.float32), "w_up_sb": np.asarray(inp["w_up_sb"], np.float32),
        "w_up_dsa": np.asarray(inp["w_up_dsa"], np.float32), "w_out": np.asarray(inp["w_out"], np.float32),
        "gains": gains, "smallp": smallp, "rope": rope, "cbf": cbf, "cf32": cf32,
    }
    for cidx in range(n_cores):
        xs = x[cidx * n_seq:(cidx + 1) * n_seq]
        m = dict(shared)
        m["xT"] = np.ascontiguousarray(xs.transpose(0, 2, 1))
        maps.append(m)
    return maps


def kernel(**inputs):
    n_cores, n_seq = 8, 2
    mdl = Model(n_seq=n_seq)
    nc = mdl.build()
    maps = make_in_maps(inputs, n_cores, n_seq)
    res = run_bass_kernel_spmd(nc, maps, core_ids=list(range(n_cores)))
    outs = [r["outT"].transpose(0, 2, 1) for r in res.results]
    return np.ascontiguousarray(np.concatenate(outs, axis=0)).astype(np.float32)
```

```python
import numpy as np
import concourse.bass as bass
import concourse.mybir as mybir
from concourse.bass_utils import run_bass_kernel_spmd

F32 = mybir.dt.float32
BF16 = mybir.dt.bfloat16
U8 = mybir.dt.uint8
AF = mybir.ActivationFunctionType
ALU = mybir.AluOpType

D = 1024
S = 2048
DEPTH = 2
DFF = 2816
NFF = DFF // 128
HD = 64
NH_A, NH_B, NH_C = 6, 5, 5
W_A, W_B, W_C = 384, 320, 320
KVL = 128
NIH = 4
D_IN = 5962
EPS = 1e-6
TB = 512
NTB = S // TB
NQB = S // 128

O_QA = 0
O_KA = O_QA + W_A
O_VA = O_KA + W_A
O_FA = O_VA + W_A
O_QB = O_FA + NH_A
O_KB = O_QB + W_B
O_VB = O_KB + W_B
O_QC = O_VB + W_B
O_CKV = O_QC + W_C
O_QI = O_CKV + KVL
O_KI = O_QI + NIH * 64
O_WI = O_KI + 64
O_G = O_WI + NIH
assert O_G + 3 * D == D_IN

EPOCH = 4096
ENGS = ['pe', 'act', 'dve', 'pool', 'sp']


class Buf:
    __slots__ = ('name', 'lw', 'rd')

    def __init__(self, name):
        self.name = name
        self.lw = None
        self.rd = {}


class Prog:
    NRING = 8

    def __init__(self):
        self.ops = {e: [] for e in ENGS}
        self.waited = {e: {} for e in ENGS}
        self.ndma = {e: 0 for e in ENGS}
        self.bufs = {}

    def buf(self, name):
        b = self.bufs.get(name)
        if b is None:
            b = Buf(name)
            self.bufs[name] = b
        return b

    def _tok(self, names):
        return [self.buf(n) if isinstance(n, str) else n for n in names]

    def op(self, eng, emit, reads=(), writes=(), dma=False):
        reads = self._tok(reads)
        writes = self._tok(writes)
        idx = len(self.ops[eng])
        deps = set()
        for b in reads:
            if b.lw is not None:
                deps.add(b.lw)
        for b in writes:
            if b.lw is not None:
                deps.add(b.lw)
            for t in b.rd.values():
                deps.add(t)
        if dma:
            d = self.ndma[eng]
            self.ndma[eng] += 1
            tok = ('d', eng, d)
            if d >= self.NRING:
                deps.add(('d', eng, d - self.NRING))
        else:
            tok = ('c', eng, idx)
        waits = []
        w = self.waited[eng]
        for t in deps:
            if t[0] == 'c':
                _, e, i = t
                if e == eng and eng == 'pe':
                    continue
                if w.get(('c', e), -1) >= i:
                    continue
                w[('c', e)] = i
                self.ops[e][i][2] = True
                waits.append(t)
            else:
                _, q, d0 = t
                key = ('d', q, d0 % self.NRING)
                if w.get(key, -1) >= d0:
                    continue
                w[key] = d0
                waits.append(t)
        self.ops[eng].append([emit, waits, False, tok if dma else None])
        rkey = (tok[0], tok[1]) if tok[0] == 'c' else (tok[0], tok[1], tok[2] % self.NRING)
        for b in reads:
            b.rd[rkey] = tok
        for b in writes:
            b.lw = tok
            b.rd = {}
        return tok

    def barrier(self):
        last = {}
        for e in ENGS:
            for i in range(len(self.ops[e]) - 1, -1, -1):
                o = self.ops[e][i]
                if o[0] is not None and o[3] is None:
                    last[e] = i
                    break
        for eng in ENGS:
            waits = []
            w = self.waited[eng]
            for e, i in last.items():
                if e == eng:
                    continue
                if w.get(('c', e), -1) >= i:
                    continue
                w[('c', e)] = i
                self.ops[e][i][2] = True
                waits.append(('c', e, i))
            for q in ENGS:
                n = self.ndma[q]
                for d0 in range(max(0, n - self.NRING), n):
                    key = ('d', q, d0 % self.NRING)
                    if w.get(key, -1) >= d0:
                        continue
                    w[key] = d0
                    waits.append(('d', q, d0))
            self.ops[eng].append([None, waits, False, None])
        for b in self.bufs.values():
            b.lw = None
            b.rd = {}

    def wait_all_dma(self, eng):
        waits = []
        for q in ENGS:
            n = self.ndma[q]
            for d in range(max(0, n - self.NRING), n):
                waits.append(('d', q, d))
        self.ops[eng].append([None, waits, False, None])

    def emit(self, nc, block_cm):
        cnt = {}
        nsem = {}
        for e in ENGS:
            c = 0
            arr = []
            for o in self.ops[e]:
                if o[2]:
                    c += 1
                arr.append(c)
            cnt[e] = arr
            nsem[e] = (c + EPOCH - 1) // EPOCH
        csem = {e: [nc.alloc_semaphore(name=f"c_{e}_{k}") for k in range(nsem[e])] for e in ENGS}
        dsem = {e: [nc.alloc_semaphore(name=f"d_{e}_{k}") for k in range(self.NRING)]
                for e in ENGS if self.ndma[e] > 0}

        def resolve(t):
            if t[0] == 'c':
                _, e, i = t
                c = cnt[e][i]
                return csem[e][(c - 1) // EPOCH], (c - 1) % EPOCH + 1
            _, q, d0 = t
            return dsem[q][d0 % self.NRING], 16 * (d0 // self.NRING + 1)

        prog = self

        def run(e, eng):
            for k, (emit, waits, marked, dtok) in enumerate(prog.ops[e]):
                for t in waits:
                    s, v = resolve(t)
                    eng.wait_ge(s, v)
                if emit is None:
                    continue
                ins = emit(eng)
                if dtok is not None:
                    s, _ = resolve(dtok)
                    ins.then_inc(s, 16)
                elif marked:
                    c = cnt[e][k]
                    ins.then_inc(csem[e][(c - 1) // EPOCH], 1)

        with block_cm as block:
            @block.tensor
            def _(eng):
                run('pe', eng)

            @block.scalar
            def _(eng):
                run('act', eng)

            @block.vector
            def _(eng):
                run('dve', eng)

            @block.gpsimd
            def _(eng):
                run('pool', eng)

            @block.sync
            def _(eng):
                run('sp', eng)


class Arena:
    def __init__(self, ap_u8, nbytes):
        self.ap = ap_u8
        self.nbytes = nbytes

    def carve(self, off, parts, free_shape, dtype, pbase=0):
        esz = 4 if dtype == F32 else 2
        n = 1
        for s in free_shape:
            n *= s
        assert off % 4 == 0 and off + n * esz <= self.nbytes, (off, n * esz, self.nbytes)
        a = self.ap[pbase:pbase + parts, off:off + n * esz].bitcast(dtype)
        if len(free_shape) == 2:
            a = a.rearrange('p (a b) -> p a b', a=free_shape[0])
        elif len(free_shape) == 3:
            a = a.rearrange('p (a b c) -> p a b c', a=free_shape[0], b=free_shape[1])
        return a


OFF_X = 0
OFF_H = OFF_X + 8 * S * 4
OFF_C = OFF_H + 8 * S * 2
OFF_Y = OFF_C + 10240
OFF_A = OFF_Y + 8 * S * 2
SB_BYTES = 212800
ARENA_BYTES = SB_BYTES - OFF_A
NGAIN = 8 * (3 * DEPTH + 1)
NCBF = 2944
NCF32 = 272
NBIS = 14


class _Stop(Exception):
    pass


class Model:
    def __init__(self, n_seq=2, depth=DEPTH, stage='full'):
        self.n_seq = n_seq
        self.depth = depth
        self.stage = stage
        nc = bass.Bass("TRN2", target_bir_lowering=False)
        self.nc = nc
        dt = nc.dram_tensor
        self.xT = dt("xT", [n_seq, D, S], F32, kind="ExternalInput").ap()
        self.outT = dt("outT", [n_seq, D, S], F32, kind="ExternalOutput").ap()
        self.w_gu = [dt(f"w_ffn{i}_gu", [DEPTH, D, 2 * DFF], F32, kind="ExternalInput").ap() for i in (1, 2)]
        self.w_dn = [dt(f"w_ffn{i}_down", [DEPTH, DFF, D], F32, kind="ExternalInput").ap() for i in (1, 2)]
        self.w_in = dt("w_in", [DEPTH, D, D_IN], F32, kind="ExternalInput").ap()
        self.w_kv_up = dt("w_kv_up", [DEPTH, KVL, 2 * HD], F32, kind="ExternalInput").ap()
        self.w_up_fox = dt("w_up_fox", [DEPTH, W_A, D], F32, kind="ExternalInput").ap()
        self.w_up_sb = dt("w_up_sb", [DEPTH, W_B, D], F32, kind="ExternalInput").ap()
        self.w_up_dsa = dt("w_up_dsa", [DEPTH, W_C, D], F32, kind="ExternalInput").ap()
        self.w_out = dt("w_out", [DEPTH, D, D], F32, kind="ExternalInput").ap()
        self.gains = dt("gains", [128, NGAIN], F32, kind="ExternalInput").ap()
        self.smallp = dt("smallp", [128, 256], F32, kind="ExternalInput").ap()
        self.rope = dt("rope", [128, 2 * S], F32, kind="ExternalInput").ap()
        self.cbf = dt("cbf", [128, NCBF], F32, kind="ExternalInput").ap()
        self.cf32 = dt("cf32", [128, NCF32], F32, kind="ExternalInput").ap()
        self.P = Prog()

    def build(self):
        nc = self.nc
        P = self.P
        with nc.sbuf_tensor("sb", [128, SB_BYTES], U8) as sb:
            self.psum_cms = [nc.psum_tensor(f"ps{k}", [128, 512], F32) for k in range(8)]
            self.ps = [cm.__enter__()[:] for cm in self.psum_cms]
            A = Arena(sb, SB_BYTES)
            self.A = A
            self.X = A.carve(OFF_X, 128, [8, S], F32)
            self.H = A.carve(OFF_H, 128, [8, S], BF16)
            self.Y = A.carve(OFF_Y, 128, [8, S], BF16)
            o = OFF_C
            self.gain_sb = A.carve(o, 128, [NGAIN], F32); o += NGAIN * 4
            self.small_sb = A.carve(o, 128, [256], F32); o += 1024
            self.cbf_sb = A.carve(o, 128, [NCBF], BF16); o += NCBF * 2
            self.cf32_sb = A.carve(o, 128, [NCF32], F32); o += NCF32 * 4
            cb = self.cbf_sb
            self.tri_incl = cb[:, 0:128]
            self.smask = [cb[:, 128 + 512 * k: 128 + 512 * (k + 1)] for k in range(4)]
            self.negtri = cb[:, 2176:2304]
            self.negones = cb[:, 2304:2432]
            self.ident = cb[:, 2432:2560]
            self.ones_mean = cb[:, 2560:2688]
            self.ones_128th = cb[:, 2688:2816]
            self.ones_64th = cb[:, 2816:2944]
            self.tri_f32 = self.cf32_sb[:, 0:128]
            self.ones_f32 = self.cf32_sb[:, 128:256]
            self.pow2 = self.cf32_sb[:, 256:272]
            self.cst = A.carve(o, 128, [16], F32); o += 64
            self.c_off = o
            assert o <= OFF_C + 10240, o
            self.consts()
            for s in range(self.n_seq):
                self.load_x(s)
                try:
                    for l in range(self.depth):
                        self.ffn(l, 0)
                        if self.stage == 'ffn1':
                            break
                        self.mixer(l)
                        self.ffn(l, 1)
                except _Stop:
                    pass
                P.barrier()
                self.final(s)
                P.barrier()
            P.wait_all_dma('sp')
            P.emit(nc, nc.Block())
            for cm in reversed(self.psum_cms):
                cm.__exit__(None, None, None)
        return nc

    def consts(self):
        P = self.P
        P.op('pool', lambda e: e.dma_start(out=self.cbf_sb, in_=self.cbf), writes=['ones_mean'], dma=True)
        P.op('sp', lambda e: e.dma_start(out=self.cf32_sb, in_=self.cf32), writes=['cf32'], dma=True)
        P.op('pool', lambda e: e.memset(self.cst[:, 0:1], EPS), writes=['cst'])
        P.op('pool', lambda e: e.memset(self.cst[:, 1:2], 1.0), writes=['cst'])
        P.op('pool', lambda e: e.memset(self.cst[:, 2:3], 0.0), writes=['cst'])
        g = self.gain_sb
        P.op('sp', lambda e: e.dma_start(out=g, in_=self.gains), writes=['gains'], dma=True)
        sm = self.small_sb
        P.op('sp', lambda e: e.dma_start(out=sm, in_=self.smallp), writes=['smallp'], dma=True)

    def load_x(self, s):
        P = self.P
        for c in range(8):
            src = self.xT[s, c * 128:(c + 1) * 128, :]
            dst = self.X[:, c, :]
            P.op('sp', lambda e, src=src, dst=dst: e.dma_start(out=dst, in_=src),
                 writes=[f'x{c}_{t}' for t in range(NTB)], dma=True)

    def rmsnorm(self, gi, out_fn, tmp_off, t_list=None):
        P = self.P
        A = self.A
        sq = A.carve(tmp_off, 128, [2, 8, TB], BF16)
        rstd = A.carve(tmp_off + 2 * 8 * TB * 2, 128, [2, TB], F32)
        for t in (range(NTB) if t_list is None else t_list):
            k = t % 2
            ts = slice(t * TB, (t + 1) * TB)
            xin = self.X[:, :, ts]
            sqk = sq[:, k]
            P.op('pool', lambda e, xin=xin, sqk=sqk: e.tensor_tensor(out=sqk, in0=xin, in1=xin, op=ALU.mult),
                 reads=[f'x{c}_{t}' for c in range(8)], writes=[f'sq{k}'])
            ps = self.ps[6 + k]
            for c in range(8):
                P.op('pe', lambda e, ps=ps, c=c, sqk=sqk: e.matmul(ps, self.ones_mean, sqk[:, c, :],
                                                                   start=(c == 0), stop=(c == 7)),
                     reads=[f'sq{k}', 'ones_mean'], writes=[f'ps{6 + k}'])
            rk = rstd[:, k]
            P.op('act', lambda e, ps=ps, rk=rk: e.activation(out=rk, in_=ps, func=AF.Ln, bias=self.cst[:, 0:1]),
                 reads=[f'ps{6 + k}', 'cst'], writes=[f'rstd{k}'])
            P.op('act', lambda e, rk=rk: e.activation(out=rk, in_=rk, func=AF.Exp, scale=-0.5),
                 reads=[f'rstd{k}'], writes=[f'rstd{k}'])
            for c in range(8):
                dst, bname = out_fn(c, t)
                xin_c = self.X[:, c, ts]
                gcol = self.gain_sb[:, gi * 8 + c: gi * 8 + c + 1]
                eng = 'dve'
                P.op(eng, lambda e, dst=dst, xin_c=xin_c, gcol=gcol, rk=rk:
                     e.scalar_tensor_tensor(out=dst, in0=xin_c, scalar=gcol, in1=rk, op0=ALU.mult, op1=ALU.mult),
                     reads=[f'x{c}_{t}', f'rstd{k}', 'gains'], writes=[bname])

    def norm_to_H(self, gi, tmp_off):
        self.rmsnorm(gi, lambda c, t: (self.H[:, c, t * TB:(t + 1) * TB], f'h_{t}'), tmp_off)

    def ffn(self, l, which):
        P = self.P
        A = self.A
        gi = l * 3 + (0 if which == 0 else 2)
        o = OFF_A
        actT = A.carve(o, 128, [NFF // 2, S], BF16); o += (NFF // 2) * S * 2
        wgu = [A.carve(o + k * 4096, 128, [8, 2, 128], BF16) for k in range(2)]; o += 8192
        wdn = [A.carve(o + k * 2816, 128, [NFF // 2, 128], BF16) for k in range(2)]; o += 2 * 2816
        sg = [A.carve(o + k * 2048, 128, [TB], F32) for k in range(2)]; o += 4096
        assert o <= SB_BYTES, o
        P.barrier()
        self.norm_to_H(gi, OFF_A)
        P.barrier()
        self.chk(f'f{which}norm')
        w_gu = self.w_gu[which][l].rearrange("(kc p) (two n) -> p kc two n", p=128, two=2)
        w_dn = self.w_dn[which][l].rearrange("(j p) n -> p j n", p=128)
        NH = NFF // 2
        cnt = 0
        for half in range(2):
            for jj in range(NH):
                j = half * NH + jj
                wb = cnt % 2
                dst = wgu[wb]
                for two in range(2):
                    src = w_gu[:, :, two, j * 128:(j + 1) * 128]
                    dd = dst[:, :, two, :]
                    P.op('pool', lambda e, src=src, dd=dd: e.dma_start(out=dd, in_=src),
                         writes=[f'wgu{wb}_{two}'], dma=True)
                for t in range(NTB):
                    pb = (cnt * NTB + t) % 2
                    ts = slice(t * TB, (t + 1) * TB)
                    pg, pu = self.ps[pb], self.ps[2 + pb]
                    for kc in range(8):
                        P.op('pe', lambda e, pg=pg, dst=dst, kc=kc, ts=ts: e.matmul(
                            pg, dst[:, kc, 0, :], self.H[:, kc, ts], start=(kc == 0), stop=(kc == 7)),
                            reads=[f'wgu{wb}_0', f'h_{t}'], writes=[f'ps{pb}'])
                    for kc in range(8):
                        P.op('pe', lambda e, pu=pu, dst=dst, kc=kc, ts=ts: e.matmul(
                            pu, dst[:, kc, 1, :], self.H[:, kc, ts], start=(kc == 0), stop=(kc == 7)),
                            reads=[f'wgu{wb}_1', f'h_{t}'], writes=[f'ps{2 + pb}'])
                    sgk = sg[pb]
                    P.op('act', lambda e, sgk=sgk, pg=pg: e.activation(out=sgk, in_=pg, func=AF.Silu),
                         reads=[f'ps{pb}'], writes=[f'sg{pb}'])
                    adst = actT[:, jj, ts]
                    P.op('dve', lambda e, adst=adst, sgk=sgk, pu=pu: e.tensor_tensor(
                        out=adst, in0=sgk, in1=pu, op=ALU.mult),
                        reads=[f'sg{pb}', f'ps{2 + pb}'], writes=[f'act{jj}_{t}'])
                cnt += 1
            for dc in range(8):
                db = dc % 2
                src = w_dn[:, half * NH:(half + 1) * NH, dc * 128:(dc + 1) * 128]
                dst = wdn[db]
                P.op('pool', lambda e, src=src, dst=dst: e.dma_start(out=dst, in_=src),
                     writes=[f'wdn{db}'], dma=True)
                for t in range(NTB):
                    pb = 4 + (dc * NTB + t) % 2
                    ts = slice(t * TB, (t + 1) * TB)
                    po = self.ps[pb]
                    for jj in range(NH):
                        P.op('pe', lambda e, po=po, dst=dst, jj=jj, ts=ts: e.matmul(
                            po, dst[:, jj, :], actT[:, jj, ts], start=(jj == 0), stop=(jj == NH - 1)),
                            reads=[f'wdn{db}', f'act{jj}_{t}'], writes=[f'ps{pb}'])
                    xd = self.X[:, dc, ts]
                    P.op('dve', lambda e, xd=xd, po=po: e.scalar_tensor_tensor(
                        out=xd, in0=po, scalar=0.5, in1=xd, op0=ALU.mult, op1=ALU.add),
                        reads=[f'ps{pb}', f'x{dc}_{t}'], writes=[f'x{dc}_{t}'])
        self.chk(f'f{which}end')

    def chk(self, name):
        if self.stage.rstrip('XYH') == name:
            raise _Stop()

    def load_w(self, slot, src, ncols, name, dcol=0):
        dst = slot[:, :, dcol:dcol + ncols]
        self.P.op('pool', lambda e, src=src, dst=dst: e.dma_start(out=dst, in_=src), writes=[name], dma=True)

    def proj_T(self, slot, wname, M, evac, banks=(0, 1)):
        P = self.P
        for T in range(NTB):
            b = banks[T % 2]
            ps = self.ps[b]
            ts = slice(T * TB, (T + 1) * TB)
            for kc in range(8):
                P.op('pe', lambda e, ps=ps, kc=kc, ts=ts: e.matmul(ps[0:M, :], slot[:, kc, 0:M], self.H[:, kc, ts],
                                                                  start=(kc == 0), stop=(kc == 7)),
                     reads=[wname, f'h_{T}'], writes=[f'ps{b}'])
            evac(T, ps, f'ps{b}')

    def proj_tok(self, slot, wname, N, evac, banks=(0, 1)):
        P = self.P
        for g in range(4):
            b = banks[g % 2]
            ps = self.ps[b]
            for cc in range(4):
                ch = 4 * g + cc
                for kc in range(8):
                    P.op('pe', lambda e, ps=ps, kc=kc, ch=ch, cc=cc: e.matmul(
                        ps[:, cc * 128:cc * 128 + N], self.H[:, kc, ch * 128:(ch + 1) * 128], slot[:, kc, 0:N],
                        start=(kc == 0), stop=(kc == 7)),
                        reads=[wname, f'h_{ch // 4}'], writes=[f'ps{b}'])
            self.chk('tok_mm')
            evac(g, ps.rearrange("p (a b) -> p a b", a=4), f'ps{b}')
            self.chk('tok_ev')

    def attn_finish(self, O, oname, hh, ychunk, T, normalize, rden, bcs, width=TB):
        P = self.P
        pb = 64 * hh
        ts = slice(T * width, (T + 1) * width)
        ydst = self.Y[pb:pb + 64, ychunk, ts]
        yname = f'y{ychunk}_{hh}_{T}_{width}'
        O = O[:, 0:width]
        rden = rden[:, 0:width]
        bcs = bcs[:, 0:width]
        if not normalize:
            P.op('act', lambda e: e.copy(out=ydst, in_=O[pb:pb + 64, :]), reads=[oname], writes=[yname])
            return
        p = 64 if hh == 0 else 0
        P.op('dve', lambda e: e.reciprocal(out=rden[p:p + 1, :], in_=O[p:p + 1, :]), reads=[oname], writes=['rden'])
        BC = self.ps[6][:, 0:width]
        P.op('pe', lambda e: e.matmul(BC, self.ones_f32[p:p + 1, :], rden[p:p + 1, :], start=True, stop=True),
             reads=['rden'], writes=['ps6'])
        P.op('act', lambda e: e.copy(out=bcs[pb:pb + 64, :], in_=BC[pb:pb + 64, :]), reads=['ps6'], writes=['bcs'])
        P.op('dve', lambda e: e.tensor_tensor(out=ydst, in0=O[pb:pb + 64, :], in1=bcs[pb:pb + 64, :], op=ALU.mult),
             reads=[oname, 'bcs'], writes=[yname])

    def mixer(self, l):
        P = self.P
        A = self.A
        P.barrier()
        self.norm_to_H(l * 3 + 1, OFF_A)
        P.barrier()
        self.chk('norm')
        w_in = self.w_in[l].rearrange("(kc p) n -> p kc n", p=128)
        o = [OFF_A]

        def take(n):
            r = o[0]
            o[0] += n
            assert o[0] <= SB_BYTES, o[0]
            return r
        WSL = [A.carve(take(2048), 128, [8, 128], BF16) for _ in range(6)]
        qT = A.carve(take(4096), 128, [S], BF16)
        kT = A.carve(take(4096), 128, [S], BF16)
        Vp = A.carve(take(16 * 192 * 2), 128, [16, 192], BF16)
        Pt = [A.carve(take(1024), 128, [512], BF16) for _ in range(4)]
        rden = A.carve(take(2048), 128, [512], F32)
        bcs = A.carve(take(2048), 128, [512], F32)
        base_common = o[0]
        self.cnt = 0
        self.ocnt = 0

        P.op('pool', lambda e: e.memset(Vp[:, :, 64:65], 1.0), writes=['Vp_c'])
        P.op('pool', lambda e: e.memset(Vp[:, :, 65:128], 0.0), writes=['Vp_c'])

        def proj_qkv(qcol, kcol, vcol, nc_, wi):
            s0, s1, s2 = WSL[wi], WSL[wi + 1], WSL[wi + 2]
            self.load_w(s0, w_in[:, :, qcol:qcol + nc_], nc_, f'wsl{wi}')
            self.load_w(s1, w_in[:, :, kcol:kcol + nc_], nc_, f'wsl{wi + 1}')
            self.load_w(s2, w_in[:, :, vcol:vcol + nc_], nc_, f'wsl{wi + 2}')
            self.proj_T(s0, f'wsl{wi}', nc_, lambda T, ps, pn: P.op(
                'dve', lambda e: e.tensor_scalar(out=qT[0:nc_, T * TB:(T + 1) * TB], in0=ps[0:nc_, :], scalar1=0.125,
                                                 scalar2=None, op0=ALU.mult),
                reads=[pn], writes=[f'qT_{T}']))
            self.chk('projq')
            self.proj_T(s1, f'wsl{wi + 1}', nc_, lambda T, ps, pn: P.op(
                'dve', lambda e: e.tensor_copy(out=kT[0:nc_, T * TB:(T + 1) * TB], in_=ps[0:nc_, :]),
                reads=[pn], writes=[f'kT_{T}']))
            self.chk('projk')

            import os
            VAR = os.environ.get('EVVAR', 'ab')

            def ev(g, ps3, pn):
                if 'a' in VAR:
                  P.op('dve', lambda e: e.tensor_copy(out=Vp[:, 4 * g:4 * g + 4, 0:64], in_=ps3[:, :, 0:64]),
                     reads=[pn], writes=[f'Vp_{g}a'])
                if nc_ > 64 and 'b' in VAR:
                    P.op('dve', lambda e: e.tensor_copy(out=Vp[:, 4 * g:4 * g + 4, 128:192], in_=ps3[:, :, 64:128]),
                         reads=[pn], writes=[f'Vp_{g}b'])
            self.proj_tok(s2, f'wsl{wi + 2}', nc_, ev)
            self.chk('proj')

        ls = A.carve(take(384), 128, [16, 6], F32)
        tot = A.carve(take(384), 128, [16, 6], F32)
        pre = A.carve(take(17 * 24), 128, [17, 6], F32)
        cpos = A.carve(take(384), 128, [16, 6], F32)
        Btab = A.carve(take(6 * 256 * 4), 128, [6, 16, 16], F32)
        base_fox = o[0]
        self.load_w(WSL[5], w_in[:, :, O_FA:O_FA + 6], 6, 'wsl5')
        ps7 = self.ps[7]
        for ch in range(16):
            for kc in range(8):
                P.op('pe', lambda e, ch=ch, kc=kc: e.matmul(ps7[:, ch * 6:(ch + 1) * 6],
                                                              self.H[:, kc, ch * 128:(ch + 1) * 128],
                                                              WSL[5][:, kc, 0:6], start=(kc == 0), stop=(kc == 7)),
                     reads=['wsl5', f'h_{ch // 4}'], writes=['ps7'])
        lsf = ls.rearrange("p a b -> p (a b)")
        P.op('dve', lambda e: e.tensor_tensor(out=lsf, in0=ps7[:, 0:96], in1=self.small_sb[:, l * 96:(l + 1) * 96],
                                              op=ALU.add), reads=['ps7'], writes=['ls'])
        P.op('act', lambda e: e.activation(out=lsf, in_=lsf, func=AF.Exp, scale=-1.0), reads=['ls'], writes=['ls'])
        P.op('act', lambda e: e.activation(out=lsf, in_=lsf, func=AF.Ln, bias=self.cst[:, 1:2]),
             reads=['ls'], writes=['ls'])
        ps6 = self.ps[6]
        P.op('pe', lambda e: e.matmul(ps6[:, 0:96], self.tri_f32, lsf, start=True, stop=True),
             reads=['ls'], writes=['ps6'])
        P.op('pe', lambda e: e.matmul(ps6[:, 128:224], self.ones_f32, lsf, start=True, stop=True),
             reads=['ls'], writes=['ps6'])
        P.op('dve', lambda e: e.tensor_copy(out=tot.rearrange("p a b -> p (a b)"), in_=ps6[:, 128:224]),
             reads=['ps6'], writes=['tot'])
        P.op('dve', lambda e: e.memset(pre[:, 0, :], 0.0), writes=['pre'])
        for ch in range(1, 17):
            P.op('dve', lambda e, ch=ch: e.tensor_tensor(out=pre[:, ch, :], in0=pre[:, ch - 1, :],
                                                         in1=tot[:, ch - 1, :], op=ALU.add),
                 reads=['tot', 'pre'], writes=['pre'])
        P.op('dve', lambda e: e.tensor_tensor(out=cpos.rearrange("p a b -> p (a b)"), in0=ps6[:, 0:96],
                                              in1=pre[:, 0:16, :].rearrange("p a b -> p (a b)"), op=ALU.add),
             reads=['ps6', 'pre'], writes=['cpos'])
        for h in range(6):
            for tb in range(16):
                P.op('dve', lambda e, h=h, tb=tb: e.tensor_scalar(
                    out=Btab[:, h, tb, :], in0=cpos[:, :, h], scalar1=pre[:, tb + 1, h:h + 1], scalar2=None,
                    op0=ALU.subtract), reads=['cpos', 'pre'], writes=['Btab'])

        self.chk('pre')
        def softmax_attn(hh, ychunk, bias_fn, mask_fn):
            pb = 64 * hh
            for T in range(NTB):
                nsc = 4 * T + 4
                ob = 4 + (self.ocnt % 2)
                self.ocnt += 1
                O = self.ps[ob]
                tinfo = {}

                def S1(sc):
                    zb = 2 + (self.cnt % 2)
                    k = self.cnt % 4
                    self.cnt += 1
                    tinfo[sc] = k
                    Z = self.ps[zb]
                    P.op('pe', lambda e, Z=Z, sc=sc, T=T: e.matmul(
                        Z, kT[pb:pb + 64, sc * 128:(sc + 1) * 128], qT[pb:pb + 64, T * TB:(T + 1) * TB],
                        start=True, stop=True), reads=[f'kT_{sc // 4}', f'qT_{T}'], writes=[f'ps{zb}'])
                    Ptk = Pt[k]
                    for tl in range(4):
                        tb = 4 * T + tl
                        cs = slice(tl * 128, (tl + 1) * 128)
                        pn = f'Pt{k}_{tl}'
                        if tb < sc:
                            P.op('pool', lambda e, Ptk=Ptk, cs=cs: e.memset(Ptk[:, cs], 0.0), writes=[pn])
                            continue
                        bias = bias_fn(sc, tb)
                        P.op('act', lambda e, Ptk=Ptk, cs=cs, Z=Z, bias=bias: e.activation(
                            out=Ptk[:, cs], in_=Z[:, cs], func=AF.Exp, bias=bias),
                            reads=[f'ps{zb}', 'Btab'], writes=[pn])
                        mask_fn(sc, tb, Ptk[:, cs], pn)

                def S2(sc):
                    k = tinfo[sc]
                    Ptk = Pt[k]
                    vs = slice(0, 65) if hh == 0 else slice(64, 192)
                    M = 65 if hh == 0 else 128
                    P.op('pe', lambda e, O=O, sc=sc, Ptk=Ptk, vs=vs, M=M, nsc=nsc: e.matmul(
                        O[0:M, :], Vp[:, sc, vs], Ptk, start=(sc == 0), stop=(sc == nsc - 1)),
                        reads=[f'Vp_{sc // 4}a', f'Vp_{sc // 4}b', 'Vp_c'] + [f'Pt{k}_{tl}' for tl in range(4)], writes=[f'ps{ob}'])

                S1(0)
                for sc in range(nsc):
                    if sc + 1 < nsc:
                        S1(sc + 1)
                    S2(sc)
                    if sc == 0:
                        self.chk('fox_sc0')
                self.chk('fox_T0n')
                self.attn_finish(O, f'ps{ob}', hh, ychunk, T, True, rden, bcs)
                self.chk('fox_T0')

        def fox_mask(sc, tb, ap, pn):
            if sc == tb:
                P.op('pool', lambda e: e.tensor_tensor(out=ap, in0=ap, in1=self.tri_incl, op=ALU.mult),
                     reads=[pn], writes=[pn])

        if True:
            for hp in range(3):
                proj_qkv(O_QA + hp * 128, O_KA + hp * 128, O_VA + hp * 128, 128, 3 * (hp % 2))
                for hh in range(2):
                    h = 2 * hp + hh
                    softmax_attn(hh, hp, lambda sc, tb, h=h: Btab[:, h, tb, sc:sc + 1], fox_mask)
        P.barrier()

        o[0] = base_common
        e32 = A.carve(take(2048), 128, [512], F32)
        spb = [A.carve(take(1024), 128, [512], BF16) for _ in range(2)]
        Ab = [A.carve(take(1024), 128, [512], BF16) for _ in range(2)]
        R = A.carve(take(2048), 128, [512], F32)
        Rb = [A.carve(take(1024), 128, [512], BF16) for _ in range(2)]

        Rb3 = Rb + [A.carve(take(1024), 128, [512], BF16)]

        def sb_attn(hh, ychunk):
            pb = 64 * hh
            for T in range(NTB):
                nsc = 4 * T + 4
                ob = 6 + (T % 2)
                O = self.ps[ob]
                order = list(reversed(range(nsc)))
                info = {}

                def S1(i):
                    sc = order[i]
                    c2 = self.cnt % 2
                    self.cnt += 1
                    info[i] = c2
                    zb = 2 + c2
                    Z = self.ps[zb]
                    kk = kT[pb:pb + 64, sc * 128:(sc + 1) * 128]
                    qq = qT[pb:pb + 64, T * TB:(T + 1) * TB]
                    P.op('pe', lambda e, Z=Z, kk=kk, qq=qq: e.matmul(Z, kk, qq, start=True, stop=True),
                         reads=[f'kT_{sc // 4}', f'qT_{T}'], writes=[f'ps{zb}'])
                    P.op('act', lambda e, Z=Z: e.activation(out=e32, in_=Z, func=AF.Exp),
                         reads=[f'ps{zb}'], writes=['e32'])
                    sp = spb[c2]
                    P.op('act', lambda e, sp=sp: e.activation(out=sp, in_=e32, func=AF.Ln, bias=self.cst[:, 1:2]),
                         reads=['e32'], writes=[f'spb{c2}'])
                    if sc >= 4 * T:
                        m = self.smask[sc - 4 * T]
                        P.op('pool', lambda e, sp=sp, m=m: e.tensor_tensor(out=sp, in0=sp, in1=m, op=ALU.mult),
                             reads=[f'spb{c2}'], writes=[f'spb{c2}'])
                    if sc > 0:
                        if i == 0:
                            P.op('dve', lambda e, sp=sp: e.tensor_copy(out=R, in_=sp), reads=[f'spb{c2}'], writes=['R'])
                        else:
                            P.op('dve', lambda e, sp=sp: e.tensor_tensor(out=R, in0=R, in1=sp, op=ALU.add),
                                 reads=[f'spb{c2}', 'R'], writes=['R'])
                        rb = Rb3[i % 3]
                        P.op('dve', lambda e, rb=rb: e.tensor_copy(out=rb, in_=R), reads=['R'],
                             writes=[f'Rb{i % 3}'])

                def S2(i):
                    sc = order[i]
                    c2 = info[i]
                    first = (i == 0)
                    lb = 4 + c2
                    L = self.ps[lb]
                    kk = kT[pb:pb + 64, sc * 128:(sc + 1) * 128]
                    qq = qT[pb:pb + 64, T * TB:(T + 1) * TB]
                    sp = spb[c2]
                    P.op('pe', lambda e, L=L, kk=kk, qq=qq: e.matmul(L, kk, qq, start=True, stop=False),
                         reads=[f'kT_{sc // 4}', f'qT_{T}'], writes=[f'ps{lb}'])
                    P.op('pe', lambda e, L=L, sp=sp, first=first: e.matmul(L, self.negtri, sp, start=False, stop=first),
                         reads=[f'spb{c2}'], writes=[f'ps{lb}'])
                    if not first:
                        rb = Rb3[(i - 1) % 3]
                        P.op('pe', lambda e, L=L, rb=rb: e.matmul(L, self.negones, rb, start=False, stop=True),
                             reads=[f'Rb{(i - 1) % 3}'], writes=[f'ps{lb}'])
                    ab = Ab[c2]
                    P.op('act', lambda e, ab=ab, L=L: e.activation(out=ab, in_=L, func=AF.Exp),
                         reads=[f'ps{lb}'], writes=[f'Ab{c2}'])
                    if sc >= 4 * T:
                        m = self.smask[sc - 4 * T]
                        P.op('pool', lambda e, ab=ab, m=m: e.tensor_tensor(out=ab, in0=ab, in1=m, op=ALU.mult),
                             reads=[f'Ab{c2}'], writes=[f'Ab{c2}'])

                def S3(i):
                    sc = order[i]
                    c2 = info[i]
                    first = (i == 0)
                    ab = Ab[c2]
                    vs = slice(0, 64) if hh == 0 else slice(64, 192)
                    M = 64 if hh == 0 else 128
                    P.op('pe', lambda e, O=O, sc=sc, ab=ab, vs=vs, M=M, first=first: e.matmul(
                        O[0:M, :], Vp[:, sc, vs], ab, start=first, stop=(sc == 0)),
                        reads=[f'Vp_{sc // 4}a', f'Vp_{sc // 4}b', 'Vp_c', f'Ab{c2}'], writes=[f'ps{ob}'])

                S1(0)
                for i in range(nsc):
                    if i + 1 < nsc:
                        S1(i + 1)
                    S2(i)
                    if i >= 1:
                        S3(i - 1)
                S3(nsc - 1)
                self.attn_finish(O, f'ps{ob}', hh, ychunk, T, False, rden, bcs)

        self.chk('fox')
        if True:
            for hp in range(3):
                nc_ = 128 if hp < 2 else 64
                proj_qkv(O_QB + hp * 128, O_KB + hp * 128, O_VB + hp * 128, nc_, 3 * (hp % 2))
                for hh in range(2 if hp < 2 else 1):
                    sb_attn(hh, 3 + hp)
        P.barrier()
        self.chk('sb')
        self.dsa(l, w_in)
        P.barrier()
        self.chk('dsa')
        self.merge(l, w_in)
        P.barrier()
        self.chk('merge')

    def dsa(self, l, w_in):
        P = self.P
        A = self.A
        o = [OFF_A]

        def take(n):
            r = o[0]
            o[0] += n
            assert o[0] <= SB_BYTES, o[0]
            return r
        qc = [A.carve(take(4096), 128, [S], BF16) for _ in range(3)]
        kcT = A.carve(take(4096), 128, [S], BF16)
        Vc = A.carve(take(16 * 192 * 2), 128, [16, 192], BF16)
        iq = [A.carve(take(4096), 128, [S], BF16) for _ in range(2)]
        ikT = A.carve(take(4096), 128, [S], BF16)
        wI = A.carve(take(256), 128, [16, 4], F32)
        base_d2 = o[0]
        WSL = [A.carve(take(2048), 128, [8, 128], BF16) for _ in range(4)]
        ROPE = A.carve(take(16384), 128, [2, S], F32)
        ckvn = A.carve(take(4096), 128, [S], BF16)
        t32 = [A.carve(take(2048), 128, [512], F32) for _ in range(3)]
        sqb = A.carve(take(1024), 128, [512], BF16)
        COS, SIN = ROPE[:, 0, :], ROPE[:, 1, :]
        P.op('sp', lambda e: e.dma_start(out=ROPE.rearrange("p a b -> p (a b)"), in_=self.rope),
             writes=['rope'], dma=True)
        P.op('pool', lambda e: e.memset(Vc[:, :, 64:65], 1.0), writes=['Vc_c'])
        P.op('pool', lambda e: e.memset(Vc[:, :, 65:128], 0.0), writes=['Vc_c'])
        gkv = self.small_sb[:, 192 + l:193 + l]
        gidx = self.small_sb[:, 194 + l:195 + l]
        gidx_sw = self.small_sb[:, 196 + l:197 + l]

        def rstd_of(src32, T):
            P.op('pool', lambda e: e.tensor_tensor(out=sqb, in0=src32, in1=src32, op=ALU.mult),
                 reads=['t32_0'], writes=['sqb'])
            P.op('pe', lambda e: e.matmul(self.ps[6], self.ones_128th, sqb, start=True, stop=True),
                 reads=['sqb'], writes=['ps6'])
            P.op('act', lambda e: e.activation(out=t32[1], in_=self.ps[6], func=AF.Ln, bias=self.cst[:, 0:1]),
                 reads=['ps6'], writes=['t32_1'])
            P.op('act', lambda e: e.activation(out=t32[1], in_=t32[1], func=AF.Exp, scale=-0.5),
                 reads=['t32_1'], writes=['t32_1'])

        self.load_w(WSL[0], w_in[:, :, O_CKV:O_CKV + 128], 128, 'dw0')

        def ev_ckv(T, ps, pn):
            ts = slice(T * TB, (T + 1) * TB)
            P.op('act', lambda e: e.copy(out=t32[0], in_=ps), reads=[pn], writes=['t32_0'])
            rstd_of(t32[0], T)
            P.op('dve', lambda e: e.scalar_tensor_tensor(out=ckvn[:, ts], in0=t32[0], scalar=gkv, in1=t32[1],
                                                         op0=ALU.mult, op1=ALU.mult),
                 reads=['t32_0', 't32_1'], writes=[f'ckvn_{T}'])
        self.proj_T(WSL[0], 'dw0', 128, ev_ckv)

        wkv = WSL[1]
        src_kv = self.w_kv_up[l]
        P.op('pool', lambda e: e.dma_start(out=wkv[:, 0, :], in_=src_kv), writes=['dw1a'], dma=True)
        P.op('pool', lambda e: e.dma_start(out=wkv[:, 2, 0:64], in_=src_kv[:, 0:64]), writes=['dw1b'], dma=True)
        P.op('pool', lambda e: e.dma_start(out=wkv[:, 2, 64:128], in_=src_kv[:, 0:64]), writes=['dw1c'], dma=True)

        def make_swapped(dst, src, names):
            d4 = dst.rearrange("p k (h d) -> p k h d", h=2)
            s4 = src.rearrange("p k (h d) -> p k h d", h=2)
            P.op('pool', lambda e: e.tensor_copy(out=dst, in_=src), reads=names, writes=['swp'])
            P.op('pool', lambda e: e.tensor_copy(out=d4[:, :, :, 0:8], in_=s4[:, :, :, 8:16]), reads=names, writes=['swp'])
            P.op('pool', lambda e: e.tensor_copy(out=d4[:, :, :, 8:16], in_=s4[:, :, :, 0:8]), reads=names, writes=['swp'])
        make_swapped(wkv[:, 3:4, :], wkv[:, 2:3, :], ['dw1b', 'dw1c'])

        def rope_combine(T, psn, pss, names, dst, dname, pre_n=None, pre_s=None):
            ts = slice(T * TB, (T + 1) * TB)
            P.op('dve', lambda e: e.tensor_tensor(out=t32[0], in0=psn, in1=COS[:, ts], op=ALU.mult),
                 reads=[names[0], 'rope'], writes=['t32_0'])
            P.op('dve', lambda e: e.tensor_tensor(out=t32[2], in0=pss, in1=SIN[:, ts], op=ALU.mult),
                 reads=[names[1], 'rope'], writes=['t32_2'])
            P.op('pool', lambda e: e.tensor_tensor(out=dst[:, ts], in0=t32[0], in1=t32[2], op=ALU.add),
                 reads=['t32_0', 't32_2'], writes=[dname])

        for T in range(NTB):
            ts = slice(T * TB, (T + 1) * TB)
            P.op('pe', lambda e, ts=ts: e.matmul(self.ps[0], wkv[:, 2, :], ckvn[:, ts], start=True, stop=True),
                 reads=['dw1b', 'dw1c', f'ckvn_{T}'], writes=['ps0'])
            P.op('pe', lambda e, ts=ts: e.matmul(self.ps[1], wkv[:, 3, :], ckvn[:, ts], start=True, stop=True),
                 reads=['swp', f'ckvn_{T}'], writes=['ps1'])
            rope_combine(T, self.ps[0], self.ps[1], ['ps0', 'ps1'], kcT, 'kcT')
        for g in range(4):
            b = 2 + g % 2
            ps = self.ps[b]
            for cc in range(4):
                ch = 4 * g + cc
                P.op('pe', lambda e, ps=ps, cc=cc, ch=ch: e.matmul(ps[:, cc * 128:cc * 128 + 64],
                                                                  ckvn[:, ch * 128:(ch + 1) * 128], wkv[:, 0, 64:128],
                                                                  start=True, stop=True),
                     reads=['dw1a', f'ckvn_{g}'], writes=[f'ps{b}'])
            ps3 = ps.rearrange("p (a b) -> p a b", a=4)
            P.op('dve', lambda e, ps3=ps3, g=g: e.tensor_copy(out=Vc[:, 4 * g:4 * g + 4, 0:64], in_=ps3[:, :, 0:64]),
                 reads=[f'ps{b}'], writes=['Vc_a'])
            P.op('dve', lambda e, ps3=ps3, g=g: e.tensor_copy(out=Vc[:, 4 * g:4 * g + 4, 128:192], in_=ps3[:, :, 0:64]),
                 reads=[f'ps{b}'], writes=['Vc_b'])

        def rope_pair(col_lo, col_hi, dst, dname):
            wn, ws = WSL[2], WSL[3]
            if col_hi == col_lo + 64:
                self.load_w(wn, w_in[:, :, col_lo:col_lo + 128], 128, 'dw2a')
                nm = ['dw2a']
            else:
                self.load_w(wn, w_in[:, :, col_lo:col_lo + 64], 64, 'dw2a')
                self.load_w(wn, w_in[:, :, col_hi:col_hi + 64], 64, 'dw2b', dcol=64)
                nm = ['dw2a', 'dw2b']
            make_swapped(ws, wn, nm)
            for T in range(NTB):
                ts = slice(T * TB, (T + 1) * TB)
                for kc in range(8):
                    P.op('pe', lambda e, kc=kc, ts=ts: e.matmul(self.ps[0], wn[:, kc, :], self.H[:, kc, ts],
                                                                start=(kc == 0), stop=(kc == 7)),
                         reads=nm + [f'h_{T}'], writes=['ps0'])
                for kc in range(8):
                    P.op('pe', lambda e, kc=kc, ts=ts: e.matmul(self.ps[1], ws[:, kc, :], self.H[:, kc, ts],
                                                                start=(kc == 0), stop=(kc == 7)),
                         reads=['swp', f'h_{T}'], writes=['ps1'])
                rope_combine(T, self.ps[0], self.ps[1], ['ps0', 'ps1'], dst, dname)

        rope_pair(O_QC, O_QC, qc[0], 'qc0')
        rope_pair(O_QC + 64, O_QC + 128, qc[1], 'qc1')
        rope_pair(O_QC + 192, O_QC + 256, qc[2], 'qc2')
        rope_pair(O_QI, O_QI + 64, iq[0], 'iq0')
        rope_pair(O_QI + 128, O_QI + 192, iq[1], 'iq1')

        wn, ws = WSL[2], WSL[3]
        self.load_w(wn, w_in[:, :, O_KI:O_KI + 64], 64, 'dw2a')
        self.load_w(wn, w_in[:, :, O_KI:O_KI + 64], 64, 'dw2b', dcol=64)
        make_swapped(ws, wn, ['dw2a', 'dw2b'])
        for T in range(NTB):
            ts = slice(T * TB, (T + 1) * TB)
            for kc in range(8):
                P.op('pe', lambda e, kc=kc, ts=ts: e.matmul(self.ps[0], wn[:, kc, :], self.H[:, kc, ts],
                                                            start=(kc == 0), stop=(kc == 7)),
                     reads=['dw2a', 'dw2b', f'h_{T}'], writes=['ps0'])
            for kc in range(8):
                P.op('pe', lambda e, kc=kc, ts=ts: e.matmul(self.ps[1], ws[:, kc, :], self.H[:, kc, ts],
                                                            start=(kc == 0), stop=(kc == 7)),
                     reads=['swp', f'h_{T}'], writes=['ps1'])
            P.op('act', lambda e: e.copy(out=t32[0], in_=self.ps[0]), reads=['ps0'], writes=['t32_0'])
            rstd_of(t32[0], T)
            P.op('dve', lambda e: e.scalar_tensor_tensor(out=t32[0], in0=t32[0], scalar=gidx, in1=t32[1],
                                                         op0=ALU.mult, op1=ALU.mult),
                 reads=['t32_0', 't32_1'], writes=['t32_0'])
            P.op('dve', lambda e: e.scalar_tensor_tensor(out=t32[2], in0=self.ps[1], scalar=gidx_sw, in1=t32[1],
                                                         op0=ALU.mult, op1=ALU.mult),
                 reads=['ps1', 't32_1'], writes=['t32_2'])
            P.op('dve', lambda e, ts=ts: e.tensor_tensor(out=t32[0], in0=t32[0], in1=COS[:, ts], op=ALU.mult),
                 reads=['t32_0', 'rope'], writes=['t32_0'])
            P.op('dve', lambda e, ts=ts: e.tensor_tensor(out=t32[2], in0=t32[2], in1=SIN[:, ts], op=ALU.mult),
                 reads=['t32_2', 'rope'], writes=['t32_2'])
            P.op('pool', lambda e, ts=ts: e.tensor_tensor(out=ikT[:, ts], in0=t32[0], in1=t32[2], op=ALU.add),
                 reads=['t32_0', 't32_2'], writes=['ikT'])

        self.load_w(WSL[0], w_in[:, :, O_WI:O_WI + 4], 4, 'dw0')
        self.proj_tok(WSL[0], 'dw0', 4, lambda g, ps3, pn: P.op(
            'dve', lambda e: e.tensor_copy(out=wI[:, 4 * g:4 * g + 4, :], in_=ps3[:, :, 0:4]),
            reads=[pn], writes=['wI']), banks=(2, 3))
        P.barrier()

        o[0] = base_d2
        sc32 = A.carve(take(8192), 128, [S], F32)
        mask = A.carve(take(4096), 128, [S], BF16)
        maskT = A.carve(take(4096), 128, [16, 128], BF16)
        Pt = [A.carve(take(256), 128, [128], BF16) for _ in range(4)]
        tt = [A.carve(take(2048), 128, [512], F32) for _ in range(2)]
        sm = A.carve(take(256), 128, [64], F32)
        mx, mn, d0, mid, cntc, tmp1, theta = [sm[:, i:i + 1] for i in range(7)]
        halfs = sm[:, 16:32]
        ps7b = self.ps[7].bitcast(BF16)
        maskT2 = [maskT, A.carve(take(4096), 128, [16, 128], BF16)]
        fin = [[A.carve(take(512), 128, [128], F32) for _ in range(3)] for _ in range(6)]
        st = {'cnt': 0, 'ocnt': 0, 'icnt': 0}

        def A1(tb):
            L = (tb + 1) * 128
            qs = slice(tb * 128, (tb + 1) * 128)
            for j in range((L + 511) // 512):
                w = min(512, L - 512 * j)
                cs = slice(512 * j, 512 * j + w)
                for h in range(NIH):
                    pb = 64 * (h % 2)
                    zb = st['icnt'] % 2
                    k2 = st['icnt'] % 2
                    st['icnt'] += 1
                    Z = self.ps[zb]
                    P.op('pe', lambda e, Z=Z, h=h, pb=pb, cs=cs, w=w, qs=qs: e.matmul(
                        Z[:, 0:w], iq[h // 2][pb:pb + 64, qs], ikT[pb:pb + 64, cs], start=True, stop=True),
                        reads=[f'iq{h // 2}', 'ikT'], writes=[f'ps{zb}'])
                    if h == 0:
                        P.op('dve', lambda e, Z=Z, cs=cs, w=w, tb=tb: e.tensor_scalar(
                            out=sc32[:, cs], in0=Z[:, 0:w], scalar1=0.0, scalar2=wI[:, tb, 0:1],
                            op0=ALU.max, op1=ALU.mult), reads=[f'ps{zb}', 'wI'], writes=[f'sc_{j}'])
                    else:
                        P.op('dve', lambda e, Z=Z, w=w, tb=tb, h=h, k2=k2: e.tensor_scalar(
                            out=tt[k2][:, 0:w], in0=Z[:, 0:w], scalar1=0.0, scalar2=wI[:, tb, h:h + 1],
                            op0=ALU.max, op1=ALU.mult), reads=[f'ps{zb}', 'wI'], writes=[f'tt{k2}'])
                        P.op('pool', lambda e, cs=cs, w=w, k2=k2: e.tensor_tensor(
                            out=sc32[:, cs], in0=sc32[:, cs], in1=tt[k2][:, 0:w], op=ALU.add),
                            reads=[f'tt{k2}', f'sc_{j}'], writes=[f'sc_{j}'])
            scn = [f'sc_{j}' for j in range((L + 511) // 512)]
            if tb >= 2:
                P.op('dve', lambda e, L=L: e.tensor_reduce(out=mx, in_=sc32[:, 0:L], axis=mybir.AxisListType.X,
                                                           op=ALU.max), reads=scn, writes=['mx'])
                P.op('dve', lambda e, L=L: e.tensor_reduce(out=mn, in_=sc32[:, 0:L], axis=mybir.AxisListType.X,
                                                           op=ALU.min), reads=scn, writes=['mn'])
            P.op('pool', lambda e, L=L: e.memset(sc32[0:64, L - 64:L], -1e30), reads=scn + ['mx', 'mn'],
                 writes=[scn[-1]])
            return scn

        def Abis(tb, scn):
            L = (tb + 1) * 128
            if tb >= 2:
                P.op('dve', lambda e: e.tensor_tensor(out=d0, in0=mx, in1=mn, op=ALU.subtract),
                     reads=['mx', 'mn'], writes=['d0'])
                P.op('dve', lambda e: e.tensor_scalar(out=halfs, in0=self.pow2, scalar1=d0, scalar2=None,
                                                      op0=ALU.mult), reads=['d0'], writes=['halfs'])
                P.op('dve', lambda e: e.tensor_tensor(out=mid, in0=mn, in1=halfs[:, 0:1], op=ALU.add),
                     reads=['mn', 'halfs'], writes=['mid'])
                for k in range(NBIS):
                    P.op('dve', lambda e, L=L: e.tensor_scalar(out=mask[:, 0:L], in0=sc32[:, 0:L], scalar1=mid,
                                                               scalar2=None, op0=ALU.is_ge, op1=ALU.add,
                                                               accum_out=cntc),
                         reads=scn + ['mid'], writes=['mask', 'cntc'])
                    P.op('dve', lambda e: e.tensor_scalar(out=tmp1, in0=cntc, scalar1=256.0, scalar2=0.5,
                                                          op0=ALU.is_ge, op1=ALU.subtract),
                         reads=['cntc'], writes=['tmp1'])
                    P.op('dve', lambda e, k=k: e.scalar_tensor_tensor(out=mid, in0=tmp1, scalar=halfs[:, k:k + 1],
                                                                      in1=mid, op0=ALU.mult, op1=ALU.add),
                         reads=['tmp1', 'halfs', 'mid'], writes=['mid'])
                P.op('dve', lambda e: e.scalar_tensor_tensor(out=theta, in0=halfs[:, NBIS - 1:NBIS], scalar=-0.5, in1=mid,
                                                             op0=ALU.mult, op1=ALU.add),
                     reads=['mid', 'halfs'], writes=['theta'])
            else:
                P.op('dve', lambda e: e.memset(theta, -1e29), writes=['theta'])

        def A2(tb, scn):
            L = (tb + 1) * 128
            mT = maskT2[tb % 2]
            P.op('dve', lambda e, L=L: e.tensor_scalar(out=mask[:, 0:L], in0=sc32[:, 0:L], scalar1=theta,
                                                       scalar2=None, op0=ALU.is_ge),
                 reads=scn + ['theta'], writes=['mask'])
            for g0 in range(0, tb + 1, 8):
                n = min(8, tb + 1 - g0)
                for i in range(n):
                    sc = g0 + i
                    P.op('pe', lambda e, i=i, sc=sc: e.transpose(ps7b[:, i * 128:(i + 1) * 128],
                                                                 mask[:, sc * 128:(sc + 1) * 128], self.ident),
                         reads=['mask'], writes=['ps7'])
                P.op('act', lambda e, g0=g0, n=n, mT=mT: e.copy(out=mT[:, g0:g0 + n, :].rearrange("p a b -> p (a b)"),
                                                                in_=ps7b[:, 0:n * 128]),
                     reads=['ps7'], writes=[f'maskT{tb % 2}'])

        def Battn(tb):
            qs = slice(tb * 128, (tb + 1) * 128)
            mT = maskT2[tb % 2]
            for hc in range(NH_C):
                i = (hc + 1) // 2
                hh = (hc + 1) % 2
                pb = 64 * hh
                ob = 4 + st['ocnt'] % 2
                fb = st['ocnt'] % 6
                st['ocnt'] += 1
                O = self.ps[ob]
                dinfo = {}

                def D1(sc):
                    zb = 2 + st['cnt'] % 2
                    k = st['cnt'] % 4
                    st['cnt'] += 1
                    dinfo[sc] = k
                    Z = self.ps[zb]
                    P.op('pe', lambda e, Z=Z, sc=sc, i=i, pb=pb, qs=qs: e.matmul(
                        Z[:, 0:128], kcT[pb:pb + 64, sc * 128:(sc + 1) * 128], qc[i][pb:pb + 64, qs],
                        start=True, stop=True), reads=['kcT', f'qc{i}'], writes=[f'ps{zb}'])
                    Ptk = Pt[k]
                    P.op('act', lambda e, Ptk=Ptk, Z=Z: e.activation(out=Ptk, in_=Z[:, 0:128], func=AF.Exp, scale=0.125),
                         reads=[f'ps{zb}'], writes=[f'dPt{k}'])
                    P.op('pool', lambda e, Ptk=Ptk, sc=sc, mT=mT: e.tensor_tensor(out=Ptk, in0=Ptk, in1=mT[:, sc, :],
                                                                                  op=ALU.mult),
                         reads=[f'dPt{k}', f'maskT{tb % 2}'], writes=[f'dPt{k}'])

                def D2(sc):
                    k = dinfo[sc]
                    Ptk = Pt[k]
                    vs = slice(0, 65) if hh == 0 else slice(64, 192)
                    M = 65 if hh == 0 else 128
                    P.op('pe', lambda e, O=O, sc=sc, Ptk=Ptk, vs=vs, M=M, tb=tb: e.matmul(
                        O[0:M, 0:128], Vc[:, sc, vs], Ptk, start=(sc == 0), stop=(sc == tb)),
                        reads=['Vc_a', 'Vc_b', 'Vc_c', f'dPt{k}'], writes=[f'ps{ob}'])

                D1(0)
                for sc in range(tb + 1):
                    if sc + 1 < tb + 1:
                        D1(sc + 1)
                    D2(sc)
                rd, bc_, osb = fin[fb]
                p = 64 if hh == 0 else 0
                P.op('act', lambda e, rd=rd, O=O, p=p: e.activation(out=rd[:, :], in_=O[:, 0:128], func=AF.Ln),
                     reads=[f'ps{ob}'], writes=[f'frd{fb}'])
                P.op('act', lambda e, rd=rd, p=p: e.activation(out=rd[:, :], in_=rd[:, :], func=AF.Exp, scale=-1.0),
                     reads=[f'frd{fb}'], writes=[f'frd{fb}'])
                BC = self.ps[6][:, 0:128]
                P.op('pe', lambda e, rd=rd, p=p, BC=BC: e.matmul(BC, self.ones_f32[p:p + 1, :], rd[p:p + 1, :],
                                                                 start=True, stop=True),
                     reads=[f'frd{fb}'], writes=['ps6'])
                P.op('act', lambda e, bc_=bc_, pb=pb, BC=BC: e.copy(out=bc_[pb:pb + 64, :], in_=BC[pb:pb + 64, :]),
                     reads=['ps6'], writes=[f'fbc{fb}'])
                P.op('act', lambda e, osb=osb, pb=pb, O=O: e.copy(out=osb[pb:pb + 64, :], in_=O[pb:pb + 64, 0:128]),
                     reads=[f'ps{ob}'], writes=[f'fos{fb}'])
                ydst = self.Y[pb:pb + 64, 5 + i, qs]
                P.op('dve', lambda e, ydst=ydst, osb=osb, bc_=bc_, pb=pb: e.tensor_tensor(
                    out=ydst, in0=osb[pb:pb + 64, :], in1=bc_[pb:pb + 64, :], op=ALU.mult),
                    reads=[f'fos{fb}', f'fbc{fb}'], writes=[f'y{5 + i}_{hh}_{tb}_128'])

        scn_cur = A1(0)
        Abis(0, scn_cur)
        A2(0, scn_cur)
        for tb in range(NQB):
            if tb + 1 < NQB:
                scn_nxt = A1(tb + 1)
                Abis(tb + 1, scn_nxt)
            Battn(tb)
            if tb + 1 < NQB:
                A2(tb + 1, scn_nxt)

    def merge(self, l, w_in):
        P = self.P
        A = self.A
        o = [OFF_A]

        def take(n):
            r = o[0]
            o[0] += n
            assert o[0] <= SB_BYTES, o[0]
            return r
        merged = A.carve(take(8 * S * 2), 128, [8, S], BF16)
        wg = [A.carve(take(6144), 128, [8, 384], BF16) for _ in range(2)]
        wu = [A.carve(take(9 * 256), 128, [9, 128], BF16) for _ in range(2)]
        wo = [A.carve(take(2048), 128, [8, 128], BF16) for _ in range(2)]
        sg = [A.carve(take(2048), 128, [512], F32) for _ in range(3)]
        mm = [A.carve(take(2048), 128, [512], F32) for _ in range(3)]
        ufox = self.w_up_fox[l].rearrange("(j p) n -> p j n", p=128)
        usb = self.w_up_sb[l]
        udsa = self.w_up_dsa[l]
        wout = self.w_out[l].rearrange("(kc p) n -> p kc n", p=128)
        for c in range(8):
            k = c % 2
            cs = slice(c * 128, (c + 1) * 128)
            for b in range(3):
                src = w_in[:, :, O_G + b * D + c * 128:O_G + b * D + (c + 1) * 128]
                dst = wg[k][:, :, b * 128:(b + 1) * 128]
                P.op('pool', lambda e, src=src, dst=dst: e.dma_start(out=dst, in_=src), writes=[f'wg{k}_{b}'], dma=True)
            W = wu[k]
            dl = [
                (W[:, 0:3, :], ufox[:, :, cs]),
                (W[:, 3:5, :], usb[0:256, :].rearrange("(j p) n -> p j n", p=128)[:, :, cs]),
                (W[0:64, 5, :], usb[256:320, cs]),
                (W[64:128, 6, :], udsa[0:64, cs]),
                (W[:, 7:9, :], udsa[64:320, :].rearrange("(j p) n -> p j n", p=128)[:, :, cs]),
            ]
            for i, (dst, src) in enumerate(dl):
                P.op('pool', lambda e, src=src, dst=dst: e.dma_start(out=dst, in_=src), writes=[f'wu{k}_{i}'], dma=True)
            wun = [f'wu{k}_{i}' for i in range(5)]
            for T in range(NTB):
                ts = slice(T * TB, (T + 1) * TB)
                yn = [nm for nm in self.P.bufs if nm.startswith('y') and nm.endswith(f'_{T}')]
                for b in range(3):
                    G = self.ps[b]
                    for kc in range(8):
                        P.op('pe', lambda e, G=G, kc=kc, b=b, ts=ts, k=k: e.matmul(
                            G, wg[k][:, kc, b * 128:(b + 1) * 128], self.H[:, kc, ts], start=(kc == 0), stop=(kc == 7)),
                            reads=[f'wg{k}_{b}', f'h_{T}'], writes=[f'ps{b}'])
                    U = self.ps[3 + b]
                    if b == 0:
                        terms = [(W[:, j, :], self.Y[:, j, ts]) for j in range(3)]
                    elif b == 1:
                        terms = [(W[:, 3, :], self.Y[:, 3, ts]), (W[:, 4, :], self.Y[:, 4, ts]),
                                 (W[0:64, 5, :], self.Y[0:64, 5, ts])]
                    else:
                        terms = [(W[64:128, 6, :], self.Y[64:128, 5, ts]), (W[:, 7, :], self.Y[:, 6, ts]),
                                 (W[:, 8, :], self.Y[:, 7, ts])]
                    for ti, (lh, rh) in enumerate(terms):
                        P.op('pe', lambda e, U=U, lh=lh, rh=rh, ti=ti: e.matmul(U, lh, rh, start=(ti == 0), stop=(ti == 2)),
                             reads=wun + ['yall'], writes=[f'ps{3 + b}'])
                    P.op('act', lambda e, G=G, b=b: e.activation(out=sg[b], in_=G, func=AF.Sigmoid),
                         reads=[f'ps{b}'], writes=[f'sg{b}'])
                    if self.stage.rstrip('XYH') == 'mgd':
                        P.op('dve', lambda e, b=b, ts=ts: e.tensor_copy(out=self.X[:, b, ts], in_=sg[b]),
                             reads=[f'sg{b}'], writes=[f'x{b}_{T}'])
                        P.op('dve', lambda e, b=b, ts=ts, U=U: e.tensor_copy(out=self.X[:, 3 + b, ts], in_=U),
                             reads=[f'ps{3 + b}'], writes=[f'x{3 + b}_{T}'])
                    P.op('dve', lambda e, U=U, b=b: e.tensor_tensor(out=mm[b], in0=sg[b], in1=U, op=ALU.mult),
                         reads=[f'sg{b}', f'ps{3 + b}'], writes=[f'mm{b}'])
                P.op('pool', lambda e: e.tensor_tensor(out=mm[0], in0=mm[0], in1=mm[1], op=ALU.add),
                     reads=['mm0', 'mm1'], writes=['mm0'])
                P.op('pool', lambda e, c=c, ts=ts: e.tensor_tensor(out=merged[:, c, ts], in0=mm[0], in1=mm[2], op=ALU.add),
                     reads=['mm0', 'mm2'], writes=[f'mg_{T}'])
                if self.stage.rstrip('XYH') == 'mgd':
                    P.op('dve', lambda e, ts=ts: e.tensor_copy(out=self.X[:, 6, ts], in_=mm[0]), reads=['mm0'], writes=[f'x6_{T}'])
                    P.op('dve', lambda e, ts=ts, c=c: e.tensor_copy(out=self.X[:, 7, ts], in_=merged[:, c, ts]), reads=[f'mg_{T}'], writes=[f'x7_{T}'])
            self.chk('mgd')
        if self.stage.rstrip('XYH') == 'mg':
            for c in range(8):
                P.op('dve', lambda e, c=c: e.tensor_copy(out=self.X[:, c, :], in_=merged[:, c, :]),
                     reads=[f'mg_{T}' for T in range(NTB)], writes=[f'x{c}_{T}' for T in range(NTB)])
            self.chk('mg')
        for c2 in range(8):
            k = c2 % 2
            src = wout[:, :, c2 * 128:(c2 + 1) * 128]
            dst = wo[k]
            P.op('pool', lambda e, src=src, dst=dst: e.dma_start(out=dst, in_=src), writes=[f'wo{k}'], dma=True)
            for T in range(NTB):
                ts = slice(T * TB, (T + 1) * TB)
                pb_ = 6 + (c2 * NTB + T) % 2
                po = self.ps[pb_]
                for kc in range(8):
                    P.op('pe', lambda e, po=po, kc=kc, dst=dst, ts=ts: e.matmul(
                        po, dst[:, kc, :], merged[:, kc, ts], start=(kc == 0), stop=(kc == 7)),
                        reads=[f'wo{k}', f'mg_{T}'], writes=[f'ps{pb_}'])
                xd = self.X[:, c2, ts]
                P.op('dve', lambda e, xd=xd, po=po: e.tensor_tensor(out=xd, in0=xd, in1=po, op=ALU.add),
                     reads=[f'ps{pb_}', f'x{c2}_{T}'], writes=[f'x{c2}_{T}'])

    def final(self, s):
        P = self.P
        A = self.A
        gi = 3 * DEPTH
        o = OFF_A
        stg = [A.carve(o + k * 16384, 128, [8, TB], F32) for k in range(2)]; o += 32768
        tmp_off = o
        for t in range(NTB):
            k = t % 2
            if self.stage[-1] in 'XYH':
                SRC = {'X': self.X, 'Y': self.Y, 'H': self.H}[self.stage[-1]]
                for c in range(8):
                    P.op('dve', lambda e, c=c, t=t, k=k: e.tensor_copy(out=stg[k][:, c, :], in_=SRC[:, c, t * TB:(t + 1) * TB]),
                         reads=[f'x{c}_{t}'], writes=[f'stg{k}_{c}'])
            else:
                self.rmsnorm(gi, lambda c, t, k=k: (stg[k][:, c, :], f'stg{k}_{c}'), tmp_off, t_list=[t])
            for c in range(8):
                dst = self.outT[s, c * 128:(c + 1) * 128, t * TB:(t + 1) * TB]
                src = stg[k][:, c, :]
                P.op('sp', lambda e, src=src, dst=dst: e.dma_start(out=dst, in_=src),
                     reads=[f'stg{k}_{c}'], dma=True)


def pack_gains(inp):
    g = np.zeros((128, NGAIN), np.float32)
    for l in range(DEPTH):
        for k, name in enumerate(("g_ffn1", "g_mix", "g_ffn2")):
            g[:, (l * 3 + k) * 8:(l * 3 + k + 1) * 8] = np.asarray(inp[name][l], np.float32).reshape(8, 128).T
    g[:, 3 * DEPTH * 8:] = np.asarray(inp["g_final"], np.float32).reshape(8, 128).T
    return g


def rope_consts():
    pos = np.arange(S, dtype=np.float32)
    inv = (np.float32(500000.0) ** (-np.arange(0, 16, 2, dtype=np.float32) / np.float32(16))).astype(np.float32)
    ang = pos[None, :] * inv[:, None]
    cos, sin = np.cos(ang).astype(np.float32), np.sin(ang).astype(np.float32)
    r = np.zeros((128, 2, S), np.float32)
    r[:, 0, :] = 1.0
    for p in range(128):
        d = p % 64
        if d < 16:
            r[p, 0] = cos[d % 8]
            r[p, 1] = -sin[d % 8] if d < 8 else sin[d % 8]
    return r.reshape(128, 2 * S)


def const_tables():
    i = np.arange(128)
    cbf = np.zeros((128, NCBF), np.float32)
    cbf[:, 0:128] = (i[:, None] <= i[None, :])
    for k in range(4):
        m = np.zeros((128, 4, 128), np.float32)
        m[:, k, :] = (i[:, None] < i[None, :])
        m[:, k + 1:, :] = 1.0
        cbf[:, 128 + 512 * k:128 + 512 * (k + 1)] = m.reshape(128, 512)
    cbf[:, 2176:2304] = -(i[:, None] >= i[None, :]).astype(np.float32)
    cbf[:, 2304:2432] = -1.0
    cbf[:, 2432:2560] = np.eye(128, dtype=np.float32)
    cbf[:, 2560:2688] = 1.0 / 1024
    cbf[:, 2688:2816] = 1.0 / 128
    cbf[:, 2816:2944] = 1.0 / 64
    cf = np.zeros((128, NCF32), np.float32)
    cf[:, 0:128] = (i[:, None] <= i[None, :])
    cf[:, 128:256] = 1.0
    cf[:, 256:272] = 2.0 ** -(np.arange(16) + 1.0)
    return cbf, cf


def make_in_maps(inp, n_cores, n_seq):
    x = np.asarray(inp["x"], np.float32)
    gains = pack_gains(inp)
    smallp = np.zeros((128, 256), np.float32)
    for l in range(DEPTH):
        smallp[:, l * 96:(l + 1) * 96] = np.tile(np.asarray(inp["b_forget"][l], np.float32), 16)[None, :]
        smallp[:, 192 + l] = np.asarray(inp["g_kv_latent"][l], np.float32)
        smallp[:64, 194 + l] = np.asarray(inp["g_idx_k"][l], np.float32)
        smallp[64:, 194 + l] = np.asarray(inp["g_idx_k"][l], np.float32)
        gsw = np.asarray(inp["g_idx_k"][l], np.float32).copy()
        gsw[0:8], gsw[8:16] = gsw[8:16].copy(), gsw[0:8].copy()
        smallp[:64, 196 + l] = gsw
        smallp[64:, 196 + l] = gsw
    rope = rope_consts()
    cbf, cf32 = const_tables()
    maps = []
    shared = {
        "w_ffn1_gu": np.asarray(inp["w_ffn1_gu"], np.float32), "w_ffn2_gu": np.asarray(inp["w_ffn2_gu"], np.float32),
        "w_ffn1_down": np.asarray(inp["w_ffn1_down"], np.float32),
        "w_ffn2_down": np.asarray(inp["w_ffn2_down"], np.float32),
        "w_in": np.asarray(inp["w_in"], np.float32), "w_kv_up": np.asarray(inp["w_kv_up"], np.float32),
        "w_up_fox": np.asarray(inp["w_up_fox"], np.float32), "w_up_sb": np.asarray(inp["w_up_sb"], np.float32),
        "w_up_dsa": np.asarray(inp["w_up_dsa"], np.float32), "w_out": np.asarray(inp["w_out"], np.float32),
        "gains": gains, "smallp": smallp, "rope": rope, "cbf": cbf, "cf32": cf32,
    }
    for cidx in range(n_cores):
        xs = x[cidx * n_seq:(cidx + 1) * n_seq]
        m = dict(shared)
        m["xT"] = np.ascontiguousarray(xs.transpose(0, 2, 1))
        maps.append(m)
    return maps


def kernel(**inputs):
    n_cores, n_seq = 8, 2
    mdl = Model(n_seq=n_seq)
    nc = mdl.build()
    maps = make_in_maps(inputs, n_cores, n_seq)
    res = run_bass_kernel_spmd(nc, maps, core_ids=list(range(n_cores)))
    outs = [r["outT"].transpose(0, 2, 1) for r in res.results]
    return np.ascontiguousarray(np.concatenate(outs, axis=0)).astype(np.float32)
```

```python
import numpy as np
import concourse.bass as bass
import concourse.mybir as mybir
from concourse.bass_utils import run_bass_kernel_spmd

F32 = mybir.dt.float32
BF16 = mybir.dt.bfloat16
U8 = mybir.dt.uint8
AF = mybir.ActivationFunctionType
ALU = mybir.AluOpType

D = 1024
S = 2048
DEPTH = 2
DFF = 2816
NFF = DFF // 128
HD = 64
NH_A, NH_B, NH_C = 6, 5, 5
W_A, W_B, W_C = 384, 320, 320
KVL = 128
NIH = 4
D_IN = 5962
EPS = 1e-6
TB = 512
NTB = S // TB
NQB = S // 128

O_QA = 0
O_KA = O_QA + W_A
O_VA = O_KA + W_A
O_FA = O_VA + W_A
O_QB = O_FA + NH_A
O_KB = O_QB + W_B
O_VB = O_KB + W_B
O_QC = O_VB + W_B
O_CKV = O_QC + W_C
O_QI = O_CKV + KVL
O_KI = O_QI + NIH * 64
O_WI = O_KI + 64
O_G = O_WI + NIH
assert O_G + 3 * D == D_IN

EPOCH = 4096
ENGS = ['pe', 'act', 'dve', 'pool', 'sp']


class Buf:
    __slots__ = ('name', 'lw', 'rd')

    def __init__(self, name):
        self.name = name
        self.lw = None
        self.rd = {}


class Prog:
    NRING = 8

    def __init__(self):
        self.ops = {e: [] for e in ENGS}
        self.waited = {e: {} for e in ENGS}
        self.ndma = {e: 0 for e in ENGS}
        self.bufs = {}

    def buf(self, name):
        b = self.bufs.get(name)
        if b is None:
            b = Buf(name)
            self.bufs[name] = b
        return b

    def _tok(self, names):
        return [self.buf(n) if isinstance(n, str) else n for n in names]

    def op(self, eng, emit, reads=(), writes=(), dma=False):
        reads = self._tok(reads)
        writes = self._tok(writes)
        idx = len(self.ops[eng])
        deps = set()
        for b in reads:
            if b.lw is not None:
                deps.add(b.lw)
        for b in writes:
            if b.lw is not None:
                deps.add(b.lw)
            for t in b.rd.values():
                deps.add(t)
        if dma:
            d = self.ndma[eng]
            self.ndma[eng] += 1
            tok = ('d', eng, d)
            if d >= self.NRING:
                deps.add(('d', eng, d - self.NRING))
        else:
            tok = ('c', eng, idx)
        waits = []
        w = self.waited[eng]
        for t in deps:
            if t[0] == 'c':
                _, e, i = t
                if e == eng and eng == 'pe':
                    continue
                if w.get(('c', e), -1) >= i:
                    continue
                w[('c', e)] = i
                self.ops[e][i][2] = True
                waits.append(t)
            else:
                _, q, d0 = t
                key = ('d', q, d0 % self.NRING)
                if w.get(key, -1) >= d0:
                    continue
                w[key] = d0
                waits.append(t)
        self.ops[eng].append([emit, waits, False, tok if dma else None])
        rkey = (tok[0], tok[1]) if tok[0] == 'c' else (tok[0], tok[1], tok[2] % self.NRING)
        for b in reads:
            b.rd[rkey] = tok
        for b in writes:
            b.lw = tok
            b.rd = {}
        return tok

    def barrier(self):
        last = {}
        for e in ENGS:
            for i in range(len(self.ops[e]) - 1, -1, -1):
                o = self.ops[e][i]
                if o[0] is not None and o[3] is None:
                    last[e] = i
                    break
        for eng in ENGS:
            waits = []
            w = self.waited[eng]
            for e, i in last.items():
                if e == eng:
                    continue
                if w.get(('c', e), -1) >= i:
                    continue
                w[('c', e)] = i
                self.ops[e][i][2] = True
                waits.append(('c', e, i))
            for q in ENGS:
                n = self.ndma[q]
                for d0 in range(max(0, n - self.NRING), n):
                    key = ('d', q, d0 % self.NRING)
                    if w.get(key, -1) >= d0:
                        continue
                    w[key] = d0
                    waits.append(('d', q, d0))
            self.ops[eng].append([None, waits, False, None])
        for b in self.bufs.values():
            b.lw = None
            b.rd = {}

    def wait_all_dma(self, eng):
        waits = []
        for q in ENGS:
            n = self.ndma[q]
            for d in range(max(0, n - self.NRING), n):
                waits.append(('d', q, d))
        self.ops[eng].append([None, waits, False, None])

    def emit(self, nc, block_cm):
        cnt = {}
        nsem = {}
        for e in ENGS:
            c = 0
            arr = []
            for o in self.ops[e]:
                if o[2]:
                    c += 1
                arr.append(c)
            cnt[e] = arr
            nsem[e] = (c + EPOCH - 1) // EPOCH
        csem = {e: [nc.alloc_semaphore(name=f"c_{e}_{k}") for k in range(nsem[e])] for e in ENGS}
        dsem = {e: [nc.alloc_semaphore(name=f"d_{e}_{k}") for k in range(self.NRING)]
                for e in ENGS if self.ndma[e] > 0}

        def resolve(t):
            if t[0] == 'c':
                _, e, i = t
                c = cnt[e][i]
                return csem[e][(c - 1) // EPOCH], (c - 1) % EPOCH + 1
            _, q, d0 = t
            return dsem[q][d0 % self.NRING], 16 * (d0 // self.NRING + 1)

        prog = self

        def run(e, eng):
            for k, (emit, waits, marked, dtok) in enumerate(prog.ops[e]):
                for t in waits:
                    s, v = resolve(t)
                    eng.wait_ge(s, v)
                if emit is None:
                    continue
                ins = emit(eng)
                if dtok is not None:
                    s, _ = resolve(dtok)
                    ins.then_inc(s, 16)
                elif marked:
                    c = cnt[e][k]
                    ins.then_inc(csem[e][(c - 1) // EPOCH], 1)

        with block_cm as block:
            @block.tensor
            def _(eng):
                run('pe', eng)

            @block.scalar
            def _(eng):
                run('act', eng)

            @block.vector
            def _(eng):
                run('dve', eng)

            @block.gpsimd
            def _(eng):
                run('pool', eng)

            @block.sync
            def _(eng):
                run('sp', eng)


class Arena:
    def __init__(self, ap_u8, nbytes):
        self.ap = ap_u8
        self.nbytes = nbytes

    def carve(self, off, parts, free_shape, dtype, pbase=0):
        esz = 4 if dtype == F32 else 2
        n = 1
        for s in free_shape:
            n *= s
        assert off % 4 == 0 and off + n * esz <= self.nbytes, (off, n * esz, self.nbytes)
        a = self.ap[pbase:pbase + parts, off:off + n * esz].bitcast(dtype)
        if len(free_shape) == 2:
            a = a.rearrange('p (a b) -> p a b', a=free_shape[0])
        elif len(free_shape) == 3:
            a = a.rearrange('p (a b c) -> p a b c', a=free_shape[0], b=free_shape[1])
        return a


OFF_X = 0
OFF_H = OFF_X + 8 * S * 4
OFF_C = OFF_H + 8 * S * 2
OFF_Y = OFF_C + 10240
OFF_A = OFF_Y + 8 * S * 2
SB_BYTES = 212800
ARENA_BYTES = SB_BYTES - OFF_A
NGAIN = 8 * (3 * DEPTH + 1)
NCBF = 2944
NCF32 = 272
NBIS = 14


class _Stop(Exception):
    pass


class Model:
    def __init__(self, n_seq=2, depth=DEPTH, stage='full'):
        self.n_seq = n_seq
        self.depth = depth
        self.stage = stage
        nc = bass.Bass("TRN2", target_bir_lowering=False)
        self.nc = nc
        dt = nc.dram_tensor
        self.xT = dt("xT", [n_seq, D, S], F32, kind="ExternalInput").ap()
        self.outT = dt("outT", [n_seq, D, S], F32, kind="ExternalOutput").ap()
        self.w_gu = [dt(f"w_ffn{i}_gu", [DEPTH, D, 2 * DFF], F32, kind="ExternalInput").ap() for i in (1, 2)]
        self.w_dn = [dt(f"w_ffn{i}_down", [DEPTH, DFF, D], F32, kind="ExternalInput").ap() for i in (1, 2)]
        self.w_in = dt("w_in", [DEPTH, D, D_IN], F32, kind="ExternalInput").ap()
        self.w_kv_up = dt("w_kv_up", [DEPTH, KVL, 2 * HD], F32, kind="ExternalInput").ap()
        self.w_up_fox = dt("w_up_fox", [DEPTH, W_A, D], F32, kind="ExternalInput").ap()
        self.w_up_sb = dt("w_up_sb", [DEPTH, W_B, D], F32, kind="ExternalInput").ap()
        self.w_up_dsa = dt("w_up_dsa", [DEPTH, W_C, D], F32, kind="ExternalInput").ap()
        self.w_out = dt("w_out", [DEPTH, D, D], F32, kind="ExternalInput").ap()
        self.gains = dt("gains", [128, NGAIN], F32, kind="ExternalInput").ap()
        self.smallp = dt("smallp", [128, 256], F32, kind="ExternalInput").ap()
        self.rope = dt("rope", [128, 2 * S], F32, kind="ExternalInput").ap()
        self.cbf = dt("cbf", [128, NCBF], F32, kind="ExternalInput").ap()
        self.cf32 = dt("cf32", [128, NCF32], F32, kind="ExternalInput").ap()
        self.P = Prog()

    def build(self):
        nc = self.nc
        P = self.P
        with nc.sbuf_tensor("sb", [128, SB_BYTES], U8) as sb:
            self.psum_cms = [nc.psum_tensor(f"ps{k}", [128, 512], F32) for k in range(8)]
            self.ps = [cm.__enter__()[:] for cm in self.psum_cms]
            A = Arena(sb, SB_BYTES)
            self.A = A
            self.X = A.carve(OFF_X, 128, [8, S], F32)
            self.H = A.carve(OFF_H, 128, [8, S], BF16)
            self.Y = A.carve(OFF_Y, 128, [8, S], BF16)
            o = OFF_C
            self.gain_sb = A.carve(o, 128, [NGAIN], F32); o += NGAIN * 4
            self.small_sb = A.carve(o, 128, [256], F32); o += 1024
            self.cbf_sb = A.carve(o, 128, [NCBF], BF16); o += NCBF * 2
            self.cf32_sb = A.carve(o, 128, [NCF32], F32); o += NCF32 * 4
            cb = self.cbf_sb
            self.tri_incl = cb[:, 0:128]
            self.smask = [cb[:, 128 + 512 * k: 128 + 512 * (k + 1)] for k in range(4)]
            self.negtri = cb[:, 2176:2304]
            self.negones = cb[:, 2304:2432]
            self.ident = cb[:, 2432:2560]
            self.ones_mean = cb[:, 2560:2688]
            self.ones_128th = cb[:, 2688:2816]
            self.ones_64th = cb[:, 2816:2944]
            self.tri_f32 = self.cf32_sb[:, 0:128]
            self.ones_f32 = self.cf32_sb[:, 128:256]
            self.pow2 = self.cf32_sb[:, 256:272]
            self.cst = A.carve(o, 128, [16], F32); o += 64
            self.c_off = o
            assert o <= OFF_C + 10240, o
            self.consts()
            for s in range(self.n_seq):
                self.load_x(s)
                try:
                    for l in range(self.depth):
                        self.ffn(l, 0)
                        if self.stage == 'ffn1':
                            break
                        self.mixer(l)
                        self.ffn(l, 1)
                except _Stop:
                    pass
                P.barrier()
                self.final(s)
                P.barrier()
            P.wait_all_dma('sp')
            P.emit(nc, nc.Block())
            for cm in reversed(self.psum_cms):
                cm.__exit__(None, None, None)
        return nc

    def consts(self):
        P = self.P
        P.op('pool', lambda e: e.dma_start(out=self.cbf_sb, in_=self.cbf), writes=['ones_mean'], dma=True)
        P.op('sp', lambda e: e.dma_start(out=self.cf32_sb, in_=self.cf32), writes=['cf32'], dma=True)
        P.op('pool', lambda e: e.memset(self.cst[:, 0:1], EPS), writes=['cst'])
        P.op('pool', lambda e: e.memset(self.cst[:, 1:2], 1.0), writes=['cst'])
        P.op('pool', lambda e: e.memset(self.cst[:, 2:3], 0.0), writes=['cst'])
        g = self.gain_sb
        P.op('sp', lambda e: e.dma_start(out=g, in_=self.gains), writes=['gains'], dma=True)
        sm = self.small_sb
        P.op('sp', lambda e: e.dma_start(out=sm, in_=self.smallp), writes=['smallp'], dma=True)

    def load_x(self, s):
        P = self.P
        for c in range(8):
            src = self.xT[s, c * 128:(c + 1) * 128, :]
            dst = self.X[:, c, :]
            P.op('sp', lambda e, src=src, dst=dst: e.dma_start(out=dst, in_=src),
                 writes=[f'x{c}_{t}' for t in range(NTB)], dma=True)

    def rmsnorm(self, gi, out_fn, tmp_off, t_list=None):
        P = self.P
        A = self.A
        sq = A.carve(tmp_off, 128, [2, 8, TB], BF16)
        rstd = A.carve(tmp_off + 2 * 8 * TB * 2, 128, [2, TB], F32)
        for t in (range(NTB) if t_list is None else t_list):
            k = t % 2
            ts = slice(t * TB, (t + 1) * TB)
            xin = self.X[:, :, ts]
            sqk = sq[:, k]
            P.op('pool', lambda e, xin=xin, sqk=sqk: e.tensor_tensor(out=sqk, in0=xin, in1=xin, op=ALU.mult),
                 reads=[f'x{c}_{t}' for c in range(8)], writes=[f'sq{k}'])
            ps = self.ps[6 + k]
            for c in range(8):
                P.op('pe', lambda e, ps=ps, c=c, sqk=sqk: e.matmul(ps, self.ones_mean, sqk[:, c, :],
                                                                   start=(c == 0), stop=(c == 7)),
                     reads=[f'sq{k}', 'ones_mean'], writes=[f'ps{6 + k}'])
            rk = rstd[:, k]
            P.op('act', lambda e, ps=ps, rk=rk: e.activation(out=rk, in_=ps, func=AF.Ln, bias=self.cst[:, 0:1]),
                 reads=[f'ps{6 + k}', 'cst'], writes=[f'rstd{k}'])
            P.op('act', lambda e, rk=rk: e.activation(out=rk, in_=rk, func=AF.Exp, scale=-0.5),
                 reads=[f'rstd{k}'], writes=[f'rstd{k}'])
            for c in range(8):
                dst, bname = out_fn(c, t)
                xin_c = self.X[:, c, ts]
                gcol = self.gain_sb[:, gi * 8 + c: gi * 8 + c + 1]
                eng = 'dve'
                P.op(eng, lambda e, dst=dst, xin_c=xin_c, gcol=gcol, rk=rk:
                     e.scalar_tensor_tensor(out=dst, in0=xin_c, scalar=gcol, in1=rk, op0=ALU.mult, op1=ALU.mult),
                     reads=[f'x{c}_{t}', f'rstd{k}', 'gains'], writes=[bname])

    def norm_to_H(self, gi, tmp_off):
        self.rmsnorm(gi, lambda c, t: (self.H[:, c, t * TB:(t + 1) * TB], f'h_{t}'), tmp_off)

    def ffn(self, l, which):
        P = self.P
        A = self.A
        gi = l * 3 + (0 if which == 0 else 2)
        o = OFF_A
        actT = A.carve(o, 128, [NFF // 2, S], BF16); o += (NFF // 2) * S * 2
        wgu = [A.carve(o + k * 4096, 128, [8, 2, 128], BF16) for k in range(2)]; o += 8192
        wdn = [A.carve(o + k * 2816, 128, [NFF // 2, 128], BF16) for k in range(2)]; o += 2 * 2816
        sg = [A.carve(o + k * 2048, 128, [TB], F32) for k in range(2)]; o += 4096
        assert o <= SB_BYTES, o
        P.barrier()
        self.norm_to_H(gi, OFF_A)
        P.barrier()
        self.chk(f'f{which}norm')
        w_gu = self.w_gu[which][l].rearrange("(kc p) (two n) -> p kc two n", p=128, two=2)
        w_dn = self.w_dn[which][l].rearrange("(j p) n -> p j n", p=128)
        NH = NFF // 2
        cnt = 0
        for half in range(2):
            for jj in range(NH):
                j = half * NH + jj
                wb = cnt % 2
                dst = wgu[wb]
                for two in range(2):
                    src = w_gu[:, :, two, j * 128:(j + 1) * 128]
                    dd = dst[:, :, two, :]
                    P.op('pool', lambda e, src=src, dd=dd: e.dma_start(out=dd, in_=src),
                         writes=[f'wgu{wb}_{two}'], dma=True)
                for t in range(NTB):
                    pb = (cnt * NTB + t) % 2
                    ts = slice(t * TB, (t + 1) * TB)
                    pg, pu = self.ps[pb], self.ps[2 + pb]
                    for kc in range(8):
                        P.op('pe', lambda e, pg=pg, dst=dst, kc=kc, ts=ts: e.matmul(
                            pg, dst[:, kc, 0, :], self.H[:, kc, ts], start=(kc == 0), stop=(kc == 7)),
                            reads=[f'wgu{wb}_0', f'h_{t}'], writes=[f'ps{pb}'])
                    for kc in range(8):
                        P.op('pe', lambda e, pu=pu, dst=dst, kc=kc, ts=ts: e.matmul(
                            pu, dst[:, kc, 1, :], self.H[:, kc, ts], start=(kc == 0), stop=(kc == 7)),
                            reads=[f'wgu{wb}_1', f'h_{t}'], writes=[f'ps{2 + pb}'])
                    sgk = sg[pb]
                    P.op('act', lambda e, sgk=sgk, pg=pg: e.activation(out=sgk, in_=pg, func=AF.Silu),
                         reads=[f'ps{pb}'], writes=[f'sg{pb}'])
                    adst = actT[:, jj, ts]
                    P.op('dve', lambda e, adst=adst, sgk=sgk, pu=pu: e.tensor_tensor(
                        out=adst, in0=sgk, in1=pu, op=ALU.mult),
                        reads=[f'sg{pb}', f'ps{2 + pb}'], writes=[f'act{jj}_{t}'])
                cnt += 1
            for dc in range(8):
                db = dc % 2
                src = w_dn[:, half * NH:(half + 1) * NH, dc * 128:(dc + 1) * 128]
                dst = wdn[db]
                P.op('pool', lambda e, src=src, dst=dst: e.dma_start(out=dst, in_=src),
                     writes=[f'wdn{db}'], dma=True)
                for t in range(NTB):
                    pb = 4 + (dc * NTB + t) % 2
                    ts = slice(t * TB, (t + 1) * TB)
                    po = self.ps[pb]
                    for jj in range(NH):
                        P.op('pe', lambda e, po=po, dst=dst, jj=jj, ts=ts: e.matmul(
                            po, dst[:, jj, :], actT[:, jj, ts], start=(jj == 0), stop=(jj == NH - 1)),
                            reads=[f'wdn{db}', f'act{jj}_{t}'], writes=[f'ps{pb}'])
                    xd = self.X[:, dc, ts]
                    P.op('dve', lambda e, xd=xd, po=po: e.scalar_tensor_tensor(
                        out=xd, in0=po, scalar=0.5, in1=xd, op0=ALU.mult, op1=ALU.add),
                        reads=[f'ps{pb}', f'x{dc}_{t}'], writes=[f'x{dc}_{t}'])
        self.chk(f'f{which}end')

    def chk(self, name):
        if self.stage.rstrip('XYH') == name:
            raise _Stop()

    def load_w(self, slot, src, ncols, name, dcol=0):
        dst = slot[:, :, dcol:dcol + ncols]
        self.P.op('pool', lambda e, src=src, dst=dst: e.dma_start(out=dst, in_=src), writes=[name], dma=True)

    def proj_T(self, slot, wname, M, evac, banks=(0, 1)):
        P = self.P
        for T in range(NTB):
            b = banks[T % 2]
            ps = self.ps[b]
            ts = slice(T * TB, (T + 1) * TB)
            for kc in range(8):
                P.op('pe', lambda e, ps=ps, kc=kc, ts=ts: e.matmul(ps[0:M, :], slot[:, kc, 0:M], self.H[:, kc, ts],
                                                                  start=(kc == 0), stop=(kc == 7)),
                     reads=[wname, f'h_{T}'], writes=[f'ps{b}'])
            evac(T, ps, f'ps{b}')

    def proj_tok(self, slot, wname, N, evac, banks=(0, 1)):
        P = self.P
        for g in range(4):
            b = banks[g % 2]
            ps = self.ps[b]
            for cc in range(4):
                ch = 4 * g + cc
                for kc in range(8):
                    P.op('pe', lambda e, ps=ps, kc=kc, ch=ch, cc=cc: e.matmul(
                        ps[:, cc * 128:cc * 128 + N], self.H[:, kc, ch * 128:(ch + 1) * 128], slot[:, kc, 0:N],
                        start=(kc == 0), stop=(kc == 7)),
                        reads=[wname, f'h_{ch // 4}'], writes=[f'ps{b}'])
            self.chk('tok_mm')
            evac(g, ps.rearrange("p (a b) -> p a b", a=4), f'ps{b}')
            self.chk('tok_ev')

    def attn_finish(self, O, oname, hh, ychunk, T, normalize, rden, bcs, width=TB):
        P = self.P
        pb = 64 * hh
        ts = slice(T * width, (T + 1) * width)
        ydst = self.Y[pb:pb + 64, ychunk, ts]
        yname = f'y{ychunk}_{hh}_{T}_{width}'
        O = O[:, 0:width]
        rden = rden[:, 0:width]
        bcs = bcs[:, 0:width]
        if not normalize:
            P.op('act', lambda e: e.copy(out=ydst, in_=O[pb:pb + 64, :]), reads=[oname], writes=[yname])
            return
        p = 64 if hh == 0 else 0
        P.op('dve', lambda e: e.reciprocal(out=rden[p:p + 1, :], in_=O[p:p + 1, :]), reads=[oname], writes=['rden'])
        BC = self.ps[6][:, 0:width]
        P.op('pe', lambda e: e.matmul(BC, self.ones_f32[p:p + 1, :], rden[p:p + 1, :], start=True, stop=True),
             reads=['rden'], writes=['ps6'])
        P.op('act', lambda e: e.copy(out=bcs[pb:pb + 64, :], in_=BC[pb:pb + 64, :]), reads=['ps6'], writes=['bcs'])
        P.op('dve', lambda e: e.tensor_tensor(out=ydst, in0=O[pb:pb + 64, :], in1=bcs[pb:pb + 64, :], op=ALU.mult),
             reads=[oname, 'bcs'], writes=[yname])

    def mixer(self, l):
        P = self.P
        A = self.A
        P.barrier()
        self.norm_to_H(l * 3 + 1, OFF_A)
        P.barrier()
        self.chk('norm')
        w_in = self.w_in[l].rearrange("(kc p) n -> p kc n", p=128)
        o = [OFF_A]

        def take(n):
            r = o[0]
            o[0] += n
            assert o[0] <= SB_BYTES, o[0]
            return r
        WSL = [A.carve(take(2048), 128, [8, 128], BF16) for _ in range(6)]
        qT = A.carve(take(4096), 128, [S], BF16)
        kT = A.carve(take(4096), 128, [S], BF16)
        Vp = A.carve(take(16 * 192 * 2), 128, [16, 192], BF16)
        Pt = [A.carve(take(1024), 128, [512], BF16) for _ in range(4)]
        rden = A.carve(take(2048), 128, [512], F32)
        bcs = A.carve(take(2048), 128, [512], F32)
        base_common = o[0]
        self.cnt = 0
        self.ocnt = 0

        P.op('pool', lambda e: e.memset(Vp[:, :, 64:65], 1.0), writes=['Vp_c'])
        P.op('pool', lambda e: e.memset(Vp[:, :, 65:128], 0.0), writes=['Vp_c'])

        def proj_qkv(qcol, kcol, vcol, nc_, wi):
            s0, s1, s2 = WSL[wi], WSL[wi + 1], WSL[wi + 2]
            self.load_w(s0, w_in[:, :, qcol:qcol + nc_], nc_, f'wsl{wi}')
            self.load_w(s1, w_in[:, :, kcol:kcol + nc_], nc_, f'wsl{wi + 1}')
            self.load_w(s2, w_in[:, :, vcol:vcol + nc_], nc_, f'wsl{wi + 2}')
            self.proj_T(s0, f'wsl{wi}', nc_, lambda T, ps, pn: P.op(
                'dve', lambda e: e.tensor_scalar(out=qT[0:nc_, T * TB:(T + 1) * TB], in0=ps[0:nc_, :], scalar1=0.125,
                                                 scalar2=None, op0=ALU.mult),
                reads=[pn], writes=[f'qT_{T}']))
            self.chk('projq')
            self.proj_T(s1, f'wsl{wi + 1}', nc_, lambda T, ps, pn: P.op(
                'dve', lambda e: e.tensor_copy(out=kT[0:nc_, T * TB:(T + 1) * TB], in_=ps[0:nc_, :]),
                reads=[pn], writes=[f'kT_{T}']))
            self.chk('projk')

            import os
            VAR = os.environ.get('EVVAR', 'ab')

            def ev(g, ps3, pn):
                if 'a' in VAR:
                  P.op('dve', lambda e: e.tensor_copy(out=Vp[:, 4 * g:4 * g + 4, 0:64], in_=ps3[:, :, 0:64]),
                     reads=[pn], writes=[f'Vp_{g}a'])
                if nc_ > 64 and 'b' in VAR:
                    P.op('dve', lambda e: e.tensor_copy(out=Vp[:, 4 * g:4 * g + 4, 128:192], in_=ps3[:, :, 64:128]),
                         reads=[pn], writes=[f'Vp_{g}b'])
            self.proj_tok(s2, f'wsl{wi + 2}', nc_, ev)
            self.chk('proj')

        ls = A.carve(take(384), 128, [16, 6], F32)
        tot = A.carve(take(384), 128, [16, 6], F32)
        pre = A.carve(take(17 * 24), 128, [17, 6], F32)
        cpos = A.carve(take(384), 128, [16, 6], F32)
        Btab = A.carve(take(6 * 256 * 4), 128, [6, 16, 16], F32)
        base_fox = o[0]
        self.load_w(WSL[5], w_in[:, :, O_FA:O_FA + 6], 6, 'wsl5')
        ps7 = self.ps[7]
        for ch in range(16):
            for kc in range(8):
                P.op('pe', lambda e, ch=ch, kc=kc: e.matmul(ps7[:, ch * 6:(ch + 1) * 6],
                                                              self.H[:, kc, ch * 128:(ch + 1) * 128],
                                                              WSL[5][:, kc, 0:6], start=(kc == 0), stop=(kc == 7)),
                     reads=['wsl5', f'h_{ch // 4}'], writes=['ps7'])
        lsf = ls.rearrange("p a b -> p (a b)")
        P.op('dve', lambda e: e.tensor_tensor(out=lsf, in0=ps7[:, 0:96], in1=self.small_sb[:, l * 96:(l + 1) * 96],
                                              op=ALU.add), reads=['ps7'], writes=['ls'])
        P.op('act', lambda e: e.activation(out=lsf, in_=lsf, func=AF.Exp, scale=-1.0), reads=['ls'], writes=['ls'])
        P.op('act', lambda e: e.activation(out=lsf, in_=lsf, func=AF.Ln, bias=self.cst[:, 1:2]),
             reads=['ls'], writes=['ls'])
        ps6 = self.ps[6]
        P.op('pe', lambda e: e.matmul(ps6[:, 0:96], self.tri_f32, lsf, start=True, stop=True),
             reads=['ls'], writes=['ps6'])
        P.op('pe', lambda e: e.matmul(ps6[:, 128:224], self.ones_f32, lsf, start=True, stop=True),
             reads=['ls'], writes=['ps6'])
        P.op('dve', lambda e: e.tensor_copy(out=tot.rearrange("p a b -> p (a b)"), in_=ps6[:, 128:224]),
             reads=['ps6'], writes=['tot'])
        P.op('dve', lambda e: e.memset(pre[:, 0, :], 0.0), writes=['pre'])
        for ch in range(1, 17):
            P.op('dve', lambda e, ch=ch: e.tensor_tensor(out=pre[:, ch, :], in0=pre[:, ch - 1, :],
                                                         in1=tot[:, ch - 1, :], op=ALU.add),
                 reads=['tot', 'pre'], writes=['pre'])
        P.op('dve', lambda e: e.tensor_tensor(out=cpos.rearrange("p a b -> p (a b)"), in0=ps6[:, 0:96],
                                              in1=pre[:, 0:16, :].rearrange("p a b -> p (a b)"), op=ALU.add),
             reads=['ps6', 'pre'], writes=['cpos'])
        for h in range(6):
            for tb in range(16):
                P.op('dve', lambda e, h=h, tb=tb: e.tensor_scalar(
                    out=Btab[:, h, tb, :], in0=cpos[:, :, h], scalar1=pre[:, tb + 1, h:h + 1], scalar2=None,
                    op0=ALU.subtract), reads=['cpos', 'pre'], writes=['Btab'])

        self.chk('pre')
        def softmax_attn(hh, ychunk, bias_fn, mask_fn):
            pb = 64 * hh
            for T in range(NTB):
                nsc = 4 * T + 4
                ob = 4 + (self.ocnt % 2)
                self.ocnt += 1
                O = self.ps[ob]
                tinfo = {}

                def S1(sc):
                    zb = 2 + (self.cnt % 2)
                    k = self.cnt % 4
                    self.cnt += 1
                    tinfo[sc] = k
                    Z = self.ps[zb]
                    P.op('pe', lambda e, Z=Z, sc=sc, T=T: e.matmul(
                        Z, kT[pb:pb + 64, sc * 128:(sc + 1) * 128], qT[pb:pb + 64, T * TB:(T + 1) * TB],
                        start=True, stop=True), reads=[f'kT_{sc // 4}', f'qT_{T}'], writes=[f'ps{zb}'])
                    Ptk = Pt[k]
                    for tl in range(4):
                        tb = 4 * T + tl
                        cs = slice(tl * 128, (tl + 1) * 128)
                        pn = f'Pt{k}_{tl}'
                        if tb < sc:
                            P.op('pool', lambda e, Ptk=Ptk, cs=cs: e.memset(Ptk[:, cs], 0.0), writes=[pn])
                            continue
                        bias = bias_fn(sc, tb)
                        P.op('act', lambda e, Ptk=Ptk, cs=cs, Z=Z, bias=bias: e.activation(
                            out=Ptk[:, cs], in_=Z[:, cs], func=AF.Exp, bias=bias),
                            reads=[f'ps{zb}', 'Btab'], writes=[pn])
                        mask_fn(sc, tb, Ptk[:, cs], pn)

                def S2(sc):
                    k = tinfo[sc]
                    Ptk = Pt[k]
                    vs = slice(0, 65) if hh == 0 else slice(64, 192)
                    M = 65 if hh == 0 else 128
                    P.op('pe', lambda e, O=O, sc=sc, Ptk=Ptk, vs=vs, M=M, nsc=nsc: e.matmul(
                        O[0:M, :], Vp[:, sc, vs], Ptk, start=(sc == 0), stop=(sc == nsc - 1)),
                        reads=[f'Vp_{sc // 4}a', f'Vp_{sc // 4}b', 'Vp_c'] + [f'Pt{k}_{tl}' for tl in range(4)], writes=[f'ps{ob}'])

                S1(0)
                for sc in range(nsc):
                    if sc + 1 < nsc:
                        S1(sc + 1)
                    S2(sc)
                    if sc == 0:
                        self.chk('fox_sc0')
                self.chk('fox_T0n')
                self.attn_finish(O, f'ps{ob}', hh, ychunk, T, True, rden, bcs)
                self.chk('fox_T0')

        def fox_mask(sc, tb, ap, pn):
            if sc == tb:
                P.op('pool', lambda e: e.tensor_tensor(out=ap, in0=ap, in1=self.tri_incl, op=ALU.mult),
                     reads=[pn], writes=[pn])

        if True:
            for hp in range(3):
                proj_qkv(O_QA + hp * 128, O_KA + hp * 128, O_VA + hp * 128, 128, 3 * (hp % 2))
                for hh in range(2):
                    h = 2 * hp + hh
                    softmax_attn(hh, hp, lambda sc, tb, h=h: Btab[:, h, tb, sc:sc + 1], fox_mask)
        P.barrier()

        o[0] = base_common
        e32 = A.carve(take(2048), 128, [512], F32)
        spb = [A.carve(take(1024), 128, [512], BF16) for _ in range(2)]
        Ab = [A.carve(take(1024), 128, [512], BF16) for _ in range(2)]
        R = A.carve(take(2048), 128, [512], F32)
        Rb = [A.carve(take(1024), 128, [512], BF16) for _ in range(2)]

        Rb3 = Rb + [A.carve(take(1024), 128, [512], BF16)]

        def sb_attn(hh, ychunk):
            pb = 64 * hh
            for T in range(NTB):
                nsc = 4 * T + 4
                ob = 6 + (T % 2)
                O = self.ps[ob]
                order = list(reversed(range(nsc)))
                info = {}

                def S1(i):
                    sc = order[i]
                    c2 = self.cnt % 2
                    self.cnt += 1
                    info[i] = c2
                    zb = 2 + c2
                    Z = self.ps[zb]
                    kk = kT[pb:pb + 64, sc * 128:(sc + 1) * 128]
                    qq = qT[pb:pb + 64, T * TB:(T + 1) * TB]
                    P.op('pe', lambda e, Z=Z, kk=kk, qq=qq: e.matmul(Z, kk, qq, start=True, stop=True),
                         reads=[f'kT_{sc // 4}', f'qT_{T}'], writes=[f'ps{zb}'])
                    P.op('act', lambda e, Z=Z: e.activation(out=e32, in_=Z, func=AF.Exp),
                         reads=[f'ps{zb}'], writes=['e32'])
                    sp = spb[c2]
                    P.op('act', lambda e, sp=sp: e.activation(out=sp, in_=e32, func=AF.Ln, bias=self.cst[:, 1:2]),
                         reads=['e32'], writes=[f'spb{c2}'])
                    if sc >= 4 * T:
                        m = self.smask[sc - 4 * T]
                        P.op('pool', lambda e, sp=sp, m=m: e.tensor_tensor(out=sp, in0=sp, in1=m, op=ALU.mult),
                             reads=[f'spb{c2}'], writes=[f'spb{c2}'])
                    if sc > 0:
                        if i == 0:
                            P.op('dve', lambda e, sp=sp: e.tensor_copy(out=R, in_=sp), reads=[f'spb{c2}'], writes=['R'])
                        else:
                            P.op('dve', lambda e, sp=sp: e.tensor_tensor(out=R, in0=R, in1=sp, op=ALU.add),
                                 reads=[f'spb{c2}', 'R'], writes=['R'])
                        rb = Rb3[i % 3]
                        P.op('dve', lambda e, rb=rb: e.tensor_copy(out=rb, in_=R), reads=['R'],
                             writes=[f'Rb{i % 3}'])

                def S2(i):
                    sc = order[i]
                    c2 = info[i]
                    first = (i == 0)
                    lb = 4 + c2
                    L = self.ps[lb]
                    kk = kT[pb:pb + 64, sc * 128:(sc + 1) * 128]
                    qq = qT[pb:pb + 64, T * TB:(T + 1) * TB]
                    sp = spb[c2]
                    P.op('pe', lambda e, L=L, kk=kk, qq=qq: e.matmul(L, kk, qq, start=True, stop=False),
                         reads=[f'kT_{sc // 4}', f'qT_{T}'], writes=[f'ps{lb}'])
                    P.op('pe', lambda e, L=L, sp=sp, first=first: e.matmul(L, self.negtri, sp, start=False, stop=first),
                         reads=[f'spb{c2}'], writes=[f'ps{lb}'])
                    if not first:
                        rb = Rb3[(i - 1) % 3]
                        P.op('pe', lambda e, L=L, rb=rb: e.matmul(L, self.negones, rb, start=False, stop=True),
                             reads=[f'Rb{(i - 1) % 3}'], writes=[f'ps{lb}'])
                    ab = Ab[c2]
                    P.op('act', lambda e, ab=ab, L=L: e.activation(out=ab, in_=L, func=AF.Exp),
                         reads=[f'ps{lb}'], writes=[f'Ab{c2}'])
                    if sc >= 4 * T:
                        m = self.smask[sc - 4 * T]
                        P.op('pool', lambda e, ab=ab, m=m: e.tensor_tensor(out=ab, in0=ab, in1=m, op=ALU.mult),
                             reads=[f'Ab{c2}'], writes=[f'Ab{c2}'])

                def S3(i):
                    sc = order[i]
                    c2 = info[i]
                    first = (i == 0)
                    ab = Ab[c2]
                    vs = slice(0, 64) if hh == 0 else slice(64, 192)
                    M = 64 if hh == 0 else 128
                    P.op('pe', lambda e, O=O, sc=sc, ab=ab, vs=vs, M=M, first=first: e.matmul(
                        O[0:M, :], Vp[:, sc, vs], ab, start=first, stop=(sc == 0)),
                        reads=[f'Vp_{sc // 4}a', f'Vp_{sc // 4}b', 'Vp_c', f'Ab{c2}'], writes=[f'ps{ob}'])

                S1(0)
                for i in range(nsc):
                    if i + 1 < nsc:
                        S1(i + 1)
                    S2(i)
                    if i >= 1:
                        S3(i - 1)
                S3(nsc - 1)
                self.attn_finish(O, f'ps{ob}', hh, ychunk, T, False, rden, bcs)

        self.chk('fox')
        if True:
            for hp in range(3):
                nc_ = 128 if hp < 2 else 64
                proj_qkv(O_QB + hp * 128, O_KB + hp * 128, O_VB + hp * 128, nc_, 3 * (hp % 2))
                for hh in range(2 if hp < 2 else 1):
                    sb_attn(hh, 3 + hp)
        P.barrier()
        self.chk('sb')
        self.dsa(l, w_in)
        P.barrier()
        self.chk('dsa')
        self.merge(l, w_in)
        P.barrier()
        self.chk('merge')

    def dsa(self, l, w_in):
        P = self.P
        A = self.A
        o = [OFF_A]

        def take(n):
            r = o[0]
            o[0] += n
            assert o[0] <= SB_BYTES, o[0]
            return r
        qc = [A.carve(take(4096), 128, [S], BF16) for _ in range(3)]
        kcT = A.carve(take(4096), 128, [S], BF16)
        Vc = A.carve(take(16 * 192 * 2), 128, [16, 192], BF16)
        iq = [A.carve(take(4096), 128, [S], BF16) for _ in range(2)]
        ikT = A.carve(take(4096), 128, [S], BF16)
        wI = A.carve(take(256), 128, [16, 4], F32)
        base_d2 = o[0]
        WSL = [A.carve(take(2048), 128, [8, 128], BF16) for _ in range(4)]
        ROPE = A.carve(take(16384), 128, [2, S], F32)
        ckvn = A.carve(take(4096), 128, [S], BF16)
        t32 = [A.carve(take(2048), 128, [512], F32) for _ in range(3)]
        sqb = A.carve(take(1024), 128, [512], BF16)
        COS, SIN = ROPE[:, 0, :], ROPE[:, 1, :]
        P.op('sp', lambda e: e.dma_start(out=ROPE.rearrange("p a b -> p (a b)"), in_=self.rope),
             writes=['rope'], dma=True)
        P.op('pool', lambda e: e.memset(Vc[:, :, 64:65], 1.0), writes=['Vc_c'])
        P.op('pool', lambda e: e.memset(Vc[:, :, 65:128], 0.0), writes=['Vc_c'])
        gkv = self.small_sb[:, 192 + l:193 + l]
        gidx = self.small_sb[:, 194 + l:195 + l]
        gidx_sw = self.small_sb[:, 196 + l:197 + l]

        def rstd_of(src32, T):
            P.op('pool', lambda e: e.tensor_tensor(out=sqb, in0=src32, in1=src32, op=ALU.mult),
                 reads=['t32_0'], writes=['sqb'])
            P.op('pe', lambda e: e.matmul(self.ps[6], self.ones_128th, sqb, start=True, stop=True),
                 reads=['sqb'], writes=['ps6'])
            P.op('act', lambda e: e.activation(out=t32[1], in_=self.ps[6], func=AF.Ln, bias=self.cst[:, 0:1]),
                 reads=['ps6'], writes=['t32_1'])
            P.op('act', lambda e: e.activation(out=t32[1], in_=t32[1], func=AF.Exp, scale=-0.5),
                 reads=['t32_1'], writes=['t32_1'])

        self.load_w(WSL[0], w_in[:, :, O_CKV:O_CKV + 128], 128, 'dw0')

        def ev_ckv(T, ps, pn):
            ts = slice(T * TB, (T + 1) * TB)
            P.op('act', lambda e: e.copy(out=t32[0], in_=ps), reads=[pn], writes=['t32_0'])
            rstd_of(t32[0], T)
            P.op('dve', lambda e: e.scalar_tensor_tensor(out=ckvn[:, ts], in0=t32[0], scalar=gkv, in1=t32[1],
                                                         op0=ALU.mult, op1=ALU.mult),
                 reads=['t32_0', 't32_1'], writes=[f'ckvn_{T}'])
        self.proj_T(WSL[0], 'dw0', 128, ev_ckv)

        wkv = WSL[1]
        src_kv = self.w_kv_up[l]
        P.op('pool', lambda e: e.dma_start(out=wkv[:, 0, :], in_=src_kv), writes=['dw1a'], dma=True)
        P.op('pool', lambda e: e.dma_start(out=wkv[:, 2, 0:64], in_=src_kv[:, 0:64]), writes=['dw1b'], dma=True)
        P.op('pool', lambda e: e.dma_start(out=wkv[:, 2, 64:128], in_=src_kv[:, 0:64]), writes=['dw1c'], dma=True)

        def make_swapped(dst, src, names):
            d4 = dst.rearrange("p k (h d) -> p k h d", h=2)
            s4 = src.rearrange("p k (h d) -> p k h d", h=2)
            P.op('pool', lambda e: e.tensor_copy(out=dst, in_=src), reads=names, writes=['swp'])
            P.op('pool', lambda e: e.tensor_copy(out=d4[:, :, :, 0:8], in_=s4[:, :, :, 8:16]), reads=names, writes=['swp'])
            P.op('pool', lambda e: e.tensor_copy(out=d4[:, :, :, 8:16], in_=s4[:, :, :, 0:8]), reads=names, writes=['swp'])
        make_swapped(wkv[:, 3:4, :], wkv[:, 2:3, :], ['dw1b', 'dw1c'])

        def rope_combine(T, psn, pss, names, dst, dname, pre_n=None, pre_s=None):
            ts = slice(T * TB, (T + 1) * TB)
            P.op('dve', lambda e: e.tensor_tensor(out=t32[0], in0=psn, in1=COS[:, ts], op=ALU.mult),
                 reads=[names[0], 'rope'], writes=['t32_0'])
            P.op('dve', lambda e: e.tensor_tensor(out=t32[2], in0=pss, in1=SIN[:, ts], op=ALU.mult),
                 reads=[names[1], 'rope'], writes=['t32_2'])
            P.op('pool', lambda e: e.tensor_tensor(out=dst[:, ts], in0=t32[0], in1=t32[2], op=ALU.add),
                 reads=['t32_0', 't32_2'], writes=[dname])

        for T in range(NTB):
            ts = slice(T * TB, (T + 1) * TB)
            P.op('pe', lambda e, ts=ts: e.matmul(self.ps[0], wkv[:, 2, :], ckvn[:, ts], start=True, stop=True),
                 reads=['dw1b', 'dw1c', f'ckvn_{T}'], writes=['ps0'])
            P.op('pe', lambda e, ts=ts: e.matmul(self.ps[1], wkv[:, 3, :], ckvn[:, ts], start=True, stop=True),
                 reads=['swp', f'ckvn_{T}'], writes=['ps1'])
            rope_combine(T, self.ps[0], self.ps[1], ['ps0', 'ps1'], kcT, 'kcT')
        for g in range(4):
            b = 2 + g % 2
            ps = self.ps[b]
            for cc in range(4):
                ch = 4 * g + cc
                P.op('pe', lambda e, ps=ps, cc=cc, ch=ch: e.matmul(ps[:, cc * 128:cc * 128 + 64],
                                                                  ckvn[:, ch * 128:(ch + 1) * 128], wkv[:, 0, 64:128],
                                                                  start=True, stop=True),
                     reads=['dw1a', f'ckvn_{g}'], writes=[f'ps{b}'])
            ps3 = ps.rearrange("p (a b) -> p a b", a=4)
            P.op('dve', lambda e, ps3=ps3, g=g: e.tensor_copy(out=Vc[:, 4 * g:4 * g + 4, 0:64], in_=ps3[:, :, 0:64]),
                 reads=[f'ps{b}'], writes=['Vc_a'])
            P.op('dve', lambda e, ps3=ps3, g=g: e.tensor_copy(out=Vc[:, 4 * g:4 * g + 4, 128:192], in_=ps3[:, :, 0:64]),
                 reads=[f'ps{b}'], writes=['Vc_b'])

        def rope_pair(col_lo, col_hi, dst, dname):
            wn, ws = WSL[2], WSL[3]
            if col_hi == col_lo + 64:
                self.load_w(wn, w_in[:, :, col_lo:col_lo + 128], 128, 'dw2a')
                nm = ['dw2a']
            else:
                self.load_w(wn, w_in[:, :, col_lo:col_lo + 64], 64, 'dw2a')
                self.load_w(wn, w_in[:, :, col_hi:col_hi + 64], 64, 'dw2b', dcol=64)
                nm = ['dw2a', 'dw2b']
            make_swapped(ws, wn, nm)
            for T in range(NTB):
                ts = slice(T * TB, (T + 1) * TB)
                for kc in range(8):
                    P.op('pe', lambda e, kc=kc, ts=ts: e.matmul(self.ps[0], wn[:, kc, :], self.H[:, kc, ts],
                                                                start=(kc == 0), stop=(kc == 7)),
                         reads=nm + [f'h_{T}'], writes=['ps0'])
                for kc in range(8):
                    P.op('pe', lambda e, kc=kc, ts=ts: e.matmul(self.ps[1], ws[:, kc, :], self.H[:, kc, ts],
                                                                start=(kc == 0), stop=(kc == 7)),
                         reads=['swp', f'h_{T}'], writes=['ps1'])
                rope_combine(T, self.ps[0], self.ps[1], ['ps0', 'ps1'], dst, dname)

        rope_pair(O_QC, O_QC, qc[0], 'qc0')
        rope_pair(O_QC + 64, O_QC + 128, qc[1], 'qc1')
        rope_pair(O_QC + 192, O_QC + 256, qc[2], 'qc2')
        rope_pair(O_QI, O_QI + 64, iq[0], 'iq0')
        rope_pair(O_QI + 128, O_QI + 192, iq[1], 'iq1')

        wn, ws = WSL[2], WSL[3]
        self.load_w(wn, w_in[:, :, O_KI:O_KI + 64], 64, 'dw2a')
        self.load_w(wn, w_in[:, :, O_KI:O_KI + 64], 64, 'dw2b', dcol=64)
        make_swapped(ws, wn, ['dw2a', 'dw2b'])
        for T in range(NTB):
            ts = slice(T * TB, (T + 1) * TB)
            for kc in range(8):
                P.op('pe', lambda e, kc=kc, ts=ts: e.matmul(self.ps[0], wn[:, kc, :], self.H[:, kc, ts],
                                                            start=(kc == 0), stop=(kc == 7)),
                     reads=['dw2a', 'dw2b', f'h_{T}'], writes=['ps0'])
            for kc in range(8):
                P.op('pe', lambda e, kc=kc, ts=ts: e.matmul(self.ps[1], ws[:, kc, :], self.H[:, kc, ts],
                                                            start=(kc == 0), stop=(kc == 7)),
                     reads=['swp', f'h_{T}'], writes=['ps1'])
            P.op('act', lambda e: e.copy(out=t32[0], in_=self.ps[0]), reads=['ps0'], writes=['t32_0'])
            rstd_of(t32[0], T)
            P.op('dve', lambda e: e.scalar_tensor_tensor(out=t32[0], in0=t32[0], scalar=gidx, in1=t32[1],
                                                         op0=ALU.mult, op1=ALU.mult),
                 reads=['t32_0', 't32_1'], writes=['t32_0'])
            P.op('dve', lambda e: e.scalar_tensor_tensor(out=t32[2], in0=self.ps[1], scalar=gidx_sw, in1=t32[1],
                                                         op0=ALU.mult, op1=ALU.mult),
                 reads=['ps1', 't32_1'], writes=['t32_2'])
            P.op('dve', lambda e, ts=ts: e.tensor_tensor(out=t32[0], in0=t32[0], in1=COS[:, ts], op=ALU.mult),
                 reads=['t32_0', 'rope'], writes=['t32_0'])
            P.op('dve', lambda e, ts=ts: e.tensor_tensor(out=t32[2], in0=t32[2], in1=SIN[:, ts], op=ALU.mult),
                 reads=['t32_2', 'rope'], writes=['t32_2'])
            P.op('pool', lambda e, ts=ts: e.tensor_tensor(out=ikT[:, ts], in0=t32[0], in1=t32[2], op=ALU.add),
                 reads=['t32_0', 't32_2'], writes=['ikT'])

        self.load_w(WSL[0], w_in[:, :, O_WI:O_WI + 4], 4, 'dw0')
        self.proj_tok(WSL[0], 'dw0', 4, lambda g, ps3, pn: P.op(
            'dve', lambda e: e.tensor_copy(out=wI[:, 4 * g:4 * g + 4, :], in_=ps3[:, :, 0:4]),
            reads=[pn], writes=['wI']), banks=(2, 3))
        P.barrier()

        o[0] = base_d2
        sc32 = A.carve(take(8192), 128, [S], F32)
        mask = A.carve(take(4096), 128, [S], BF16)
        maskT = A.carve(take(4096), 128, [16, 128], BF16)
        Pt = [A.carve(take(256), 128, [128], BF16) for _ in range(4)]
        tt = [A.carve(take(2048), 128, [512], F32) for _ in range(2)]
        sm = A.carve(take(256), 128, [64], F32)
        mx, mn, d0, mid, cntc, tmp1, theta = [sm[:, i:i + 1] for i in range(7)]
        halfs = sm[:, 16:32]
        ps7b = self.ps[7].bitcast(BF16)
        maskT2 = [maskT, A.carve(take(4096), 128, [16, 128], BF16)]
        fin = [[A.carve(take(512), 128, [128], F32) for _ in range(3)] for _ in range(6)]
        st = {'cnt': 0, 'ocnt': 0, 'icnt': 0}

        def A1(tb):
            L = (tb + 1) * 128
            qs = slice(tb * 128, (tb + 1) * 128)
            for j in range((L + 511) // 512):
                w = min(512, L - 512 * j)
                cs = slice(512 * j, 512 * j + w)
                for h in range(NIH):
                    pb = 64 * (h % 2)
                    zb = st['icnt'] % 2
                    k2 = st['icnt'] % 2
                    st['icnt'] += 1
                    Z = self.ps[zb]
                    P.op('pe', lambda e, Z=Z, h=h, pb=pb, cs=cs, w=w, qs=qs: e.matmul(
                        Z[:, 0:w], iq[h // 2][pb:pb + 64, qs], ikT[pb:pb + 64, cs], start=True, stop=True),
                        reads=[f'iq{h // 2}', 'ikT'], writes=[f'ps{zb}'])
                    if h == 0:
                        P.op('dve', lambda e, Z=Z, cs=cs, w=w, tb=tb: e.tensor_scalar(
                            out=sc32[:, cs], in0=Z[:, 0:w], scalar1=0.0, scalar2=wI[:, tb, 0:1],
                            op0=ALU.max, op1=ALU.mult), reads=[f'ps{zb}', 'wI'], writes=[f'sc_{j}'])
                    else:
                        P.op('dve', lambda e, Z=Z, w=w, tb=tb, h=h, k2=k2: e.tensor_scalar(
                            out=tt[k2][:, 0:w], in0=Z[:, 0:w], scalar1=0.0, scalar2=wI[:, tb, h:h + 1],
                            op0=ALU.max, op1=ALU.mult), reads=[f'ps{zb}', 'wI'], writes=[f'tt{k2}'])
                        P.op('pool', lambda e, cs=cs, w=w, k2=k2: e.tensor_tensor(
                            out=sc32[:, cs], in0=sc32[:, cs], in1=tt[k2][:, 0:w], op=ALU.add),
                            reads=[f'tt{k2}', f'sc_{j}'], writes=[f'sc_{j}'])
            scn = [f'sc_{j}' for j in range((L + 511) // 512)]
            if tb >= 2:
                P.op('dve', lambda e, L=L: e.tensor_reduce(out=mx, in_=sc32[:, 0:L], axis=mybir.AxisListType.X,
                                                           op=ALU.max), reads=scn, writes=['mx'])
                P.op('dve', lambda e, L=L: e.tensor_reduce(out=mn, in_=sc32[:, 0:L], axis=mybir.AxisListType.X,
                                                           op=ALU.min), reads=scn, writes=['mn'])
            P.op('pool', lambda e, L=L: e.memset(sc32[0:64, L - 64:L], -1e30), reads=scn + ['mx', 'mn'],
                 writes=[scn[-1]])
            return scn

        def Abis(tb, scn):
            L = (tb + 1) * 128
            if tb >= 2:
                P.op('dve', lambda e: e.tensor_tensor(out=d0, in0=mx, in1=mn, op=ALU.subtract),
                     reads=['mx', 'mn'], writes=['d0'])
                P.op('dve', lambda e: e.tensor_scalar(out=halfs, in0=self.pow2, scalar1=d0, scalar2=None,
                                                      op0=ALU.mult), reads=['d0'], writes=['halfs'])
                P.op('dve', lambda e: e.tensor_tensor(out=mid, in0=mn, in1=halfs[:, 0:1], op=ALU.add),
                     reads=['mn', 'halfs'], writes=['mid'])
                for k in range(NBIS):
                    P.op('dve', lambda e, L=L: e.tensor_scalar(out=mask[:, 0:L], in0=sc32[:, 0:L], scalar1=mid,
                                                               scalar2=None, op0=ALU.is_ge, op1=ALU.add,
                                                               accum_out=cntc),
                         reads=scn + ['mid'], writes=['mask', 'cntc'])
                    P.op('dve', lambda e: e.tensor_scalar(out=tmp1, in0=cntc, scalar1=256.0, scalar2=0.5,
                                                          op0=ALU.is_ge, op1=ALU.subtract),
                         reads=['cntc'], writes=['tmp1'])
                    P.op('dve', lambda e, k=k: e.scalar_tensor_tensor(out=mid, in0=tmp1, scalar=halfs[:, k:k + 1],
                                                                      in1=mid, op0=ALU.mult, op1=ALU.add),
                         reads=['tmp1', 'halfs', 'mid'], writes=['mid'])
                P.op('dve', lambda e: e.scalar_tensor_tensor(out=theta, in0=halfs[:, NBIS - 1:NBIS], scalar=-0.5, in1=mid,
                                                             op0=ALU.mult, op1=ALU.add),
                     reads=['mid', 'halfs'], writes=['theta'])
            else:
                P.op('dve', lambda e: e.memset(theta, -1e29), writes=['theta'])

        def A2(tb, scn):
            L = (tb + 1) * 128
            mT = maskT2[tb % 2]
            P.op('dve', lambda e, L=L: e.tensor_scalar(out=mask[:, 0:L], in0=sc32[:, 0:L], scalar1=theta,
                                                       scalar2=None, op0=ALU.is_ge),
                 reads=scn + ['theta'], writes=['mask'])
            for g0 in range(0, tb + 1, 8):
                n = min(8, tb + 1 - g0)
                for i in range(n):
                    sc = g0 + i
                    P.op('pe', lambda e, i=i, sc=sc: e.transpose(ps7b[:, i * 128:(i + 1) * 128],
                                                                 mask[:, sc * 128:(sc + 1) * 128], self.ident),
                         reads=['mask'], writes=['ps7'])
                P.op('act', lambda e, g0=g0, n=n, mT=mT: e.copy(out=mT[:, g0:g0 + n, :].rearrange("p a b -> p (a b)"),
                                                                in_=ps7b[:, 0:n * 128]),
                     reads=['ps7'], writes=[f'maskT{tb % 2}'])

        def Battn(tb):
            qs = slice(tb * 128, (tb + 1) * 128)
            mT = maskT2[tb % 2]
            for hc in range(NH_C):
                i = (hc + 1) // 2
                hh = (hc + 1) % 2
                pb = 64 * hh
                ob = 4 + st['ocnt'] % 2
                fb = st['ocnt'] % 6
                st['ocnt'] += 1
                O = self.ps[ob]
                dinfo = {}

                def D1(sc):
                    zb = 2 + st['cnt'] % 2
                    k = st['cnt'] % 4
                    st['cnt'] += 1
                    dinfo[sc] = k
                    Z = self.ps[zb]
                    P.op('pe', lambda e, Z=Z, sc=sc, i=i, pb=pb, qs=qs: e.matmul(
                        Z[:, 0:128], kcT[pb:pb + 64, sc * 128:(sc + 1) * 128], qc[i][pb:pb + 64, qs],
                        start=True, stop=True), reads=['kcT', f'qc{i}'], writes=[f'ps{zb}'])
                    Ptk = Pt[k]
                    P.op('act', lambda e, Ptk=Ptk, Z=Z: e.activation(out=Ptk, in_=Z[:, 0:128], func=AF.Exp, scale=0.125),
                         reads=[f'ps{zb}'], writes=[f'dPt{k}'])
                    P.op('pool', lambda e, Ptk=Ptk, sc=sc, mT=mT: e.tensor_tensor(out=Ptk, in0=Ptk, in1=mT[:, sc, :],
                                                                                  op=ALU.mult),
                         reads=[f'dPt{k}', f'maskT{tb % 2}'], writes=[f'dPt{k}'])

                def D2(sc):
                    k = dinfo[sc]
                    Ptk = Pt[k]
                    vs = slice(0, 65) if hh == 0 else slice(64, 192)
                    M = 65 if hh == 0 else 128
                    P.op('pe', lambda e, O=O, sc=sc, Ptk=Ptk, vs=vs, M=M, tb=tb: e.matmul(
                        O[0:M, 0:128], Vc[:, sc, vs], Ptk, start=(sc == 0), stop=(sc == tb)),
                        reads=['Vc_a', 'Vc_b', 'Vc_c', f'dPt{k}'], writes=[f'ps{ob}'])

                D1(0)
                for sc in range(tb + 1):
                    if sc + 1 < tb + 1:
                        D1(sc + 1)
                    D2(sc)
                rd, bc_, osb = fin[fb]
                p = 64 if hh == 0 else 0
                P.op('act', lambda e, rd=rd, O=O, p=p: e.activation(out=rd[:, :], in_=O[:, 0:128], func=AF.Ln),
                     reads=[f'ps{ob}'], writes=[f'frd{fb}'])
                P.op('act', lambda e, rd=rd, p=p: e.activation(out=rd[:, :], in_=rd[:, :], func=AF.Exp, scale=-1.0),
                     reads=[f'frd{fb}'], writes=[f'frd{fb}'])
                BC = self.ps[6][:, 0:128]
                P.op('pe', lambda e, rd=rd, p=p, BC=BC: e.matmul(BC, self.ones_f32[p:p + 1, :], rd[p:p + 1, :],
                                                                 start=True, stop=True),
                     reads=[f'frd{fb}'], writes=['ps6'])
                P.op('act', lambda e, bc_=bc_, pb=pb, BC=BC: e.copy(out=bc_[pb:pb + 64, :], in_=BC[pb:pb + 64, :]),
                     reads=['ps6'], writes=[f'fbc{fb}'])
                P.op('act', lambda e, osb=osb, pb=pb, O=O: e.copy(out=osb[pb:pb + 64, :], in_=O[pb:pb + 64, 0:128]),
                     reads=[f'ps{ob}'], writes=[f'fos{fb}'])
                ydst = self.Y[pb:pb + 64, 5 + i, qs]
                P.op('dve', lambda e, ydst=ydst, osb=osb, bc_=bc_, pb=pb: e.tensor_tensor(
                    out=ydst, in0=osb[pb:pb + 64, :], in1=bc_[pb:pb + 64, :], op=ALU.mult),
                    reads=[f'fos{fb}', f'fbc{fb}'], writes=[f'y{5 + i}_{hh}_{tb}_128'])

        scn_cur = A1(0)
        Abis(0, scn_cur)
        A2(0, scn_cur)
        for tb in range(NQB):
            if tb + 1 < NQB:
                scn_nxt = A1(tb + 1)
                Abis(tb + 1, scn_nxt)
            Battn(tb)
            if tb + 1 < NQB:
                A2(tb + 1, scn_nxt)

    def merge(self, l, w_in):
        P = self.P
        A = self.A
        o = [OFF_A]

        def take(n):
            r = o[0]
            o[0] += n
            assert o[0] <= SB_BYTES, o[0]
            return r
        merged = A.carve(take(8 * S * 2), 128, [8, S], BF16)
        wg = [A.carve(take(6144), 128, [8, 384], BF16) for _ in range(2)]
        wu = [A.carve(take(9 * 256), 128, [9, 128], BF16) for _ in range(2)]
        wo = [A.carve(take(2048), 128, [8, 128], BF16) for _ in range(2)]
        sg = [A.carve(take(2048), 128, [512], F32) for _ in range(3)]
        mm = [A.carve(take(2048), 128, [512], F32) for _ in range(3)]
        ufox = self.w_up_fox[l].rearrange("(j p) n -> p j n", p=128)
        usb = self.w_up_sb[l]
        udsa = self.w_up_dsa[l]
        wout = self.w_out[l].rearrange("(kc p) n -> p kc n", p=128)
        mcnt = [0]
        for c in range(8):
            k = c % 2
            cs = slice(c * 128, (c + 1) * 128)
            for b in range(3):
                src = w_in[:, :, O_G + b * D + c * 128:O_G + b * D + (c + 1) * 128]
                dst = wg[k][:, :, b * 128:(b + 1) * 128]
                P.op('pool', lambda e, src=src, dst=dst: e.dma_start(out=dst, in_=src), writes=[f'wg{k}_{b}'], dma=True)
            W = wu[k]
            dl = [
                (W[:, 0:3, :], ufox[:, :, cs]),
                (W[:, 3:5, :], usb[0:256, :].rearrange("(j p) n -> p j n", p=128)[:, :, cs]),
                (W[0:64, 5, :], usb[256:320, cs]),
                (W[64:128, 6, :], udsa[0:64, cs]),
                (W[:, 7:9, :], udsa[64:320, :].rearrange("(j p) n -> p j n", p=128)[:, :, cs]),
            ]
            for i, (dst, src) in enumerate(dl):
                P.op('pool', lambda e, src=src, dst=dst: e.dma_start(out=dst, in_=src), writes=[f'wu{k}_{i}'], dma=True)
            wun = [f'wu{k}_{i}' for i in range(5)]
            for T in range(NTB):
                ts = slice(T * TB, (T + 1) * TB)
                yn = [nm for nm in self.P.bufs if nm.startswith('y') and nm.endswith(f'_{T}')]
                for b in range(3):
                    gb = mcnt[0] % 4
                    ub = 4 + mcnt[0] % 4
                    mcnt[0] += 1
                    G = self.ps[gb]
                    for kc in range(8):
                        P.op('pe', lambda e, G=G, kc=kc, b=b, ts=ts, k=k: e.matmul(
                            G, wg[k][:, kc, b * 128:(b + 1) * 128], self.H[:, kc, ts], start=(kc == 0), stop=(kc == 7)),
                            reads=[f'wg{k}_{b}', f'h_{T}'], writes=[f'ps{gb}'])
                    U = self.ps[ub]
                    if b == 0:
                        terms = [(W[:, j, :], self.Y[:, j, ts]) for j in range(3)]
                    elif b == 1:
                        terms = [(W[:, 3, :], self.Y[:, 3, ts]), (W[:, 4, :], self.Y[:, 4, ts]),
                                 (W[0:64, 5, :], self.Y[0:64, 5, ts])]
                    else:
                        terms = [(W[64:128, 6, :], self.Y[64:128, 5, ts]), (W[:, 7, :], self.Y[:, 6, ts]),
                                 (W[:, 8, :], self.Y[:, 7, ts])]
                    for ti, (lh, rh) in enumerate(terms):
                        P.op('pe', lambda e, U=U, lh=lh, rh=rh, ti=ti: e.matmul(U, lh, rh, start=(ti == 0), stop=(ti == 2)),
                             reads=wun + ['yall'], writes=[f'ps{ub}'])
                    P.op('act', lambda e, G=G, b=b: e.activation(out=sg[b], in_=G, func=AF.Sigmoid),
                         reads=[f'ps{gb}'], writes=[f'sg{b}'])
                    if self.stage.rstrip('XYH') == 'mgd':
                        P.op('dve', lambda e, b=b, ts=ts: e.tensor_copy(out=self.X[:, b, ts], in_=sg[b]),
                             reads=[f'sg{b}'], writes=[f'x{b}_{T}'])
                        P.op('dve', lambda e, b=b, ts=ts, U=U: e.tensor_copy(out=self.X[:, 3 + b, ts], in_=U),
                             reads=[f'ps{ub}'], writes=[f'x{3 + b}_{T}'])
                    P.op('dve', lambda e, U=U, b=b: e.tensor_tensor(out=mm[b], in0=sg[b], in1=U, op=ALU.mult),
                         reads=[f'sg{b}', f'ps{ub}'], writes=[f'mm{b}'])
                P.op('pool', lambda e: e.tensor_tensor(out=mm[0], in0=mm[0], in1=mm[1], op=ALU.add),
                     reads=['mm0', 'mm1'], writes=['mm0'])
                P.op('pool', lambda e, c=c, ts=ts: e.tensor_tensor(out=merged[:, c, ts], in0=mm[0], in1=mm[2], op=ALU.add),
                     reads=['mm0', 'mm2'], writes=[f'mg_{T}'])
                if self.stage.rstrip('XYH') == 'mgd':
                    P.op('dve', lambda e, ts=ts: e.tensor_copy(out=self.X[:, 6, ts], in_=mm[0]), reads=['mm0'], writes=[f'x6_{T}'])
                    P.op('dve', lambda e, ts=ts, c=c: e.tensor_copy(out=self.X[:, 7, ts], in_=merged[:, c, ts]), reads=[f'mg_{T}'], writes=[f'x7_{T}'])
            self.chk('mgd')
        if self.stage.rstrip('XYH') == 'mg':
            for c in range(8):
                P.op('dve', lambda e, c=c: e.tensor_copy(out=self.X[:, c, :], in_=merged[:, c, :]),
                     reads=[f'mg_{T}' for T in range(NTB)], writes=[f'x{c}_{T}' for T in range(NTB)])
            self.chk('mg')
        for c2 in range(8):
            k = c2 % 2
            src = wout[:, :, c2 * 128:(c2 + 1) * 128]
            dst = wo[k]
            P.op('pool', lambda e, src=src, dst=dst: e.dma_start(out=dst, in_=src), writes=[f'wo{k}'], dma=True)
            for T in range(NTB):
                ts = slice(T * TB, (T + 1) * TB)
                pb_ = (c2 * NTB + T) % 2
                po = self.ps[pb_]
                for kc in range(8):
                    P.op('pe', lambda e, po=po, kc=kc, dst=dst, ts=ts: e.matmul(
                        po, dst[:, kc, :], merged[:, kc, ts], start=(kc == 0), stop=(kc == 7)),
                        reads=[f'wo{k}', f'mg_{T}'], writes=[f'ps{pb_}'])
                xd = self.X[:, c2, ts]
                P.op('dve', lambda e, xd=xd, po=po: e.tensor_tensor(out=xd, in0=xd, in1=po, op=ALU.add),
                     reads=[f'ps{pb_}', f'x{c2}_{T}'], writes=[f'x{c2}_{T}'])

    def final(self, s):
        P = self.P
        A = self.A
        gi = 3 * DEPTH
        o = OFF_A
        stg = [A.carve(o + k * 16384, 128, [8, TB], F32) for k in range(2)]; o += 32768
        tmp_off = o
        for t in range(NTB):
            k = t % 2
            if self.stage[-1] in 'XYH':
                SRC = {'X': self.X, 'Y': self.Y, 'H': self.H}[self.stage[-1]]
                for c in range(8):
                    P.op('dve', lambda e, c=c, t=t, k=k: e.tensor_copy(out=stg[k][:, c, :], in_=SRC[:, c, t * TB:(t + 1) * TB]),
                         reads=[f'x{c}_{t}'], writes=[f'stg{k}_{c}'])
            else:
                self.rmsnorm(gi, lambda c, t, k=k: (stg[k][:, c, :], f'stg{k}_{c}'), tmp_off, t_list=[t])
            for c in range(8):
                dst = self.outT[s, c * 128:(c + 1) * 128, t * TB:(t + 1) * TB]
                src = stg[k][:, c, :]
                P.op('sp', lambda e, src=src, dst=dst: e.dma_start(out=dst, in_=src),
                     reads=[f'stg{k}_{c}'], dma=True)


def pack_gains(inp):
    g = np.zeros((128, NGAIN), np.float32)
    for l in range(DEPTH):
        for k, name in enumerate(("g_ffn1", "g_mix", "g_ffn2")):
            g[:, (l * 3 + k) * 8:(l * 3 + k + 1) * 8] = np.asarray(inp[name][l], np.float32).reshape(8, 128).T
    g[:, 3 * DEPTH * 8:] = np.asarray(inp["g_final"], np.float32).reshape(8, 128).T
    return g


def rope_consts():
    pos = np.arange(S, dtype=np.float32)
    inv = (np.float32(500000.0) ** (-np.arange(0, 16, 2, dtype=np.float32) / np.float32(16))).astype(np.float32)
    ang = pos[None, :] * inv[:, None]
    cos, sin = np.cos(ang).astype(np.float32), np.sin(ang).astype(np.float32)
    r = np.zeros((128, 2, S), np.float32)
    r[:, 0, :] = 1.0
    for p in range(128):
        d = p % 64
        if d < 16:
            r[p, 0] = cos[d % 8]
            r[p, 1] = -sin[d % 8] if d < 8 else sin[d % 8]
    return r.reshape(128, 2 * S)


def const_tables():
    i = np.arange(128)
    cbf = np.zeros((128, NCBF), np.float32)
    cbf[:, 0:128] = (i[:, None] <= i[None, :])
    for k in range(4):
        m = np.zeros((128, 4, 128), np.float32)
        m[:, k, :] = (i[:, None] < i[None, :])
        m[:, k + 1:, :] = 1.0
        cbf[:, 128 + 512 * k:128 + 512 * (k + 1)] = m.reshape(128, 512)
    cbf[:, 2176:2304] = -(i[:, None] >= i[None, :]).astype(np.float32)
    cbf[:, 2304:2432] = -1.0
    cbf[:, 2432:2560] = np.eye(128, dtype=np.float32)
    cbf[:, 2560:2688] = 1.0 / 1024
    cbf[:, 2688:2816] = 1.0 / 128
    cbf[:, 2816:2944] = 1.0 / 64
    cf = np.zeros((128, NCF32), np.float32)
    cf[:, 0:128] = (i[:, None] <= i[None, :])
    cf[:, 128:256] = 1.0
    cf[:, 256:272] = 2.0 ** -(np.arange(16) + 1.0)
    return cbf, cf


def make_in_maps(inp, n_cores, n_seq):
    x = np.asarray(inp["x"], np.float32)
    gains = pack_gains(inp)
    smallp = np.zeros((128, 256), np.float32)
    for l in range(DEPTH):
        smallp[:, l * 96:(l + 1) * 96] = np.tile(np.asarray(inp["b_forget"][l], np.float32), 16)[None, :]
        smallp[:, 192 + l] = np.asarray(inp["g_kv_latent"][l], np.float32)
        smallp[:64, 194 + l] = np.asarray(inp["g_idx_k"][l], np.float32)
        smallp[64:, 194 + l] = np.asarray(inp["g_idx_k"][l], np.float32)
        gsw = np.asarray(inp["g_idx_k"][l], np.float32).copy()
        gsw[0:8], gsw[8:16] = gsw[8:16].copy(), gsw[0:8].copy()
        smallp[:64, 196 + l] = gsw
        smallp[64:, 196 + l] = gsw
    rope = rope_consts()
    cbf, cf32 = const_tables()
    maps = []
    shared = {
        "w_ffn1_gu": np.asarray(inp["w_ffn1_gu"], np.float32), "w_ffn2_gu": np.asarray(inp["w_ffn2_gu"], np.float32),
        "w_ffn1_down": np.asarray(inp["w_ffn1_down"], np.float32),
        "w_ffn2_down": np.asarray(inp["w_ffn2_down"], np.float32),
        "w_in": np.asarray(inp["w_in"], np.float32), "w_kv_up": np.asarray(inp["w_kv_up"], np.float32),
        "w_up_fox": np.asarray(inp["w_up_fox"], np.float32), "w_up_sb": np.asarray(inp["w_up_sb"], np.float32),
        "w_up_dsa": np.asarray(inp["w_up_dsa"], np.float32), "w_out": np.asarray(inp["w_out"], np.float32),
        "gains": gains, "smallp": smallp, "rope": rope, "cbf": cbf, "cf32": cf32,
    }
    for cidx in range(n_cores):
        xs = x[cidx * n_seq:(cidx + 1) * n_seq]
        m = dict(shared)
        m["xT"] = np.ascontiguousarray(xs.transpose(0, 2, 1))
        maps.append(m)
    return maps


def kernel(**inputs):
    n_cores, n_seq = 8, 2
    mdl = Model(n_seq=n_seq)
    nc = mdl.build()
    maps = make_in_maps(inputs, n_cores, n_seq)
    res = run_bass_kernel_spmd(nc, maps, core_ids=list(range(n_cores)))
    outs = [r["outT"].transpose(0, 2, 1) for r in res.results]
    return np.ascontiguousarray(np.concatenate(outs, axis=0)).astype(np.float32)
```
